# Optimizing a Trainium2 kernel written in Bass

```python
import jax, jax.numpy as jnp
from jax import lax
import numpy as np

D_MODEL = 2048
BATCH = 2
SEQ = 16384
DEPTH = 1

MEM_LEN = 256
EPS = 1e-6
POOL_WINDOWS = (2, 4, 8, 16)
POOL_GROUP_DIM = D_MODEL // 8
POOL_DIM = POOL_GROUP_DIM * len(POOL_WINDOWS)
HGRN_DK = 128
HGRN_DV = 128
HGRN_HEADS = D_MODEL // 256
HGRN_DIM = HGRN_HEADS * HGRN_DK
MIX_DIM = POOL_DIM + HGRN_DIM
IN_DIM = POOL_DIM + 4 * HGRN_DIM
CHUNK = 64
XATTN_HEADS = 4
XATTN_HEAD_DIM = D_MODEL // XATTN_HEADS
N_GROUPS = 4
EXPERTS_PER_GROUP = 4
N_EXPERTS = N_GROUPS * EXPERTS_PER_GROUP
TOP_K_INNER = 2
D_EXPERT = D_MODEL // 4

kernel_name = "hybrid_pool_hgrn2_memxattn_hmoe"


def rmsnorm(x, gain):
    xf = x.astype(jnp.float32)
    y = xf * lax.rsqrt(jnp.mean(xf * xf, axis=-1, keepdims=True) + EPS)
    return (y * gain.astype(jnp.float32)).astype(x.dtype)


def causal_pool_mixer(u, pool_w, pool_scale):
    b, s, _ = u.shape
    uf = u.astype(jnp.float32)
    cs = jnp.concatenate([jnp.zeros((b, 1, POOL_DIM), jnp.float32), jnp.cumsum(uf, axis=1)], axis=1)
    t_plus_1 = jnp.arange(1, s + 1, dtype=jnp.int32)
    outs = []
    for g, w in enumerate(POOL_WINDOWS):
        sl = slice(g * POOL_GROUP_DIM, (g + 1) * POOL_GROUP_DIM)
        c = cs[:, :, sl]
        upper = c[:, 1:]
        lower = jnp.pad(c[:, : s + 1 - w], ((0, 0), (w - 1, 0), (0, 0)))
        count = jnp.minimum(t_plus_1, w).astype(jnp.float32)[None, :, None]
        pooled = (upper - lower) / count - uf[:, :, sl]
        outs.append(jnp.einsum('bsc,cd->bsd', pooled.astype(u.dtype), pool_w[g]))
    return (jnp.concatenate(outs, axis=-1) * pool_scale).astype(u.dtype)


def hgrn2_mixer(q_in, f_in, i_in, g_in, lb, norm_gain):
    b, s, _ = q_in.shape
    n_chunks = s // CHUNK
    q = jax.nn.silu(q_in.astype(jnp.float32))
    lb = lb.astype(jnp.float32)
    log_f = jnp.logaddexp(jnp.log(lb), jnp.log1p(-lb) + jax.nn.log_sigmoid(f_in.astype(jnp.float32)))
    k = -jnp.expm1(log_f)
    v = i_in.astype(jnp.float32)

    def to_chunks(t, d):
        return t.reshape(b, n_chunks, CHUNK, HGRN_HEADS, d).transpose(1, 0, 3, 2, 4)

    causal = jnp.tril(jnp.ones((CHUNK, CHUNK), dtype=bool))[None, None, :, :, None]

    def step(state, xs):
        qc, lfc, kc, vc = xs
        a = jnp.cumsum(lfc, axis=2)
        rel = a[:, :, :, None, :] - a[:, :, None, :, :]
        decay = jnp.exp(jnp.where(causal, rel, -jnp.inf))
        scores = jnp.einsum('bhtd,bhtsd,bhsd->bhts', qc, decay, kc)
        o = jnp.einsum('bhts,bhsv->bhtv', scores, vc) + jnp.einsum('bhtd,bhdv->bhtv', qc * jnp.exp(a), state)
        a_last = a[:, :, -1:, :]
        new_state = jnp.exp(a_last[:, :, 0, :])[..., None] * state + jnp.einsum(
            'bhsd,bhsv->bhdv', kc * jnp.exp(a_last - a), vc)
        return new_state, o

    s0 = jnp.zeros((b, HGRN_HEADS, HGRN_DK, HGRN_DV), jnp.float32)
    _, o = lax.scan(step, s0, (to_chunks(q, HGRN_DK), to_chunks(log_f, HGRN_DK),
                               to_chunks(k, HGRN_DK), to_chunks(v, HGRN_DV)))
    o = o.transpose(1, 0, 3, 2, 4).reshape(b, s, HGRN_HEADS, HGRN_DV)
    o = rmsnorm(o, norm_gain)
    gate = jax.nn.silu(g_in.astype(jnp.float32)).reshape(b, s, HGRN_HEADS, HGRN_DV)
    return (o * gate).reshape(b, s, HGRN_HEADS * HGRN_DV).astype(q_in.dtype)


def memory_cross_attention(h, mem_n, wq, wk, wv, wo):
    b, s, _ = h.shape
    m = mem_n.shape[1]
    q = (h @ wq).reshape(b, s, XATTN_HEADS, XATTN_HEAD_DIM)
    k = (mem_n @ wk).reshape(b, m, XATTN_HEADS, XATTN_HEAD_DIM)
    v = (mem_n @ wv).reshape(b, m, XATTN_HEADS, XATTN_HEAD_DIM)
    scores = jnp.einsum('bshd,bmhd->bhsm', q, k).astype(jnp.float32) * (XATTN_HEAD_DIM ** -0.5)
    p = jax.nn.softmax(scores, axis=-1).astype(v.dtype)
    o = jnp.einsum('bhsm,bmhd->bshd', p, v).reshape(b, s, D_MODEL)
    return o @ wo


def hierarchical_moe(h, router_group, router_expert, w_gate, w_up, w_down):
    b, s, _ = h.shape
    p_group = jax.nn.softmax(jnp.einsum('bsd,dg->bsg', h, router_group).astype(jnp.float32), axis=-1)
    pg_top, g_idx = lax.top_k(p_group, 1)
    logits_e = jnp.einsum('bsd,de->bse', h, router_expert).astype(jnp.float32)
    logits_e = logits_e.reshape(b, s, N_GROUPS, EXPERTS_PER_GROUP)
    g_onehot = jax.nn.one_hot(g_idx[..., 0], N_GROUPS, dtype=jnp.float32)
    sel = jnp.einsum('bsg,bsge->bse', g_onehot, logits_e)
    top_vals, top_idx = lax.top_k(sel, TOP_K_INNER)
    w_inner = jax.nn.softmax(top_vals, axis=-1)
    expert_id = g_idx * EXPERTS_PER_GROUP + top_idx
    combine = jnp.sum(jax.nn.one_hot(expert_id, N_EXPERTS, dtype=jnp.float32)
                      * (pg_top * w_inner)[..., None], axis=2).astype(h.dtype)
    y = jnp.zeros_like(h)
    for e in range(N_EXPERTS):
        hid = jax.nn.silu(h @ w_gate[e]) * (h @ w_up[e])
        y = y + combine[..., e:e + 1] * (hid @ w_down[e])
    return y


def setup_inputs(seed: int = 0) -> dict:
    key = jax.random.key(seed)
    ks = jax.random.split(key, 24)
    f32 = jnp.float32

    def nrm(k, shape, scale):
        return jax.random.normal(k, shape, f32) * scale

    def gain(k, shape):
        return 1.0 + 0.02 * jax.random.normal(k, shape, f32)

    return {
        "x": nrm(ks[0], (BATCH, SEQ, D_MODEL), 1.0),
        "mem": nrm(ks[1], (BATCH, MEM_LEN, D_MODEL), 1.0),
        "norm_mix": gain(ks[2], (DEPTH, D_MODEL)),
        "w_in": nrm(ks[3], (DEPTH, D_MODEL, IN_DIM), D_MODEL ** -0.5),
        "pool_w": nrm(ks[4], (DEPTH, len(POOL_WINDOWS), POOL_GROUP_DIM, POOL_GROUP_DIM), POOL_GROUP_DIM ** -0.5),
        "pool_scale": gain(ks[5], (DEPTH, POOL_DIM)),
        "hgrn_lb_logits": nrm(ks[6], (DEPTH + 1, HGRN_DIM), 0.5),
        "hgrn_norm": gain(ks[7], (DEPTH, HGRN_DV)),
        "w_out": nrm(ks[8], (DEPTH, MIX_DIM, D_MODEL), MIX_DIM ** -0.5),
        "norm_xattn": gain(ks[9], (DEPTH, D_MODEL)),
        "norm_mem": gain(ks[10], (DEPTH, D_MODEL)),
        "xattn_wq": nrm(ks[11], (DEPTH, D_MODEL, D_MODEL), D_MODEL ** -0.5),
        "xattn_wk": nrm(ks[12], (DEPTH, D_MODEL, D_MODEL), D_MODEL ** -0.5),
        "xattn_wv": nrm(ks[13], (DEPTH, D_MODEL, D_MODEL), D_MODEL ** -0.5),
        "xattn_wo": nrm(ks[14], (DEPTH, D_MODEL, D_MODEL), D_MODEL ** -0.5),
        "norm_ffn": gain(ks[15], (DEPTH, D_MODEL)),
        "router_group": nrm(ks[16], (DEPTH, D_MODEL, N_GROUPS), D_MODEL ** -0.5),
        "router_expert": nrm(ks[17], (DEPTH, D_MODEL, N_EXPERTS), D_MODEL ** -0.5),
        "w_gate": nrm(ks[18], (DEPTH, N_EXPERTS, D_MODEL, D_EXPERT), D_MODEL ** -0.5),
        "w_up": nrm(ks[19], (DEPTH, N_EXPERTS, D_MODEL, D_EXPERT), D_MODEL ** -0.5),
        "w_down": nrm(ks[20], (DEPTH, N_EXPERTS, D_EXPERT, D_MODEL), D_EXPERT ** -0.5),
        "norm_final": gain(ks[21], (D_MODEL,)),
    }


def reference(x, mem, norm_mix, w_in, pool_w, pool_scale, hgrn_lb_logits, hgrn_norm, w_out,
              norm_xattn, norm_mem, xattn_wq, xattn_wk, xattn_wv, xattn_wo, norm_ffn,
              router_group, router_expert, w_gate, w_up, w_down, norm_final):
    lower_bounds = jnp.cumsum(jax.nn.softmax(hgrn_lb_logits.astype(jnp.float32), axis=0), axis=0)
    splits = [POOL_DIM, POOL_DIM + HGRN_DIM, POOL_DIM + 2 * HGRN_DIM, POOL_DIM + 3 * HGRN_DIM]
    for l in range(DEPTH):
        h = rmsnorm(x, norm_mix[l])
        proj = h @ w_in[l]
        u, q_in, f_in, i_in, g_in = jnp.split(proj, splits, axis=-1)
        pool_out = causal_pool_mixer(u, pool_w[l], pool_scale[l])
        hgrn_out = hgrn2_mixer(q_in, f_in, i_in, g_in, lower_bounds[l], hgrn_norm[l])
        x = x + jnp.concatenate([pool_out, hgrn_out], axis=-1) @ w_out[l]
        h = rmsnorm(x, norm_xattn[l])
        m = rmsnorm(mem, norm_mem[l])
        x = x + memory_cross_attention(h, m, xattn_wq[l], xattn_wk[l], xattn_wv[l], xattn_wo[l])
        h = rmsnorm(x, norm_ffn[l])
        x = x + hierarchical_moe(h, router_group[l], router_expert[l], w_gate[l], w_up[l], w_down[l])
    return rmsnorm(x, norm_final)
```

```python
import os
import numpy as np
import concourse.bass as bass
import concourse.mybir as mybir
from concourse.bass_utils import run_bass_kernel_spmd

F32 = mybir.dt.float32
BF16 = mybir.dt.bfloat16
U32 = mybir.dt.uint32
AF = mybir.ActivationFunctionType
ALU = mybir.AluOpType
AX = mybir.AxisListType

ENGS = ["pe", "act", "dve", "pool", "sp"]
N_DMA_SEMS = 32
D = 2048
NCH = 16
EPS = 1e-6
HALO = 512


class Buf:
    __slots__ = ("name", "w", "r")

    def __init__(self, name=""):
        self.name = name
        self.w = {}
        self.r = {}


class Sched:
    def __init__(self, nc):
        self.nc = nc
        self.prog = {e: [] for e in ENGS}
        self.cnt = {e: 0 for e in ENGS}
        self.seen = {e: {} for e in ENGS}
        self.sem = {e: nc.alloc_semaphore("sem_" + e) for e in ENGS}
        self.dsem = [nc.alloc_semaphore("dsem%d" % i) for i in range(N_DMA_SEMS)]
        self.dcnt = [0] * N_DMA_SEMS
        self.dnext2 = [0, 0]
        self.mute = False
        self.touched = set()

    def _deps(self, reads, writes):
        deps = {}
        for b in reads:
            for k, n in b.w.items():
                if deps.get(k, 0) < n:
                    deps[k] = n
        for b in writes:
            for d in (b.w, b.r):
                for k, n in d.items():
                    if deps.get(k, 0) < n:
                        deps[k] = n
        return deps

    def _waits(self, eng, deps):
        waits = []
        seen = self.seen[eng]
        for k, n in deps.items():
            if k == "pe" and eng == "pe":
                continue
            if seen.get(k, 0) < n:
                seen[k] = n
                waits.append((k, n))
        return waits

    def _semof(self, k):
        if isinstance(k, tuple):
            return self.dsem[k[1]], 16
        return self.sem[k], 1

    def op(self, eng, emit, reads=(), writes=()):
        if self.mute:
            return
        deps = self._deps(reads, writes)
        waits = self._waits(eng, deps)
        self.cnt[eng] += 1
        my = self.cnt[eng]
        self.prog[eng].append((waits, emit, self.sem[eng], 1))
        self.touched.update(reads)
        self.touched.update(writes)
        for b in writes:
            b.w = {eng: my}
            b.r = {}
        for b in reads:
            if b.r.get(eng, 0) < my:
                b.r[eng] = my

    def dma(self, eng, emit, reads=(), writes=()):
        if self.mute:
            return
        half = N_DMA_SEMS // 2
        q = 1 if eng == "pool" else 0
        i = q * half + self.dnext2[q]
        self.dnext2[q] = (self.dnext2[q] + 1) % half
        key = ("dma", i)
        deps = self._deps(reads, writes)
        if self.dcnt[i] > 0:
            deps[key] = max(deps.get(key, 0), self.dcnt[i])
        waits = self._waits(eng, deps)
        self.dcnt[i] += 1
        my = self.dcnt[i]
        self.prog[eng].append((waits, emit, self.dsem[i], 16))
        self.touched.update(reads)
        self.touched.update(writes)
        for b in writes:
            b.w = {key: my}
            b.r = {}
        for b in reads:
            if b.r.get(key, 0) < my:
                b.r[key] = my

    def final_wait(self, eng, bufs):
        deps = {}
        for b in bufs:
            for k, n in b.w.items():
                deps[k] = max(deps.get(k, 0), n)
        waits = self._waits(eng, deps)
        self.prog[eng].append((waits, None, None, 0))

    def barrier(self):
        deps = {e: self.cnt[e] for e in ENGS if self.cnt[e] > 0}
        for i in range(N_DMA_SEMS):
            if self.dcnt[i] > 0:
                deps[("dma", i)] = self.dcnt[i]
        for eng in ENGS:
            waits = []
            seen = self.seen[eng]
            for k, n in deps.items():
                if k == eng:
                    continue
                if seen.get(k, 0) < n:
                    seen[k] = n
                    waits.append((k, n))
            self.prog[eng].append((waits, None, None, 0))

    def emit_all(self):
        nc = self.nc

        def run(e, name):
            for waits, emit, sem, inc in self.prog[name]:
                for k, n in waits:
                    s, mult = self._semof(k)
                    e.wait_ge(s, n * mult)
                if emit is not None:
                    emit(e).then_inc(sem, inc)

        with nc.Block() as block:
            @block.tensor
            def _(e):
                run(e, "pe")

            @block.scalar
            def _(e):
                run(e, "act")

            @block.vector
            def _(e):
                run(e, "dve")

            @block.gpsimd
            def _(e):
                run(e, "pool")

            @block.sync
            def _(e):
                run(e, "sp")
        self.prog = {e: [] for e in ENGS}
        for b in self.touched:
            b.w = {}
            b.r = {}
        self.touched = set()
        self.cnt = {e: 0 for e in ENGS}
        self.seen = {e: {} for e in ENGS}
        self.dcnt = [0] * N_DMA_SEMS


class Ring:
    def __init__(self, items):
        self.items = items
        self.i = 0

    def next(self):
        it = self.items[self.i % len(self.items)]
        self.i += 1
        return it


def build(T, W, dbg=False, limit=99):
    nc = bass.Bass("TRN2", target_bir_lowering=False)
    S = Sched(nc)
    NBM = (W + T) // 512
    HB = W // 512
    NB = T // 512
    EB = 1024 if T % 1024 == 0 else 512
    ETI = EB // 128

    uid = [0]

    def sb(name, shape, dt):
        uid[0] += 1
        return nc.alloc_sbuf_tensor("%s_%d" % (name, uid[0]), shape, dt)

    def din(name, shape, dt=F32):
        return nc.dram_tensor(name, list(shape), dt, kind="ExternalInput").ap()

    xseg = din("xseg", [W + T, D])
    mem = din("mem", [256, D])
    w_in = din("w_in", [D, 5120])
    pool_w = din("pool_w", [4, 256, 256])
    w_out = din("w_out", [D, D])
    wq = din("wq", [D, D])
    wk = din("wk", [D, D])
    wv = din("wv", [D, D])
    wo = din("wo", [D, D])
    wr = din("wr", [128, NCH * 20])
    w_gate = din("w_gate", [16, D, 512])
    w_up = din("w_up", [16, D, 512])
    w_down = din("w_down", [16, 512, D])
    gains = din("gains", [5, D])
    pscale = din("pscale", [128, 8])
    lbl = din("lbl", [128, 16])
    hnorm = din("hnorm", [128, 1])
    ident = din("ident", [128, 128])
    bdmask = din("bdmask", [128, 128], U32)
    rmask = din("rmask", [128, 512])
    invcnt = din("invcnt", [128, 4 * 512])
    out = nc.dram_tensor("out", [T, D], F32, kind="ExternalOutput").ap()
    if dbg:
        x1d = nc.dram_tensor("x1d", [T, D], F32, kind="ExternalOutput").ap()
        x2d = nc.dram_tensor("x2d", [T, D], F32, kind="ExternalOutput").ap()

    hT1 = nc.dram_tensor("hT1", [NBM, 128, NCH * 512], BF16).ap()
    x1s = nc.dram_tensor("x1s", [T, D], F32).ap()
    aTs = nc.dram_tensor("aTs", [NB, 128, NCH * 512], BF16).ap()
    x2s = nc.dram_tensor("x2s", [T, D], F32).ap()
    h3Ts = nc.dram_tensor("h3Ts", [NB, 128, NCH * 512], BF16).ap()
    combs = nc.dram_tensor("combs", [T, 16], F32).ap()
    b_hT1 = [Buf() for _ in range(NBM)]
    b_x1 = [Buf() for _ in range(T // 128)]
    b_aT = [Buf() for _ in range(NB)]
    b_x2 = [Buf() for _ in range(T // 128)]
    b_h3T = [Buf() for _ in range(NB)]
    b_comb = [Buf() for _ in range(T // 128)]
    outs = []

    PS = [nc.alloc_psum_tensor("ps%d" % i, [128, 512], F32) for i in range(8)]
    BPS = [Buf("ps%d" % i) for i in range(8)]

    def psring(idx):
        return Ring([(PS[i], BPS[i]) for i in idx])

    id_f = sb("id_f", [128, 128], F32)
    id_b = sb("id_b", [128, 128], BF16)
    ones_f = sb("ones_f", [128, 128], F32)
    eps_t = sb("eps_t", [128, 1], F32)
    bconst = Buf("const")
    S.dma("sp", lambda e: e.dma_start(out=id_f[:], in_=ident), writes=[bconst])
    S.dma("pool", lambda e: e.dma_start(out=id_b[:], in_=ident), writes=[bconst])
    S.op("dve", lambda e: e.memset(ones_f[:], 1.0), writes=[bconst])
    S.op("dve", lambda e: e.memset(eps_t[:], EPS), writes=[bconst])

    flip = [0]

    def evac_copy(dst, src, reads, writes):
        flip[0] ^= 1
        if flip[0]:
            S.op("act", lambda e: e.activation(out=dst, in_=src, func=AF.Copy), reads=reads, writes=writes)
        else:
            S.op("dve", lambda e: e.tensor_copy(out=dst, in_=src), reads=reads, writes=writes)

    def mm_group(ps_ap, bps, pairs, reads):
        n = len(pairs)
        for i, (l, r) in enumerate(pairs):
            S.op("pe", lambda e, l=l, r=r, i=i: e.matmul(ps_ap, lhsT=l, rhs=r, start=(i == 0), stop=(i == n - 1)),
                 reads=reads, writes=[bps])

    class NormT:
        def __init__(self, tag, banks):
            self.tmp = Ring([(sb(tag + "junk%d" % i, [128, D], BF16), sb(tag + "xn%d" % i, [128, D], F32), sb(tag + "st%d" % i, [128, 4], F32),
                              Buf(), Buf(), Buf()) for i in range(2)])
            self.gB = sb(tag + "gB", [128, D], F32)
            self.bg = Buf()
            self.ring = psring(banks)

        def load_gain(self, row):
            S.dma("sp", lambda e: e.dma_start(out=self.gB[:], in_=gains[row:row + 1, :].to_broadcast([128, D])), writes=[self.bg])

        def run(self, src, bsrc, dst_bf, bdst, dst_f=None, bdst_f=None):
            junk, xn, st, bj, bxn, bst = self.tmp.next()
            S.op("act", lambda e: e.activation(out=junk[:], in_=src, func=AF.Square, accum_out=st[:, 0:1]),
                 reads=[bsrc], writes=[bj, bst])
            S.op("act", lambda e: e.activation(out=st[:, 1:2], in_=st[:, 0:1], func=AF.Sqrt, scale=1.0 / D, bias=eps_t[:]),
                 reads=[bst, bconst], writes=[bst])
            S.op("dve", lambda e: e.reciprocal(out=st[:, 2:3], in_=st[:, 1:2]), reads=[bst], writes=[bst])
            S.op("dve", lambda e: e.scalar_tensor_tensor(out=xn[:], in0=src, scalar=st[:, 2:3], in1=self.gB[:],
                                                         op0=ALU.mult, op1=ALU.mult),
                 reads=[bsrc, bst, self.bg], writes=[bxn])
            for b4 in range(4):
                ps, bps = self.ring.next()
                for j in range(4):
                    c = b4 * 4 + j
                    S.op("pe", lambda e, ps=ps, j=j, c=c: e.transpose(ps[:, j * 128:(j + 1) * 128], xn[:, c * 128:(c + 1) * 128], id_f[:]),
                         reads=[bxn, bconst], writes=[bps])
                src3 = ps[:].rearrange("p (c n) -> p c n", c=4)
                if dst_f is None:
                    evac_copy(dst_bf[:, b4 * 4:(b4 + 1) * 4, :], src3, [bps], [bdst])
                else:
                    evac_copy(dst_f[:, b4 * 4:(b4 + 1) * 4, :], src3, [bps], [bdst_f])
                    S.op("pool", lambda e, b4=b4: e.tensor_copy(out=dst_bf[:, b4 * 4:(b4 + 1) * 4, :], in_=dst_f[:, b4 * 4:(b4 + 1) * 4, :]),
                         reads=[bdst_f], writes=[bdst])

    def wslice(w, r0, r1, c0, c1):
        return w[r0:r1, c0:c1].rearrange("(c p) n -> p c n", p=128)

    S.mute = limit < 1
    with nc.reset_on_exit():
        nt = NormT("n1", [0, 1, 2, 3, 4, 5, 6, 7])
        nt.load_gain(0)
        xts = Ring([(sb("n1x%d" % i, [128, D], F32), Buf()) for i in range(3)])
        hbs = Ring([(sb("n1h%d" % i, [128, NCH, 512], BF16), Buf()) for i in range(2)])
        for b in range(NBM):
            hb, bhb = hbs.next()
            for t in range(4):
                xt, bxt = xts.next()
                r0 = b * 512 + t * 128
                S.dma("sp", lambda e, xt=xt, r0=r0: e.dma_start(out=xt[:], in_=xseg[r0:r0 + 128, :]), writes=[bxt])
                nt.run(xt[:], bxt, hb[:, :, t * 128:(t + 1) * 128], bhb)
            S.dma("sp", lambda e, hb=hb, b=b: e.dma_start(out=hT1[b], in_=hb[:].rearrange("p c n -> p (c n)")),
                  reads=[bhb], writes=[b_hT1[b]])
        S.barrier()
        S.emit_all()

    def interleave(gens):
        gens = list(gens)
        while gens:
            for g in list(gens):
                try:
                    next(g)
                except StopIteration:
                    gens.remove(g)

    for p in range(4):
        S.mute = limit < 2 + p
        with nc.reset_on_exit():
            win = sb("win", [128, NCH, 5, 256], BF16)
            wout = sb("wout", [128, 4, D], BF16)
            pw = sb("pw", [128, 2, 256], BF16)
            bwin, bwout, bpw = [Buf() for _ in range(5)], [Buf() for _ in range(4)], Buf()
            for g5 in (0, 3, 1, 2, 4):
                c0 = g5 * 1024 + 256 * p
                S.dma("pool", lambda e, g5=g5, c0=c0: e.dma_start(out=win[:, :, g5, :], in_=wslice(w_in, 0, D, c0, c0 + 256)), writes=[bwin[g5]])
            S.dma("pool", lambda e: e.dma_start(out=pw[:], in_=pool_w[p].rearrange("(c q) n -> q c n", q=128)), writes=[bpw])
            for n in range(4):
                S.dma("pool", lambda e, n=n: e.dma_start(out=wout[:, 0:2, n * 512:(n + 1) * 512], in_=wslice(w_out, 256 * p, 256 * p + 256, n * 512, (n + 1) * 512)), writes=[bwout[n]])
                S.dma("pool", lambda e, n=n: e.dma_start(out=wout[:, 2:4, n * 512:(n + 1) * 512], in_=wslice(w_out, 1024 + 256 * p, 1024 + 256 * p + 256, n * 512, (n + 1) * 512)), writes=[bwout[n]])
            small = sb("msmall", [128, 64], F32)
            bsmall = Buf()
            S.dma("sp", lambda e: e.dma_start(out=small[:, 0:8], in_=pscale), writes=[bsmall])
            S.dma("sp", lambda e: e.dma_start(out=small[:, 8:24], in_=lbl), writes=[bsmall])
            S.dma("sp", lambda e: e.dma_start(out=small[:, 24:25], in_=hnorm), writes=[bsmall])
            S.op("dve", lambda e: e.tensor_tensor(out=small[:, 32:34], in0=small[:, 8 + 2 * p:10 + 2 * p], in1=small[:, 16 + 2 * p:18 + 2 * p], op=ALU.subtract),
                 reads=[bsmall], writes=[bsmall])
            S.op("act", lambda e: e.activation(out=small[:, 25:27], in_=small[:, 32:34], func=AF.Sigmoid), reads=[bsmall], writes=[bsmall])
            S.op("dve", lambda e: e.tensor_scalar(out=small[:, 27:29], in0=small[:, 25:27], scalar1=-1.0, scalar2=1.0, op0=ALU.mult, op1=ALU.add),
                 reads=[bsmall], writes=[bsmall])
            S.op("dve", lambda e: e.tensor_scalar(out=small[:, 29:31], in0=small[:, 27:29], scalar1=-1.0, scalar2=None, op0=ALU.mult),
                 reads=[bsmall], writes=[bsmall])
            rm_sb = sb("rm_sb", [128, 512], F32)
            bd_sb = sb("bd_sb", [128, 128], U32)
            ic_sb = sb("ic_sb", [128, 512], F32)
            bcm_ = Buf()
            S.dma("sp", lambda e: e.dma_start(out=rm_sb[:], in_=rmask), writes=[bcm_])
            S.dma("sp", lambda e: e.dma_start(out=bd_sb[:], in_=bdmask), writes=[bcm_])
            S.dma("sp", lambda e: e.dma_start(out=ic_sb[:], in_=invcnt[:, p * 512:(p + 1) * 512]), writes=[bcm_])

            hTr = Ring([(sb("mh%d" % i, [128, NCH, 512], BF16), Buf()) for i in range(2)])
            uT = sb("uT", [128, 2, 528], F32)
            b_uh, b_um = Buf(), Buf()
            s_a = sb("s_a", [128, 2, 528], F32)
            s_b = sb("s_b", [128, 2, 528], F32)
            bsa, bsb = Buf(), Buf()
            pooled = sb("pooled", [128, 2, 512], BF16)
            bpooled = Buf()
            tmpH = []
            for hh in range(2):
                d_ = {}
                for nm in ["q", "sig", "lf", "kk", "a", "at"]:
                    d_[nm] = sb("h%d%s" % (hh, nm), [128, 512], F32)
                    d_["b" + nm] = Buf()
                d_["e1"], d_["be1"] = d_["lf"], d_["blf"]
                d_["e2"], d_["be2"] = d_["sig"], d_["bsig"]
                tmpH.append(d_)
            sets = []
            for si in range(2):
                st_ = {"hd": []}
                for hh in range(2):
                    d_ = {}
                    for nm in ["qt", "ktT"]:
                        d_[nm] = sb("s%dh%d%s" % (si, hh, nm), [128, 512], BF16)
                        d_["b" + nm] = Buf()
                    d_["ktok"] = sb("s%dh%dktok" % (si, hh), [128, 4, 128], BF16)
                    d_["bktok"] = Buf()
                    d_["ex"] = sb("s%dh%dex" % (si, hh), [128, 32], F32)
                    d_["bex"] = Buf()
                    st_["hd"].append(d_)
                st_["gT"] = sb("s%dgT" % si, [128, 2, 512], F32)
                st_["bgT"] = [Buf(), Buf()]
                st_["v"] = sb("s%dv" % si, [128, 4, 256], BF16)
                st_["bv"] = Buf()
                st_["mixT"] = sb("s%dmixT" % si, [128, 4, 512], BF16)
                st_["bmix"] = [Buf() for _ in range(4)]
                sets.append(st_)
            rec = []
            for hh in range(2):
                d_ = {}
                d_["S"] = sb("h%dS" % hh, [128, 128], F32)
                d_["bS"] = Buf()
                d_["sref"] = Ring([(sb("h%dsr%d" % (hh, i), [128, 128], BF16), Buf()) for i in range(3)])
                d_["tst8"] = [(sb("h%dts%d" % (hh, i), [128, 128], F32), Buf()) for i in range(8)]
                d_["ssb4"] = [(sb("h%dss%d" % (hh, i), [128, 128], BF16), Buf()) for i in range(4)]
                for (tt, bb) in d_["ssb4"]:
                    S.op("pool", lambda e, tt=tt: e.memset(tt[:], 0.0), writes=[bb])
                S.op("pool", lambda e, d_=d_: e.memset(d_["S"][:], 0.0), writes=[d_["bS"]])
                rec.append(d_)
            o_sb = sb("o_sb", [128, 512], F32)
            sq_sb = sb("sq_sb", [128, 512], F32)
            rs_sb = sb("rs_sb", [128, 512], F32)
            bo, bsq, brs = Buf(), Buf(), Buf()
            accr = Ring([(sb("acc%d" % i, [128, D], F32), Buf()) for i in range(2)])
            ringA = psring([0, 1, 2])
            PS_S, PS_O, PS_ST, PS_KT, PS_SS = 3, 4, 6, 7, 7

            def front(b):
                halo = b < HB
                st_ = sets[b % 2]
                mixT, bmix = st_["mixT"], st_["bmix"]
                v_sb, bv = st_["v"], st_["bv"]
                gT, bgT = st_["gT"], st_["bgT"]
                hT, bhT = hTr.next()
                S.dma("sp", lambda e, hT=hT, b=b: e.dma_start(out=hT[:].rearrange("p c n -> p (c n)"), in_=hT1[b]),
                      reads=[b_hT1[b]], writes=[bhT])
                if b == 0:
                    S.op("dve", lambda e: e.memset(uT[:, :, 0:16], 0.0), writes=[b_uh])
                else:
                    S.op("dve", lambda e: e.tensor_copy(out=uT[:, :, 0:16], in_=uT[:, :, 512:528]), reads=[b_um], writes=[b_uh])
                for j in range(2):
                    ps, bps = ringA.next()
                    mm_group(ps[:], bps, [(win[:, c, 0, j * 128:(j + 1) * 128], hT[:, c, :]) for c in range(NCH)], [bwin[0], bhT])
                    S.op("act", lambda e, ps=ps, j=j: e.activation(out=uT[:, j, 16:528], in_=ps[:], func=AF.Copy), reads=[bps], writes=[b_um])
                    yield
                for t in range(4):
                    ps, bps = ringA.next()
                    mm_group(ps[:, 0:256], bps, [(hT[:, c, t * 128:(t + 1) * 128], win[:, c, 3, :]) for c in range(NCH)], [bwin[3], bhT])
                    S.op("dve", lambda e, ps=ps, t=t: e.tensor_copy(out=v_sb[:, t, :], in_=ps[:, 0:256]), reads=[bps], writes=[bv])
                    if t % 2 == 1:
                        yield
                if not halo:
                    k = p + 1
                    src, bsrc = uT, None
                    for step in range(1, k + 1):
                        sh = 1 << (step - 1)
                        lo = 1 << step
                        dst, bdst = (s_a, bsa) if step % 2 == 1 else (s_b, bsb)
                        if step == 1:
                            S.op("pool", lambda e, dst=dst, lo=lo, sh=sh: e.tensor_tensor(out=dst[:, :, lo:528], in0=uT[:, :, lo:528], in1=uT[:, :, lo - sh:528 - sh], op=ALU.add),
                                 reads=[b_uh, b_um], writes=[bdst])
                        else:
                            S.op("pool", lambda e, dst=dst, src=src, lo=lo, sh=sh: e.tensor_tensor(out=dst[:, :, lo:528], in0=src[:, :, lo:528], in1=src[:, :, lo - sh:528 - sh], op=ALU.add),
                                 reads=[bsrc], writes=[bdst])
                        src, bsrc = dst, bdst
                    wnd = float(1 << k)
                    if b == HB:
                        other, bother = (s_b, bsb) if src is s_a else (s_a, bsa)
                        for j in range(2):
                            S.op("dve", lambda e, j=j, src=src, other=other: e.tensor_tensor(out=other[:, j, 16:528], in0=src[:, j, 16:528], in1=ic_sb[:], op=ALU.mult),
                                 reads=[bsrc, bcm_], writes=[bother])
                        S.op("dve", lambda e, other=other: e.tensor_tensor(out=pooled[:], in0=other[:, :, 16:528], in1=uT[:, :, 16:528], op=ALU.subtract),
                             reads=[bother, b_um], writes=[bpooled])
                    else:
                        S.op("dve", lambda e, src=src, wnd=wnd: e.scalar_tensor_tensor(out=pooled[:], in0=src[:, :, 16:528], scalar=1.0 / wnd, in1=uT[:, :, 16:528],
                                                                                   op0=ALU.mult, op1=ALU.subtract),
                             reads=[bsrc, b_um], writes=[bpooled])
                    yield
                for hh in range(2):
                    h_ = tmpH[hh]
                    o_ = st_["hd"][hh]
                    lb_ap = small[:, 25 + hh:26 + hh]
                    oml_ap = small[:, 27 + hh:28 + hh]
                    noml_ap = small[:, 29 + hh:30 + hh]
                    ps, bps = ringA.next()
                    mm_group(ps[:], bps, [(win[:, c, 2, hh * 128:(hh + 1) * 128], hT[:, c, :]) for c in range(NCH)], [bwin[2], bhT])
                    S.op("act", lambda e, ps=ps, h_=h_: e.activation(out=h_["sig"][:], in_=ps[:], func=AF.Sigmoid), reads=[bps], writes=[h_["bsig"]])
                    yield
                    ps, bps = ringA.next()
                    mm_group(ps[:], bps, [(win[:, c, 1, hh * 128:(hh + 1) * 128], hT[:, c, :]) for c in range(NCH)], [bwin[1], bhT])
                    S.op("act", lambda e, ps=ps, h_=h_: e.activation(out=h_["q"][:], in_=ps[:], func=AF.Silu), reads=[bps], writes=[h_["bq"]])
                    S.op("act", lambda e, h_=h_, oml_ap=oml_ap, lb_ap=lb_ap: e.activation(out=h_["lf"][:], in_=h_["sig"][:], func=AF.Ln, scale=oml_ap, bias=lb_ap),
                         reads=[h_["bsig"], bsmall], writes=[h_["blf"]])
                    S.op("dve", lambda e, h_=h_: e.tensor_tensor_scan(out=h_["a"][:], data0=rm_sb[:], data1=h_["lf"][:], initial=0.0, op0=ALU.mult, op1=ALU.add),
                         reads=[h_["blf"], bcm_], writes=[h_["ba"]])
                    yield
                    ps, bps = ringA.next()
                    mm_group(ps[:], bps, [(win[:, c, 4, hh * 128:(hh + 1) * 128], hT[:, c, :]) for c in range(NCH)], [bwin[4], bhT])
                    S.op("act", lambda e, ps=ps, hh=hh, gT=gT: e.activation(out=gT[:, hh, :], in_=ps[:], func=AF.Silu), reads=[bps], writes=[bgT[hh]])
                    a3 = h_["a"][:].rearrange("p (c n) -> p c n", c=8)
                    S.op("dve", lambda e, h_=h_, a3=a3: e.tensor_tensor(out=h_["at"][:].rearrange("p (c n) -> p c n", c=8), in0=a3, in1=a3[:, :, 31:32].to_broadcast([128, 8, 64]), op=ALU.subtract),
                         reads=[h_["ba"]], writes=[h_["bat"]])
                    S.op("pool", lambda e, h_=h_, noml_ap=noml_ap, oml_ap=oml_ap: e.tensor_scalar(out=h_["kk"][:], in0=h_["sig"][:], scalar1=noml_ap, scalar2=oml_ap, op0=ALU.mult, op1=ALU.add),
                         reads=[h_["bsig"], bsmall], writes=[h_["bkk"]])
                    yield
                    S.op("act", lambda e, h_=h_: e.activation(out=h_["e1"][:], in_=h_["at"][:], func=AF.Exp), reads=[h_["bat"]], writes=[h_["be1"]])
                    S.op("act", lambda e, h_=h_: e.activation(out=h_["e2"][:], in_=h_["at"][:], func=AF.Exp, scale=-1.0), reads=[h_["bat"]], writes=[h_["be2"]])
                    a16 = h_["a"][:].rearrange("p (c n) -> p c n", n=32)[:, :, 31:32]
                    S.op("act", lambda e, o_=o_, a16=a16: e.activation(out=o_["ex"][:, 0:16].rearrange("p (c n) -> p c n", n=1), in_=a16, func=AF.Exp),
                         reads=[h_["ba"]], writes=[o_["bex"]])
                    S.op("dve", lambda e, o_=o_, a3=a3: e.tensor_tensor(out=o_["ex"][:, 16:24].rearrange("p (c n) -> p c n", n=1), in0=a3[:, :, 63:64], in1=a3[:, :, 31:32], op=ALU.subtract),
                         reads=[h_["ba"]], writes=[o_["bex"]])
                    S.op("act", lambda e, o_=o_: e.activation(out=o_["ex"][:, 24:32], in_=o_["ex"][:, 16:24], func=AF.Exp), reads=[o_["bex"]], writes=[o_["bex"]])
                    yield
                    S.op("dve", lambda e, h_=h_, o_=o_: e.tensor_tensor(out=o_["qt"][:], in0=h_["q"][:], in1=h_["e1"][:], op=ALU.mult),
                         reads=[h_["bq"], h_["be1"]], writes=[o_["bqt"]])
                    S.op("pool", lambda e, h_=h_, o_=o_: e.tensor_tensor(out=o_["ktT"][:], in0=h_["kk"][:], in1=h_["e2"][:], op=ALU.mult),
                         reads=[h_["bkk"], h_["be2"]], writes=[o_["bktT"]])
                    yield
                    psb = PS[PS_KT][:].bitcast(BF16)
                    for t in range(4):
                        S.op("pe", lambda e, o_=o_, t=t, psb=psb: e.transpose(psb[:, t * 128:(t + 1) * 128], o_["ktT"][:, t * 128:(t + 1) * 128], id_b[:]),
                             reads=[o_["bktT"], bconst], writes=[BPS[PS_KT]])
                    S.op("act", lambda e, o_=o_, psb=psb: e.activation(out=o_["ktok"][:].rearrange("p t n -> p (t n)"), in_=psb[:, 0:512], func=AF.Copy),
                         reads=[BPS[PS_KT]], writes=[o_["bktok"]])
                    yield
                if not halo:
                    for oc in range(2):
                        ps, bps = ringA.next()
                        mm_group(ps[:], bps, [(pw[:, ic, oc * 128:(oc + 1) * 128], pooled[:, ic, :]) for ic in range(2)], [bpw, bpooled])
                        S.op("act", lambda e, ps=ps, oc=oc, mixT=mixT: e.activation(out=mixT[:, oc, :], in_=ps[:], func=AF.Copy, scale=small[:, 2 * p + oc:2 * p + oc + 1]),
                             reads=[bps, bsmall], writes=[bmix[oc]])
                    yield

            def back(b):
                halo = b < HB
                st_ = sets[b % 2]
                mixT, bmix = st_["mixT"], st_["bmix"]
                v_sb, bv = st_["v"], st_["bv"]
                gT, bgT = st_["gT"], st_["bgT"]
                for t in range(4):
                    for hh in range(2):
                        h_ = st_["hd"][hh]
                        r_ = rec[hh]
                        S.op("pe", lambda e, h_=h_, t=t: e.matmul(PS[PS_S][:, 0:128], lhsT=h_["ktT"][:, t * 128:(t + 1) * 128], rhs=h_["qt"][:, t * 128:(t + 1) * 128], start=True, stop=True),
                             reads=[h_["bktT"], h_["bqt"]], writes=[BPS[PS_S]])
                        ssb, bssb = r_["ssb4"][t]
                        S.op("dve", lambda e, ssb=ssb: e.copy_predicated(out=ssb[:], mask=bd_sb[:], data=PS[PS_S][:, 0:128]),
                             reads=[BPS[PS_S], bcm_], writes=[bssb])
                        for cc in range(2):
                            c = 2 * t + cc
                            S.op("pe", lambda e, h_=h_, t=t, cc=cc, hh=hh, v_sb=v_sb: e.matmul(PS[PS_ST][:, 0:128], lhsT=h_["ktok"][cc * 64:(cc + 1) * 64, t, :], rhs=v_sb[cc * 64:(cc + 1) * 64, t, hh * 128:(hh + 1) * 128], start=True, stop=True),
                                 reads=[h_["bktok"], bv], writes=[BPS[PS_ST]])
                            tst, btst = r_["tst8"][c]
                            S.op("act", lambda e, h_=h_, tst=tst, c=c: e.activation(out=tst[:], in_=PS[PS_ST][:, 0:128], func=AF.Copy, scale=h_["ex"][:, 24 + c:25 + c]),
                                 reads=[BPS[PS_ST], h_["bex"]], writes=[btst])
                        yield
                for t in range(4):
                    for hh in range(2):
                        h_ = st_["hd"][hh]
                        r_ = rec[hh]
                        pso, bpso = PS[PS_O + hh], BPS[PS_O + hh]
                        ssb, bssb = r_["ssb4"][t]
                        S.op("pe", lambda e, ssb=ssb, t=t, hh=hh, pso=pso, v_sb=v_sb: e.matmul(pso[:, t * 128:(t + 1) * 128], lhsT=v_sb[:, t, hh * 128:(hh + 1) * 128], rhs=ssb[:], start=True, stop=False),
                             reads=[bssb, bv], writes=[bpso])
                        for cc in range(2):
                            c = 2 * t + cc
                            sref, bsref = r_["sref"].next()
                            S.op("pool", lambda e, h_=h_, r_=r_, sref=sref, c=c: e.tensor_scalar(out=sref[:], in0=r_["S"][:], scalar1=h_["ex"][:, 2 * c:2 * c + 1], scalar2=0.0, op0=ALU.mult, op1=ALU.add),
                                 reads=[r_["bS"], h_["bex"]], writes=[bsref])
                            tst, btst = r_["tst8"][c]
                            S.op("pool", lambda e, h_=h_, r_=r_, c=c: e.tensor_scalar(out=r_["S"][:], in0=r_["S"][:], scalar1=h_["ex"][:, 2 * c + 1:2 * c + 2], scalar2=0.0, op0=ALU.mult, op1=ALU.add),
                                 reads=[r_["bS"], h_["bex"]], writes=[r_["bS"]])
                            S.op("pool", lambda e, r_=r_, tst=tst: e.tensor_tensor(out=r_["S"][:], in0=r_["S"][:], in1=tst[:], op=ALU.add),
                                 reads=[r_["bS"], btst], writes=[r_["bS"]])
                            c0 = t * 128 + cc * 64
                            S.op("pe", lambda e, h_=h_, sref=sref, c0=c0, pso=pso, cc=cc: e.matmul(pso[:, c0:c0 + 64], lhsT=sref[:], rhs=h_["qt"][:, c0:c0 + 64], start=False, stop=(cc == 1)),
                                 reads=[bsref, h_["bqt"]], writes=[bpso])
                        yield
                if halo:
                    return
                for hh in range(2):
                    pso, bpso = PS[PS_O + hh], BPS[PS_O + hh]
                    S.op("act", lambda e, pso=pso: e.activation(out=o_sb[:], in_=pso[:], func=AF.Copy), reads=[bpso], writes=[bo])
                    S.op("act", lambda e, pso=pso: e.activation(out=sq_sb[:], in_=pso[:], func=AF.Square), reads=[bpso], writes=[bsq])
                    yield
                    S.op("pe", lambda e: e.matmul(PS[PS_SS][:], lhsT=ones_f[:], rhs=sq_sb[:], start=True, stop=True), reads=[bsq, bconst], writes=[BPS[PS_SS]])
                    S.op("act", lambda e: e.activation(out=rs_sb[:], in_=PS[PS_SS][:], func=AF.Ln, scale=1.0 / 128, bias=eps_t[:]), reads=[BPS[PS_SS], bconst], writes=[brs])
                    S.op("act", lambda e: e.activation(out=rs_sb[:], in_=rs_sb[:], func=AF.Exp, scale=-0.5), reads=[brs], writes=[brs])
                    S.op("dve", lambda e: e.scalar_tensor_tensor(out=o_sb[:], in0=o_sb[:], scalar=small[:, 24:25], in1=rs_sb[:], op0=ALU.mult, op1=ALU.mult),
                         reads=[bo, brs, bsmall], writes=[bo])
                    S.op("dve", lambda e, hh=hh, mixT=mixT, gT=gT: e.tensor_tensor(out=mixT[:, 2 + hh, :], in0=o_sb[:], in1=gT[:, hh, :], op=ALU.mult),
                         reads=[bo, bgT[hh]], writes=[bmix[2 + hh]])
                    yield
                for t in range(4):
                    acc, bacc = accr.next()
                    ti = (b - HB) * 4 + t
                    if p == 0:
                        r0 = b * 512 + t * 128
                        S.dma("sp", lambda e, acc=acc, r0=r0: e.dma_start(out=acc[:], in_=xseg[r0:r0 + 128, :]), writes=[bacc])
                    else:
                        S.dma("sp", lambda e, acc=acc, ti=ti: e.dma_start(out=acc[:], in_=x1s[ti * 128:(ti + 1) * 128, :]), reads=[b_x1[ti]], writes=[bacc])
                    for n in range(4):
                        ps, bps = ringA.next()
                        mm_group(ps[:], bps, [(mixT[:, c, t * 128:(t + 1) * 128], wout[:, c, n * 512:(n + 1) * 512]) for c in range(4)], [bwout[n]] + bmix)
                        S.op("dve", lambda e, acc=acc, ps=ps, n=n: e.tensor_tensor(out=acc[:, n * 512:(n + 1) * 512], in0=ps[:], in1=acc[:, n * 512:(n + 1) * 512], op=ALU.add),
                             reads=[bps, bacc], writes=[bacc])
                        if n % 2 == 1 and not os.environ.get("NOYIELD_OP"):
                            yield
                    S.dma("sp", lambda e, acc=acc, ti=ti: e.dma_start(out=x1s[ti * 128:(ti + 1) * 128, :], in_=acc[:]), reads=[bacc], writes=[b_x1[ti]])
                    if dbg and p == 3:
                        bo_ = Buf()
                        S.dma("sp", lambda e, acc=acc, ti=ti: e.dma_start(out=x1d[ti * 128:(ti + 1) * 128, :], in_=acc[:]), reads=[bacc], writes=[bo_])
                        outs.append(bo_)

            interleave([front(0)])
            for b in range(1, NBM):
                interleave([back(b - 1), front(b)])
            interleave([back(NBM - 1)])
            S.barrier()
            S.emit_all()

    S.mute = limit < 6
    with nc.reset_on_exit():
        KT = sb("KT", [128, NCH, 256], BF16)
        V = sb("V", [128, 2, D], BF16)
        bKT, bV = Buf(), Buf()
        nt = NormT("x1n", [0, 1, 2, 3])
        ringB = psring([4, 5, 6, 7])
        with nc.reset_on_exit():
            nt.load_gain(4)
            mT = sb("mT", [128, NCH, 256], BF16)
            bmT = Buf()
            mts = Ring([(sb("memt%d" % i, [128, D], F32), Buf()) for i in range(2)])
            for mt in range(2):
                xt, bxt = mts.next()
                S.dma("sp", lambda e, xt=xt, mt=mt: e.dma_start(out=xt[:], in_=mem[mt * 128:(mt + 1) * 128, :]), writes=[bxt])
                nt.run(xt[:], bxt, mT[:, :, mt * 128:(mt + 1) * 128], bmT)
            wkr = Ring([(sb("wkv%d" % i, [128, NCH, 512], BF16), Buf()) for i in range(2)])
            for n in range(4):
                wt, bwt = wkr.next()
                S.dma("pool", lambda e, wt=wt, n=n: e.dma_start(out=wt[:], in_=wslice(wk, 0, D, n * 512, (n + 1) * 512)), writes=[bwt])
                for jj in range(4):
                    ps, bps = ringB.next()
                    mm_group(ps[:, 0:256], bps, [(wt[:, c, jj * 128:(jj + 1) * 128], mT[:, c, :]) for c in range(NCH)], [bwt, bmT])
                    evac_copy(KT[:, n * 4 + jj, :], ps[:, 0:256], [bps], [bKT])
            for n in range(4):
                wt, bwt = wkr.next()
                S.dma("pool", lambda e, wt=wt, n=n: e.dma_start(out=wt[:], in_=wslice(wv, 0, D, n * 512, (n + 1) * 512)), writes=[bwt])
                for mt in range(2):
                    ps, bps = ringB.next()
                    mm_group(ps[:], bps, [(mT[:, c, mt * 128:(mt + 1) * 128], wt[:, c, :]) for c in range(NCH)], [bwt, bmT])
                    evac_copy(V[:, mt, n * 512:(n + 1) * 512], ps[:], [bps], [bV])
            S.barrier()
            S.emit_all()
        nt.load_gain(1)
        wq_sb = sb("wq_sb", [128, NCH, D], BF16)
        bwq4 = [Buf() for _ in range(4)]
        for n in range(4):
            S.dma("pool", lambda e, n=n: e.dma_start(out=wq_sb[:, :, n * 512:(n + 1) * 512], in_=wslice(wq, 0, D, n * 512, (n + 1) * 512)), writes=[bwq4[n]])
        xts = Ring([(sb("x1x%d" % i, [128, D], F32), Buf()) for i in range(2)])
        h2T = sb("h2T", [128, NCH, 512], BF16)
        bh2 = Buf()
        qT = sb("qT", [128, NCH, 512], BF16)
        bqT = Buf()
        aTr = Ring([(sb("aT%d" % i, [128, NCH, 512], BF16), Buf()) for i in range(2)])
        attr = Ring([(sb("pex%d" % i, [128, 256], F32), sb("pn%d" % i, [128, 256], BF16), sb("PT%d" % i, [128, 2, 128], BF16), sb("sst%d" % i, [128, 8], F32),
                      Buf(), Buf(), Buf(), Buf()) for i in range(3)])
        SC = 512 ** -0.5
        for b in range(NB):
            for t in range(4):
                xt, bxt = xts.next()
                ti = b * 4 + t
                S.dma("sp", lambda e, xt=xt, ti=ti: e.dma_start(out=xt[:], in_=x1s[ti * 128:(ti + 1) * 128, :]), reads=[b_x1[ti]], writes=[bxt])
                nt.run(xt[:], bxt, h2T[:, :, t * 128:(t + 1) * 128], bh2)
            for j in range(NCH):
                ps, bps = ringB.next()
                mm_group(ps[:], bps, [(wq_sb[:, c, j * 128:(j + 1) * 128], h2T[:, c, :]) for c in range(NCH)], [bwq4[j // 4], bh2])
                evac_copy(qT[:, j, :], ps[:], [bps], [bqT])
            aT, baT = aTr.next()
            for t in range(4):
                for h in range(4):
                    pex, pn, PT, sst, bpex, bpn, bPT, bsst = attr.next()
                    ps, bps = ringB.next()
                    mm_group(ps[:, 0:256], bps, [(qT[:, 4 * h + c4, t * 128:(t + 1) * 128], KT[:, 4 * h + c4, :]) for c4 in range(4)], [bqT, bKT])
                    S.op("dve", lambda e, ps=ps, sst=sst: e.tensor_reduce(out=sst[:, 0:1], in_=ps[:, 0:256], axis=AX.X, op=ALU.max), reads=[bps], writes=[bsst])
                    S.op("dve", lambda e, sst=sst: e.tensor_scalar(out=sst[:, 1:2], in0=sst[:, 0:1], scalar1=-SC, scalar2=None, op0=ALU.mult), reads=[bsst], writes=[bsst])
                    S.op("act", lambda e, ps=ps, sst=sst, pex=pex: e.activation(out=pex[:], in_=ps[:, 0:256], func=AF.Exp, scale=SC, bias=sst[:, 1:2], accum_out=sst[:, 2:3]),
                         reads=[bps, bsst], writes=[bpex, bsst])
                    S.op("dve", lambda e, sst=sst: e.reciprocal(out=sst[:, 3:4], in_=sst[:, 2:3]), reads=[bsst], writes=[bsst])
                    S.op("dve", lambda e, sst=sst, pex=pex, pn=pn: e.tensor_scalar(out=pn[:], in0=pex[:], scalar1=sst[:, 3:4], scalar2=None, op0=ALU.mult), reads=[bpex, bsst], writes=[bpn])
                    ps2, bps2 = ringB.next()
                    psb = ps2[:].bitcast(BF16)
                    for mc in range(2):
                        S.op("pe", lambda e, psb=psb, mc=mc, pn=pn: e.transpose(psb[:, mc * 128:(mc + 1) * 128], pn[:, mc * 128:(mc + 1) * 128], id_b[:]),
                             reads=[bpn, bconst], writes=[bps2])
                    S.op("act", lambda e, psb=psb, PT=PT: e.activation(out=PT[:].rearrange("p c n -> p (c n)"), in_=psb[:, 0:256], func=AF.Copy), reads=[bps2], writes=[bPT])
                    ps3, bps3 = ringB.next()
                    for j in range(4):
                        mm_group(ps3[:, j * 128:(j + 1) * 128], bps3,
                                 [(V[:, mc, h * 512 + j * 128:h * 512 + (j + 1) * 128], PT[:, mc, :]) for mc in range(2)], [bV, bPT])
                    evac_copy(aT[:, 4 * h:4 * h + 4, t * 128:(t + 1) * 128], ps3[:].rearrange("p (c n) -> p c n", c=4), [bps3], [baT])
            S.dma("sp", lambda e, aT=aT, b=b: e.dma_start(out=aTs[b], in_=aT[:].rearrange("p c n -> p (c n)")), reads=[baT], writes=[b_aT[b]])
        S.barrier()
        S.emit_all()

    S.mute = limit < 6.5
    with nc.reset_on_exit():
        nt = NormT("x2n", [0, 1, 2, 3])
        nt.load_gain(2)
        ringB = psring([4, 5, 6])
        wo_sb = sb("wo_sb", [128, NCH, D], BF16)
        bwo = Buf()
        for n in range(4):
            S.dma("pool", lambda e, n=n: e.dma_start(out=wo_sb[:, :, n * 512:(n + 1) * 512], in_=wslice(wo, 0, D, n * 512, (n + 1) * 512)), writes=[bwo])
        wr_sb = sb("wr_sb", [128, NCH, 20], F32)
        bwr = Buf()
        S.dma("sp", lambda e: e.dma_start(out=wr_sb[:].rearrange("p c n -> p (c n)"), in_=wr), writes=[bwr])
        aTr = Ring([(sb("aTi%d" % i, [128, NCH, 512], BF16), Buf()) for i in range(2)])
        xts = Ring([(sb("x2x%d" % i, [128, D], F32), Buf()) for i in range(3)])
        h3r = Ring([(sb("h3b%d" % i, [128, NCH, 512], BF16), Buf()) for i in range(2)])
        h3fr = Ring([(sb("h3f%d" % i, [128, NCH, 128], F32), Buf()) for i in range(2)])
        rtr = Ring([(sb("rt%d" % i, [128, 128], F32), Buf()) for i in range(2)])
        cmr = Ring([(sb("cmb%d" % i, [128, 16], F32), Buf()) for i in range(2)])
        def x2_normrouter(b, t, ti, xt, bxt, h3b, bh3b):
            h3f, bh3f = h3fr.next()
            rt, brt = rtr.next()
            nt.run(xt[:], bxt, h3b[:, :, t * 128:(t + 1) * 128], bh3b, h3f, bh3f)
            if limit == 6.5:
                S.mute = True
            psr, bpsr = PS[7], BPS[7]
            mm_group(psr[:, 0:20], bpsr, [(h3f[:, c, :], wr_sb[:, c, :]) for c in range(NCH)], [bh3f, bwr])
            cm, bcm = cmr.next()
            R_ = rt
            S.op("act", lambda e, R_=R_: e.activation(out=R_[:, 0:20], in_=psr[:, 0:20], func=AF.Copy), reads=[bpsr], writes=[brt])
            S.op("dve", lambda e, R_=R_: e.tensor_reduce(out=R_[:, 20:21], in_=R_[:, 0:4], axis=AX.X, op=ALU.max), reads=[brt], writes=[brt])
            S.op("dve", lambda e, R_=R_: e.tensor_scalar(out=R_[:, 21:22], in0=R_[:, 20:21], scalar1=-1.0, scalar2=None, op0=ALU.mult), reads=[brt], writes=[brt])
            S.op("act", lambda e, R_=R_: e.activation(out=R_[:, 24:28], in_=R_[:, 0:4], func=AF.Exp, bias=R_[:, 21:22], accum_out=R_[:, 22:23]), reads=[brt], writes=[brt])
            S.op("dve", lambda e, R_=R_: e.reciprocal(out=R_[:, 23:24], in_=R_[:, 22:23]), reads=[brt], writes=[brt])
            S.op("dve", lambda e, R_=R_: e.tensor_scalar(out=R_[:, 24:28], in0=R_[:, 0:4], scalar1=R_[:, 20:21], scalar2=None, op0=ALU.is_equal), reads=[brt], writes=[brt])
            S.op("dve", lambda e, R_=R_: e.tensor_scalar(out=R_[:, 24:28], in0=R_[:, 24:28], scalar1=-1.0, scalar2=1e30, op0=ALU.add, op1=ALU.mult), reads=[brt], writes=[brt])
            S.op("dve", lambda e, R_=R_: e.tensor_tensor(out=R_[:, 32:48].rearrange("p (g k) -> p g k", g=4), in0=R_[:, 4:20].rearrange("p (g k) -> p g k", g=4),
                                                  in1=R_[:, 24:28].rearrange("p (g k) -> p g k", k=1).to_broadcast([128, 4, 4]), op=ALU.add), reads=[brt], writes=[brt])
            S.op("dve", lambda e, R_=R_: e.max(out=R_[:, 48:56], in_=R_[:, 32:48]), reads=[brt], writes=[brt])
            S.op("dve", lambda e, R_=R_: e.tensor_tensor(out=R_[:, 56:57], in0=R_[:, 49:50], in1=R_[:, 48:49], op=ALU.subtract), reads=[brt], writes=[brt])
            S.op("act", lambda e, R_=R_: e.activation(out=R_[:, 57:58], in_=R_[:, 56:57], func=AF.Exp), reads=[brt], writes=[brt])
            S.op("dve", lambda e, R_=R_: e.tensor_scalar(out=R_[:, 58:59], in0=R_[:, 57:58], scalar1=1.0, scalar2=None, op0=ALU.add), reads=[brt], writes=[brt])
            S.op("dve", lambda e, R_=R_: e.reciprocal(out=R_[:, 59:60], in_=R_[:, 58:59]), reads=[brt], writes=[brt])
            S.op("dve", lambda e, R_=R_: e.tensor_tensor(out=R_[:, 60:61], in0=R_[:, 57:58], in1=R_[:, 59:60], op=ALU.mult), reads=[brt], writes=[brt])
            S.op("dve", lambda e, R_=R_: e.tensor_scalar(out=R_[:, 61:63], in0=R_[:, 59:61], scalar1=R_[:, 23:24], scalar2=None, op0=ALU.mult), reads=[brt], writes=[brt])
            S.op("dve", lambda e, R_=R_: e.tensor_scalar(out=R_[:, 64:80], in0=R_[:, 32:48], scalar1=R_[:, 48:49], scalar2=R_[:, 61:62], op0=ALU.is_equal, op1=ALU.mult), reads=[brt], writes=[brt])
            S.op("dve", lambda e, R_=R_: e.tensor_scalar(out=R_[:, 80:96], in0=R_[:, 32:48], scalar1=R_[:, 49:50], scalar2=R_[:, 62:63], op0=ALU.is_equal, op1=ALU.mult), reads=[brt], writes=[brt])
            S.op("dve", lambda e, cm=cm, R_=R_: e.tensor_tensor(out=cm[:], in0=R_[:, 64:80], in1=R_[:, 80:96], op=ALU.add), reads=[brt], writes=[bcm])
            S.dma("sp", lambda e, cm=cm, ti=ti: e.dma_start(out=combs[ti * 128:(ti + 1) * 128, :], in_=cm[:]), reads=[bcm], writes=[b_comb[ti]])
            if limit == 6.5:
                S.mute = False
            if t == 3:
                S.dma("sp", lambda e, h3b=h3b, b=b: e.dma_start(out=h3Ts[b], in_=h3b[:].rearrange("p c n -> p (c n)")), reads=[bh3b], writes=[b_h3T[b]])

        pend = None
        for b in range(NB):
            aT, baT = aTr.next()
            S.dma("sp", lambda e, aT=aT, b=b: e.dma_start(out=aT[:].rearrange("p c n -> p (c n)"), in_=aTs[b]), reads=[b_aT[b]], writes=[baT])
            h3b, bh3b = h3r.next()
            for t in range(4):
                ti = b * 4 + t
                xt, bxt = xts.next()
                S.dma("sp", lambda e, xt=xt, ti=ti: e.dma_start(out=xt[:], in_=x1s[ti * 128:(ti + 1) * 128, :]), reads=[b_x1[ti]], writes=[bxt])
                for n in range(4):
                    ps, bps = ringB.next()
                    mm_group(ps[:], bps, [(aT[:, c, t * 128:(t + 1) * 128], wo_sb[:, c, n * 512:(n + 1) * 512]) for c in range(NCH)], [bwo, baT])
                    S.op("dve", lambda e, xt=xt, ps=ps, n=n: e.tensor_tensor(out=xt[:, n * 512:(n + 1) * 512], in0=ps[:], in1=xt[:, n * 512:(n + 1) * 512], op=ALU.add),
                         reads=[bps, bxt], writes=[bxt])
                S.dma("sp", lambda e, xt=xt, ti=ti: e.dma_start(out=x2s[ti * 128:(ti + 1) * 128, :], in_=xt[:]), reads=[bxt], writes=[b_x2[ti]])
                if dbg:
                    bo_ = Buf()
                    S.dma("sp", lambda e, xt=xt, ti=ti: e.dma_start(out=x2d[ti * 128:(ti + 1) * 128, :], in_=xt[:]), reads=[bxt], writes=[bo_])
                    outs.append(bo_)
                if pend is not None:
                    x2_normrouter(*pend)
                pend = (b, t, ti, xt, bxt, h3b, bh3b)
        x2_normrouter(*pend)
        S.barrier()
        S.emit_all()

    S.mute = limit < 8
    with nc.reset_on_exit():
        h3T = sb("e_h3T", [128, NCH, EB], BF16)
        bh3 = Buf()
        y = sb("e_y", [128, ETI, D], F32)
        by = [Buf() for _ in range(ETI)]
        wring = Ring([(sb("e_w%d" % i, [128, 8192], BF16), Buf()) for i in range(4)])
        cmb = sb("e_cmb", [128, ETI, 16], F32)
        bcmb = Buf()
        sgr = Ring([(sb("e_sg%d" % i, [128, 512], F32), Buf()) for i in range(2)])
        hidr = Ring([(sb("e_hid%d" % i, [128, 512], BF16), Buf()) for i in range(2)])
        hTr_ = Ring([(sb("e_hT%d" % i, [128, 4, 128], BF16), Buf()) for i in range(2)])
        xts = Ring([(sb("e_x%d" % i, [128, D], F32), Buf()) for i in range(2)])
        junk = sb("e_junk", [128, D], BF16)
        gB = sb("e_gB", [128, D], F32)
        fst = sb("e_fst", [128, 4], F32)
        bjunk, bgB, bfst = Buf(), Buf(), Buf()
        S.dma("sp", lambda e: e.dma_start(out=gB[:], in_=gains[3:4, :].to_broadcast([128, D])), writes=[bgB])
        gur = Ring([((PS[0], BPS[0]), (PS[1], BPS[1])), ((PS[2], BPS[2]), (PS[3], BPS[3]))])
        ringY = psring([5, 6, 7])
        PS_T = 4
        pending_fn = None
        for eb in range(T // EB):
            for half in range(EB // 512):
                bi = eb * (EB // 512) + half
                S.dma("sp", lambda e, half=half, bi=bi: e.dma_start(out=h3T[:, :, half * 512:(half + 1) * 512], in_=h3Ts[bi].rearrange("p (c n) -> p c n", c=NCH)),
                      reads=[b_h3T[bi]], writes=[bh3])
            for i in range(ETI):
                ti = eb * ETI + i
                S.dma("sp", lambda e, i=i, ti=ti: e.dma_start(out=cmb[:, i, :], in_=combs[ti * 128:(ti + 1) * 128, :]), reads=[b_comb[ti]], writes=[bcmb])
            for ex in range(16):
                wg, bwg = wring.next()
                S.dma("pool", lambda e, wg=wg, ex=ex: e.dma_start(out=wg[:].rearrange("p (c n) -> p c n", c=NCH), in_=w_gate[ex].rearrange("(c p) n -> p c n", p=128)), writes=[bwg])
                wu, bwu = wring.next()
                S.dma("pool", lambda e, wu=wu, ex=ex: e.dma_start(out=wu[:].rearrange("p (c n) -> p c n", c=NCH), in_=w_up[ex].rearrange("(c p) n -> p c n", p=128)), writes=[bwu])
                wd, bwd = wring.next()
                for n in range(4):
                    S.dma("pool", lambda e, wd=wd, ex=ex, n=n: e.dma_start(out=wd[:].rearrange("p (c n) -> p c n", c=4)[:, :, n * 512:(n + 1) * 512],
                                                                    in_=w_down[ex][:, n * 512:(n + 1) * 512].rearrange("(c p) n -> p c n", p=128)), writes=[bwd])
                wg3 = wg[:].rearrange("p (c n) -> p c n", c=NCH)
                wu3 = wu[:].rearrange("p (c n) -> p c n", c=NCH)
                wd3 = wd[:].rearrange("p (c n) -> p c n", c=4)

                def GUg(i):
                    (pg, bpg), (pu, bpu) = gur.next()
                    mm_group(pg[:], bpg, [(h3T[:, c, i * 128:(i + 1) * 128], wg3[:, c, :]) for c in range(NCH)], [bh3, bwg])
                    return (pg, bpg, pu, bpu)

                def GUu(i, gu):
                    pg, bpg, pu, bpu = gu
                    mm_group(pu[:], bpu, [(h3T[:, c, i * 128:(i + 1) * 128], wu3[:, c, :]) for c in range(NCH)], [bh3, bwu])

                def HID(i, gu, ex=ex):
                    pg, bpg, pu, bpu = gu
                    sg, bsg = sgr.next()
                    S.op("act", lambda e: e.activation(out=sg[:], in_=pg[:], func=AF.Silu), reads=[bpg], writes=[bsg])
                    hid, bhid = hidr.next()
                    S.op("dve", lambda e: e.scalar_tensor_tensor(out=hid[:], in0=sg[:], scalar=cmb[:, i, ex:ex + 1], in1=pu[:], op0=ALU.mult, op1=ALU.mult),
                         reads=[bsg, bpu, bcmb], writes=[bhid])
                    return hid, bhid

                def TR(i, hb):
                    hid, bhid = hb
                    psb = PS[PS_T][:].bitcast(BF16)
                    for c4 in range(4):
                        S.op("pe", lambda e, c4=c4: e.transpose(psb[:, c4 * 128:(c4 + 1) * 128], hid[:, c4 * 128:(c4 + 1) * 128], id_b[:]),
                             reads=[bhid, bconst], writes=[BPS[PS_T]])
                    hT_, bhT_ = hTr_.next()
                    S.op("act", lambda e: e.activation(out=hT_[:].rearrange("p c n -> p (c n)"), in_=psb[:, 0:512], func=AF.Copy), reads=[BPS[PS_T]], writes=[bhT_])
                    return hT_, bhT_

                def DOWN(i, hb, ex=ex):
                    hT_, bhT_ = hb
                    for n in range(4):
                        ps, bps = ringY.next()
                        mm_group(ps[:], bps, [(hT_[:, c4, :], wd3[:, c4, n * 512:(n + 1) * 512]) for c4 in range(4)], [bhT_, bwd])
                        if ex == 0:
                            S.op("dve", lambda e, ps=ps, n=n: e.tensor_copy(out=y[:, i, n * 512:(n + 1) * 512], in_=ps[:]), reads=[bps], writes=[by[i]])
                        else:
                            S.op("dve", lambda e, ps=ps, n=n: e.tensor_tensor(out=y[:, i, n * 512:(n + 1) * 512], in0=ps[:], in1=y[:, i, n * 512:(n + 1) * 512], op=ALU.add),
                                 reads=[bps, by[i]], writes=[by[i]])

                g_cur = GUg(0)
                GUu(0, g_cur)
                for i in range(ETI):
                    hb = HID(i, g_cur)
                    g_next = GUg(i + 1) if i + 1 < ETI else None
                    if ex == 0 and pending_fn is not None:
                        pending_fn(i)
                    tb = TR(i, hb)
                    if g_next is not None:
                        GUu(i + 1, g_next)
                    DOWN(i, tb)
                    g_cur = g_next
                if ex == 0:
                    pending_fn = None

            def final_norm(i, eb=eb):
                ti = eb * ETI + i
                xt, bxt = xts.next()
                S.dma("sp", lambda e, xt=xt, ti=ti: e.dma_start(out=xt[:], in_=x2s[ti * 128:(ti + 1) * 128, :]), reads=[b_x2[ti]], writes=[bxt])
                S.op("dve", lambda e, xt=xt, i=i: e.tensor_tensor(out=y[:, i, :], in0=y[:, i, :], in1=xt[:], op=ALU.add), reads=[bxt, by[i]], writes=[by[i]])
                S.op("act", lambda e, i=i: e.activation(out=junk[:], in_=y[:, i, :], func=AF.Square, accum_out=fst[:, 0:1]), reads=[by[i]], writes=[bjunk, bfst])
                S.op("act", lambda e: e.activation(out=fst[:, 1:2], in_=fst[:, 0:1], func=AF.Sqrt, scale=1.0 / D, bias=eps_t[:]), reads=[bfst, bconst], writes=[bfst])
                S.op("dve", lambda e: e.reciprocal(out=fst[:, 2:3], in_=fst[:, 1:2]), reads=[bfst], writes=[bfst])
                S.op("dve", lambda e, xt=xt, i=i: e.scalar_tensor_tensor(out=xt[:], in0=y[:, i, :], scalar=fst[:, 2:3], in1=gB[:], op0=ALU.mult, op1=ALU.mult),
                     reads=[by[i], bfst, bgB], writes=[bxt])
                bo_ = Buf()
                S.dma("sp", lambda e, xt=xt, ti=ti: e.dma_start(out=out[ti * 128:(ti + 1) * 128, :], in_=xt[:]), reads=[bxt], writes=[bo_])
                outs.append(bo_)

            if eb + 1 < T // EB:
                pending_fn = final_norm
            else:
                for i in range(ETI):
                    final_norm(i)
        S.mute = False
        S.final_wait("sp", outs)
        S.barrier()
        S.emit_all()
    return nc


_CACHE = {}


def _consts():
    s = np.arange(128)[:, None]
    t = np.arange(128)[None, :]
    bd = ((s // 64 == t // 64) & (s <= t)).astype(np.uint32)
    rm = np.ones((128, 512), np.float32)
    rm[:, ::64] = 0.0
    return np.eye(128, dtype=np.float32), bd, rm


def run(inputs, dbg=False, limit=99):
    x = np.asarray(inputs["x"], np.float32)
    B, SEQ, _ = x.shape
    SEG = 4
    T = SEQ // SEG
    W = HALO
    key = (T, W, dbg, limit)
    if key not in _CACHE:
        _CACHE[key] = build(T, W, dbg, limit)
    nc = _CACHE[key]
    f = lambda k: np.ascontiguousarray(np.asarray(inputs[k], np.float32))
    ident, bd, rm = _consts()
    gains = np.stack([f("norm_mix")[0], f("norm_xattn")[0], f("norm_ffn")[0], f("norm_final"), f("norm_mem")[0]], 0)
    pscale = np.ascontiguousarray(f("pool_scale")[0].reshape(8, 128).T)
    lbl = np.ascontiguousarray(f("hgrn_lb_logits").reshape(2, 8, 128).transpose(2, 0, 1).reshape(128, 16))
    hnorm = np.ascontiguousarray(f("hgrn_norm")[0].reshape(128, 1))
    wr = np.concatenate([f("router_group")[0], f("router_expert")[0]], axis=1)
    wr = np.ascontiguousarray(wr.reshape(NCH, 128, 20).transpose(1, 0, 2).reshape(128, NCH * 20))
    shared = {
        "w_in": f("w_in")[0], "pool_w": f("pool_w")[0], "w_out": f("w_out")[0],
        "wq": f("xattn_wq")[0], "wk": f("xattn_wk")[0], "wv": f("xattn_wv")[0], "wo": f("xattn_wo")[0],
        "wr": wr, "w_gate": f("w_gate")[0], "w_up": f("w_up")[0], "w_down": f("w_down")[0],
        "gains": np.ascontiguousarray(gains), "pscale": pscale, "lbl": lbl, "hnorm": hnorm,
        "ident": ident, "bdmask": bd, "rmask": rm,
    }
    memf = f("mem")
    in_maps = []
    for c in range(B * SEG):
        b, j = divmod(c, SEG)
        seg = np.zeros((W + T, D), np.float32)
        if j == 0:
            seg[W:] = x[b, 0:T]
        else:
            seg[:] = x[b, j * T - W:(j + 1) * T]
        ic = np.empty((4, 512), np.float32)
        for g, w_ in enumerate((2, 4, 8, 16)):
            if j == 0:
                ic[g] = 1.0 / np.minimum(np.arange(512) + 1, w_)
            else:
                ic[g] = 1.0 / w_
        m = dict(shared)
        m["xseg"] = seg
        m["mem"] = np.ascontiguousarray(memf[b])
        m["invcnt"] = np.ascontiguousarray(np.broadcast_to(ic.reshape(1, 4 * 512), (128, 4 * 512)))
        in_maps.append(m)
    res = run_bass_kernel_spmd(nc, in_maps, core_ids=list(range(B * SEG)))
    names = ["out"] + (["x1d", "x2d"] if dbg else [])
    outd = {}
    for nm in names:
        o = np.empty((B, SEQ, D), np.float32)
        for c in range(B * SEG):
            b, j = divmod(c, SEG)
            o[b, j * T:(j + 1) * T] = np.asarray(res.results[c][nm])
        outd[nm] = o
    return outd


def kernel(**inputs):
    return run(inputs)["out"]
```

```python
import os
import numpy as np
import concourse.bass as bass
import concourse.mybir as mybir
from concourse.bass_utils import run_bass_kernel_spmd

F32 = mybir.dt.float32
BF16 = mybir.dt.bfloat16
U32 = mybir.dt.uint32
AF = mybir.ActivationFunctionType
ALU = mybir.AluOpType
AX = mybir.AxisListType

ENGS = ["pe", "act", "dve", "pool", "sp"]
N_DMA_SEMS = 32
D = 2048
NCH = 16
EPS = 1e-6
HALO = 512


class Buf:
    __slots__ = ("name", "w", "r")

    def __init__(self, name=""):
        self.name = name
        self.w = {}
        self.r = {}


class Sched:
    def __init__(self, nc):
        self.nc = nc
        self.prog = {e: [] for e in ENGS}
        self.cnt = {e: 0 for e in ENGS}
        self.seen = {e: {} for e in ENGS}
        self.sem = {e: nc.alloc_semaphore("sem_" + e) for e in ENGS}
        self.dsem = [nc.alloc_semaphore("dsem%d" % i) for i in range(N_DMA_SEMS)]
        self.dcnt = [0] * N_DMA_SEMS
        self.dnext2 = [0, 0]
        self.mute = False
        self.touched = set()

    def _deps(self, reads, writes):
        deps = {}
        for b in reads:
            for k, n in b.w.items():
                if deps.get(k, 0) < n:
                    deps[k] = n
        for b in writes:
            for d in (b.w, b.r):
                for k, n in d.items():
                    if deps.get(k, 0) < n:
                        deps[k] = n
        return deps

    def _waits(self, eng, deps):
        waits = []
        seen = self.seen[eng]
        for k, n in deps.items():
            if k == "pe" and eng == "pe":
                continue
            if seen.get(k, 0) < n:
                seen[k] = n
                waits.append((k, n))
        return waits

    def _semof(self, k):
        if isinstance(k, tuple):
            return self.dsem[k[1]], 16
        return self.sem[k], 1

    def op(self, eng, emit, reads=(), writes=()):
        if self.mute:
            return
        deps = self._deps(reads, writes)
        waits = self._waits(eng, deps)
        self.cnt[eng] += 1
        my = self.cnt[eng]
        self.prog[eng].append((waits, emit, self.sem[eng], 1))
        self.touched.update(reads)
        self.touched.update(writes)
        for b in writes:
            b.w = {eng: my}
            b.r = {}
        for b in reads:
            if b.r.get(eng, 0) < my:
                b.r[eng] = my

    def dma(self, eng, emit, reads=(), writes=()):
        if self.mute:
            return
        half = N_DMA_SEMS // 2
        q = 1 if eng == "pool" else 0
        i = q * half + self.dnext2[q]
        self.dnext2[q] = (self.dnext2[q] + 1) % half
        key = ("dma", i)
        deps = self._deps(reads, writes)
        if self.dcnt[i] > 0:
            deps[key] = max(deps.get(key, 0), self.dcnt[i])
        waits = self._waits(eng, deps)
        self.dcnt[i] += 1
        my = self.dcnt[i]
        self.prog[eng].append((waits, emit, self.dsem[i], 16))
        self.touched.update(reads)
        self.touched.update(writes)
        for b in writes:
            b.w = {key: my}
            b.r = {}
        for b in reads:
            if b.r.get(key, 0) < my:
                b.r[key] = my

    def final_wait(self, eng, bufs):
        deps = {}
        for b in bufs:
            for k, n in b.w.items():
                deps[k] = max(deps.get(k, 0), n)
        waits = self._waits(eng, deps)
        self.prog[eng].append((waits, None, None, 0))

    def barrier(self):
        deps = {e: self.cnt[e] for e in ENGS if self.cnt[e] > 0}
        for i in range(N_DMA_SEMS):
            if self.dcnt[i] > 0:
                deps[("dma", i)] = self.dcnt[i]
        for eng in ENGS:
            waits = []
            seen = self.seen[eng]
            for k, n in deps.items():
                if k == eng:
                    continue
                if seen.get(k, 0) < n:
                    seen[k] = n
                    waits.append((k, n))
            self.prog[eng].append((waits, None, None, 0))

    def emit_all(self):
        nc = self.nc

        def run(e, name):
            for waits, emit, sem, inc in self.prog[name]:
                for k, n in waits:
                    s, mult = self._semof(k)
                    e.wait_ge(s, n * mult)
                if emit is not None:
                    emit(e).then_inc(sem, inc)

        with nc.Block() as block:
            @block.tensor
            def _(e):
                run(e, "pe")

            @block.scalar
            def _(e):
                run(e, "act")

            @block.vector
            def _(e):
                run(e, "dve")

            @block.gpsimd
            def _(e):
                run(e, "pool")

            @block.sync
            def _(e):
                run(e, "sp")
        self.prog = {e: [] for e in ENGS}
        for b in self.touched:
            b.w = {}
            b.r = {}
        self.touched = set()
        self.cnt = {e: 0 for e in ENGS}
        self.seen = {e: {} for e in ENGS}
        self.dcnt = [0] * N_DMA_SEMS


class Ring:
    def __init__(self, items):
        self.items = items
        self.i = 0

    def next(self):
        it = self.items[self.i % len(self.items)]
        self.i += 1
        return it


def build(T, W, dbg=False, limit=99):
    nc = bass.Bass("TRN2", target_bir_lowering=False)
    S = Sched(nc)
    NBM = (W + T) // 512
    HB = W // 512
    NB = T // 512
    EB = 1024 if T % 1024 == 0 else 512
    ETI = EB // 128

    uid = [0]

    def sb(name, shape, dt):
        uid[0] += 1
        return nc.alloc_sbuf_tensor("%s_%d" % (name, uid[0]), shape, dt)

    def din(name, shape, dt=F32):
        return nc.dram_tensor(name, list(shape), dt, kind="ExternalInput").ap()

    xseg = din("xseg", [W + T, D])
    mem = din("mem", [256, D])
    w_in = din("w_in", [D, 5120])
    pool_w = din("pool_w", [4, 256, 256])
    w_out = din("w_out", [D, D])
    wq = din("wq", [D, D])
    wk = din("wk", [D, D])
    wv = din("wv", [D, D])
    wo = din("wo", [D, D])
    wr = din("wr", [128, NCH * 20])
    w_gate = din("w_gate", [16, D, 512])
    w_up = din("w_up", [16, D, 512])
    w_down = din("w_down", [16, 512, D])
    gains = din("gains", [5, D])
    pscale = din("pscale", [128, 8])
    lbl = din("lbl", [128, 16])
    hnorm = din("hnorm", [128, 1])
    ident = din("ident", [128, 128])
    bdmask = din("bdmask", [128, 128], U32)
    rmask = din("rmask", [128, 512])
    invcnt = din("invcnt", [128, 4 * 512])
    out = nc.dram_tensor("out", [T, D], F32, kind="ExternalOutput").ap()
    if dbg:
        x1d = nc.dram_tensor("x1d", [T, D], F32, kind="ExternalOutput").ap()
        x2d = nc.dram_tensor("x2d", [T, D], F32, kind="ExternalOutput").ap()

    hT1 = nc.dram_tensor("hT1", [NBM, 128, NCH * 512], BF16).ap()
    x1s = nc.dram_tensor("x1s", [T, D], F32).ap()
    aTs = nc.dram_tensor("aTs", [NB, 128, NCH * 512], BF16).ap()
    x2s = nc.dram_tensor("x2s", [T, D], F32).ap()
    h3Ts = nc.dram_tensor("h3Ts", [NB, 128, NCH * 512], BF16).ap()
    combs = nc.dram_tensor("combs", [T, 16], F32).ap()
    b_hT1 = [Buf() for _ in range(NBM)]
    b_x1 = [Buf() for _ in range(T // 128)]
    b_aT = [Buf() for _ in range(NB)]
    b_x2 = [Buf() for _ in range(T // 128)]
    b_h3T = [Buf() for _ in range(NB)]
    b_comb = [Buf() for _ in range(T // 128)]
    outs = []

    PS = [nc.alloc_psum_tensor("ps%d" % i, [128, 512], F32) for i in range(8)]
    BPS = [Buf("ps%d" % i) for i in range(8)]

    def psring(idx):
        return Ring([(PS[i], BPS[i]) for i in idx])

    id_f = sb("id_f", [128, 128], F32)
    id_b = sb("id_b", [128, 128], BF16)
    ones_f = sb("ones_f", [128, 128], F32)
    eps_t = sb("eps_t", [128, 1], F32)
    bconst = Buf("const")
    S.dma("sp", lambda e: e.dma_start(out=id_f[:], in_=ident), writes=[bconst])
    S.dma("pool", lambda e: e.dma_start(out=id_b[:], in_=ident), writes=[bconst])
    S.op("dve", lambda e: e.memset(ones_f[:], 1.0), writes=[bconst])
    S.op("dve", lambda e: e.memset(eps_t[:], EPS), writes=[bconst])

    flip = [0]

    def evac_copy(dst, src, reads, writes):
        flip[0] ^= 1
        if flip[0]:
            S.op("act", lambda e: e.activation(out=dst, in_=src, func=AF.Copy), reads=reads, writes=writes)
        else:
            S.op("dve", lambda e: e.tensor_copy(out=dst, in_=src), reads=reads, writes=writes)

    def mm_group(ps_ap, bps, pairs, reads):
        n = len(pairs)
        for i, (l, r) in enumerate(pairs):
            S.op("pe", lambda e, l=l, r=r, i=i: e.matmul(ps_ap, lhsT=l, rhs=r, start=(i == 0), stop=(i == n - 1)),
                 reads=reads, writes=[bps])

    class NormT:
        def __init__(self, tag, banks):
            self.tmp = Ring([(sb(tag + "junk%d" % i, [128, D], BF16), sb(tag + "xn%d" % i, [128, D], F32), sb(tag + "st%d" % i, [128, 4], F32),
                              Buf(), Buf(), Buf()) for i in range(2)])
            self.gB = sb(tag + "gB", [128, D], F32)
            self.bg = Buf()
            self.ring = psring(banks)

        def load_gain(self, row):
            S.dma("sp", lambda e: e.dma_start(out=self.gB[:], in_=gains[row:row + 1, :].to_broadcast([128, D])), writes=[self.bg])

        def run(self, src, bsrc, dst_bf, bdst, dst_f=None, bdst_f=None):
            junk, xn, st, bj, bxn, bst = self.tmp.next()
            S.op("act", lambda e: e.activation(out=junk[:], in_=src, func=AF.Square, accum_out=st[:, 0:1]),
                 reads=[bsrc], writes=[bj, bst])
            S.op("act", lambda e: e.activation(out=st[:, 1:2], in_=st[:, 0:1], func=AF.Sqrt, scale=1.0 / D, bias=eps_t[:]),
                 reads=[bst, bconst], writes=[bst])
            S.op("dve", lambda e: e.reciprocal(out=st[:, 2:3], in_=st[:, 1:2]), reads=[bst], writes=[bst])
            S.op("dve", lambda e: e.scalar_tensor_tensor(out=xn[:], in0=src, scalar=st[:, 2:3], in1=self.gB[:],
                                                         op0=ALU.mult, op1=ALU.mult),
                 reads=[bsrc, bst, self.bg], writes=[bxn])
            for b4 in range(4):
                ps, bps = self.ring.next()
                for j in range(4):
                    c = b4 * 4 + j
                    S.op("pe", lambda e, ps=ps, j=j, c=c: e.transpose(ps[:, j * 128:(j + 1) * 128], xn[:, c * 128:(c + 1) * 128], id_f[:]),
                         reads=[bxn, bconst], writes=[bps])
                src3 = ps[:].rearrange("p (c n) -> p c n", c=4)
                if dst_f is None:
                    evac_copy(dst_bf[:, b4 * 4:(b4 + 1) * 4, :], src3, [bps], [bdst])
                else:
                    evac_copy(dst_f[:, b4 * 4:(b4 + 1) * 4, :], src3, [bps], [bdst_f])
                    S.op("pool", lambda e, b4=b4: e.tensor_copy(out=dst_bf[:, b4 * 4:(b4 + 1) * 4, :], in_=dst_f[:, b4 * 4:(b4 + 1) * 4, :]),
                         reads=[bdst_f], writes=[bdst])

    def wslice(w, r0, r1, c0, c1):
        return w[r0:r1, c0:c1].rearrange("(c p) n -> p c n", p=128)

    S.mute = limit < 1
    with nc.reset_on_exit():
        nt = NormT("n1", [0, 1, 2, 3, 4, 5, 6, 7])
        nt.load_gain(0)
        xts = Ring([(sb("n1x%d" % i, [128, D], F32), Buf()) for i in range(3)])
        hbs = Ring([(sb("n1h%d" % i, [128, NCH, 512], BF16), Buf()) for i in range(2)])
        for b in range(NBM):
            hb, bhb = hbs.next()
            for t in range(4):
                xt, bxt = xts.next()
                r0 = b * 512 + t * 128
                S.dma("sp", lambda e, xt=xt, r0=r0: e.dma_start(out=xt[:], in_=xseg[r0:r0 + 128, :]), writes=[bxt])
                nt.run(xt[:], bxt, hb[:, :, t * 128:(t + 1) * 128], bhb)
            S.dma("sp", lambda e, hb=hb, b=b: e.dma_start(out=hT1[b], in_=hb[:].rearrange("p c n -> p (c n)")),
                  reads=[bhb], writes=[b_hT1[b]])
        S.barrier()
        S.emit_all()

    def interleave(gens):
        gens = list(gens)
        while gens:
            for g in list(gens):
                try:
                    next(g)
                except StopIteration:
                    gens.remove(g)

    for p in range(4):
        S.mute = limit < 2 + p
        with nc.reset_on_exit():
            win = sb("win", [128, NCH, 5, 256], BF16)
            wout = sb("wout", [128, 4, D], BF16)
            pw = sb("pw", [128, 2, 256], BF16)
            bwin, bwout, bpw = [Buf() for _ in range(5)], [Buf() for _ in range(4)], Buf()
            for g5 in (0, 3, 1, 2, 4):
                c0 = g5 * 1024 + 256 * p
                S.dma("pool", lambda e, g5=g5, c0=c0: e.dma_start(out=win[:, :, g5, :], in_=wslice(w_in, 0, D, c0, c0 + 256)), writes=[bwin[g5]])
            S.dma("pool", lambda e: e.dma_start(out=pw[:], in_=pool_w[p].rearrange("(c q) n -> q c n", q=128)), writes=[bpw])
            for n in range(4):
                S.dma("pool", lambda e, n=n: e.dma_start(out=wout[:, 0:2, n * 512:(n + 1) * 512], in_=wslice(w_out, 256 * p, 256 * p + 256, n * 512, (n + 1) * 512)), writes=[bwout[n]])
                S.dma("pool", lambda e, n=n: e.dma_start(out=wout[:, 2:4, n * 512:(n + 1) * 512], in_=wslice(w_out, 1024 + 256 * p, 1024 + 256 * p + 256, n * 512, (n + 1) * 512)), writes=[bwout[n]])
            small = sb("msmall", [128, 64], F32)
            bsmall = Buf()
            S.dma("sp", lambda e: e.dma_start(out=small[:, 0:8], in_=pscale), writes=[bsmall])
            S.dma("sp", lambda e: e.dma_start(out=small[:, 8:24], in_=lbl), writes=[bsmall])
            S.dma("sp", lambda e: e.dma_start(out=small[:, 24:25], in_=hnorm), writes=[bsmall])
            S.op("dve", lambda e: e.tensor_tensor(out=small[:, 32:34], in0=small[:, 8 + 2 * p:10 + 2 * p], in1=small[:, 16 + 2 * p:18 + 2 * p], op=ALU.subtract),
                 reads=[bsmall], writes=[bsmall])
            S.op("act", lambda e: e.activation(out=small[:, 25:27], in_=small[:, 32:34], func=AF.Sigmoid), reads=[bsmall], writes=[bsmall])
            S.op("dve", lambda e: e.tensor_scalar(out=small[:, 27:29], in0=small[:, 25:27], scalar1=-1.0, scalar2=1.0, op0=ALU.mult, op1=ALU.add),
                 reads=[bsmall], writes=[bsmall])
            S.op("dve", lambda e: e.tensor_scalar(out=small[:, 29:31], in0=small[:, 27:29], scalar1=-1.0, scalar2=None, op0=ALU.mult),
                 reads=[bsmall], writes=[bsmall])
            rm_sb = sb("rm_sb", [128, 512], F32)
            bd_sb = sb("bd_sb", [128, 128], U32)
            ic_sb = sb("ic_sb", [128, 512], F32)
            bcm_ = Buf()
            S.dma("sp", lambda e: e.dma_start(out=rm_sb[:], in_=rmask), writes=[bcm_])
            S.dma("sp", lambda e: e.dma_start(out=bd_sb[:], in_=bdmask), writes=[bcm_])
            S.dma("sp", lambda e: e.dma_start(out=ic_sb[:], in_=invcnt[:, p * 512:(p + 1) * 512]), writes=[bcm_])

            hTr = Ring([(sb("mh%d" % i, [128, NCH, 512], BF16), Buf()) for i in range(2)])
            uT = sb("uT", [128, 2, 528], F32)
            b_uh, b_um = Buf(), Buf()
            s_a = sb("s_a", [128, 2, 528], F32)
            s_b = sb("s_b", [128, 2, 528], F32)
            bsa, bsb = Buf(), Buf()
            pooled = sb("pooled", [128, 2, 512], BF16)
            bpooled = Buf()
            tmpH = []
            for hh in range(2):
                d_ = {}
                for nm in ["q", "sig", "lf", "kk", "a", "at"]:
                    d_[nm] = sb("h%d%s" % (hh, nm), [128, 512], F32)
                    d_["b" + nm] = Buf()
                d_["e1"], d_["be1"] = d_["lf"], d_["blf"]
                d_["e2"], d_["be2"] = d_["sig"], d_["bsig"]
                tmpH.append(d_)
            sets = []
            for si in range(2):
                st_ = {"hd": []}
                for hh in range(2):
                    d_ = {}
                    for nm in ["qt", "ktT"]:
                        d_[nm] = sb("s%dh%d%s" % (si, hh, nm), [128, 512], BF16)
                        d_["b" + nm] = Buf()
                    d_["ktok"] = sb("s%dh%dktok" % (si, hh), [128, 4, 128], BF16)
                    d_["bktok"] = Buf()
                    d_["ex"] = sb("s%dh%dex" % (si, hh), [128, 32], F32)
                    d_["bex"] = Buf()
                    st_["hd"].append(d_)
                st_["gT"] = sb("s%dgT" % si, [128, 2, 512], F32)
                st_["bgT"] = [Buf(), Buf()]
                st_["v"] = sb("s%dv" % si, [128, 4, 256], BF16)
                st_["bv"] = Buf()
                st_["mixT"] = sb("s%dmixT" % si, [128, 4, 512], BF16)
                st_["bmix"] = [Buf() for _ in range(4)]
                sets.append(st_)
            rec = []
            for hh in range(2):
                d_ = {}
                d_["S"] = sb("h%dS" % hh, [128, 128], F32)
                d_["bS"] = Buf()
                d_["sref"] = Ring([(sb("h%dsr%d" % (hh, i), [128, 128], BF16), Buf()) for i in range(3)])
                d_["tst8"] = [(sb("h%dts%d" % (hh, i), [128, 128], F32), Buf()) for i in range(8)]
                d_["ssb4"] = [(sb("h%dss%d" % (hh, i), [128, 128], BF16), Buf()) for i in range(4)]
                for (tt, bb) in d_["ssb4"]:
                    S.op("pool", lambda e, tt=tt: e.memset(tt[:], 0.0), writes=[bb])
                S.op("pool", lambda e, d_=d_: e.memset(d_["S"][:], 0.0), writes=[d_["bS"]])
                rec.append(d_)
            o_sb = sb("o_sb", [128, 512], F32)
            sq_sb = sb("sq_sb", [128, 512], F32)
            rs_sb = sb("rs_sb", [128, 512], F32)
            bo, bsq, brs = Buf(), Buf(), Buf()
            accr = Ring([(sb("acc%d" % i, [128, D], F32), Buf()) for i in range(2)])
            ringA = psring([0, 1, 2])
            PS_S, PS_O, PS_ST, PS_KT, PS_SS = 3, 4, 6, 7, 7

            def front(b):
                halo = b < HB
                st_ = sets[b % 2]
                mixT, bmix = st_["mixT"], st_["bmix"]
                v_sb, bv = st_["v"], st_["bv"]
                gT, bgT = st_["gT"], st_["bgT"]
                hT, bhT = hTr.next()
                S.dma("sp", lambda e, hT=hT, b=b: e.dma_start(out=hT[:].rearrange("p c n -> p (c n)"), in_=hT1[b]),
                      reads=[b_hT1[b]], writes=[bhT])
                if b == 0:
                    S.op("dve", lambda e: e.memset(uT[:, :, 0:16], 0.0), writes=[b_uh])
                else:
                    S.op("dve", lambda e: e.tensor_copy(out=uT[:, :, 0:16], in_=uT[:, :, 512:528]), reads=[b_um], writes=[b_uh])
                for j in range(2):
                    ps, bps = ringA.next()
                    mm_group(ps[:], bps, [(win[:, c, 0, j * 128:(j + 1) * 128], hT[:, c, :]) for c in range(NCH)], [bwin[0], bhT])
                    S.op("act", lambda e, ps=ps, j=j: e.activation(out=uT[:, j, 16:528], in_=ps[:], func=AF.Copy), reads=[bps], writes=[b_um])
                    yield
                for t in range(4):
                    ps, bps = ringA.next()
                    mm_group(ps[:, 0:256], bps, [(hT[:, c, t * 128:(t + 1) * 128], win[:, c, 3, :]) for c in range(NCH)], [bwin[3], bhT])
                    S.op("dve", lambda e, ps=ps, t=t: e.tensor_copy(out=v_sb[:, t, :], in_=ps[:, 0:256]), reads=[bps], writes=[bv])
                    if t % 2 == 1:
                        yield
                if not halo:
                    k = p + 1
                    src, bsrc = uT, None
                    for step in range(1, k + 1):
                        sh = 1 << (step - 1)
                        lo = 1 << step
                        dst, bdst = (s_a, bsa) if step % 2 == 1 else (s_b, bsb)
                        if step == 1:
                            S.op("pool", lambda e, dst=dst, lo=lo, sh=sh: e.tensor_tensor(out=dst[:, :, lo:528], in0=uT[:, :, lo:528], in1=uT[:, :, lo - sh:528 - sh], op=ALU.add),
                                 reads=[b_uh, b_um], writes=[bdst])
                        else:
                            S.op("pool", lambda e, dst=dst, src=src, lo=lo, sh=sh: e.tensor_tensor(out=dst[:, :, lo:528], in0=src[:, :, lo:528], in1=src[:, :, lo - sh:528 - sh], op=ALU.add),
                                 reads=[bsrc], writes=[bdst])
                        src, bsrc = dst, bdst
                    wnd = float(1 << k)
                    if b == HB:
                        other, bother = (s_b, bsb) if src is s_a else (s_a, bsa)
                        for j in range(2):
                            S.op("dve", lambda e, j=j, src=src, other=other: e.tensor_tensor(out=other[:, j, 16:528], in0=src[:, j, 16:528], in1=ic_sb[:], op=ALU.mult),
                                 reads=[bsrc, bcm_], writes=[bother])
                        S.op("dve", lambda e, other=other: e.tensor_tensor(out=pooled[:], in0=other[:, :, 16:528], in1=uT[:, :, 16:528], op=ALU.subtract),
                             reads=[bother, b_um], writes=[bpooled])
                    else:
                        S.op("dve", lambda e, src=src, wnd=wnd: e.scalar_tensor_tensor(out=pooled[:], in0=src[:, :, 16:528], scalar=1.0 / wnd, in1=uT[:, :, 16:528],
                                                                                   op0=ALU.mult, op1=ALU.subtract),
                             reads=[bsrc, b_um], writes=[bpooled])
                    yield
                for hh in range(2):
                    h_ = tmpH[hh]
                    o_ = st_["hd"][hh]
                    lb_ap = small[:, 25 + hh:26 + hh]
                    oml_ap = small[:, 27 + hh:28 + hh]
                    noml_ap = small[:, 29 + hh:30 + hh]
                    ps, bps = ringA.next()
                    mm_group(ps[:], bps, [(win[:, c, 2, hh * 128:(hh + 1) * 128], hT[:, c, :]) for c in range(NCH)], [bwin[2], bhT])
                    S.op("act", lambda e, ps=ps, h_=h_: e.activation(out=h_["sig"][:], in_=ps[:], func=AF.Sigmoid), reads=[bps], writes=[h_["bsig"]])
                    yield
                    ps, bps = ringA.next()
                    mm_group(ps[:], bps, [(win[:, c, 1, hh * 128:(hh + 1) * 128], hT[:, c, :]) for c in range(NCH)], [bwin[1], bhT])
                    S.op("act", lambda e, ps=ps, h_=h_: e.activation(out=h_["q"][:], in_=ps[:], func=AF.Silu), reads=[bps], writes=[h_["bq"]])
                    S.op("act", lambda e, h_=h_, oml_ap=oml_ap, lb_ap=lb_ap: e.activation(out=h_["lf"][:], in_=h_["sig"][:], func=AF.Ln, scale=oml_ap, bias=lb_ap),
                         reads=[h_["bsig"], bsmall], writes=[h_["blf"]])
                    S.op("dve", lambda e, h_=h_: e.tensor_tensor_scan(out=h_["a"][:], data0=rm_sb[:], data1=h_["lf"][:], initial=0.0, op0=ALU.mult, op1=ALU.add),
                         reads=[h_["blf"], bcm_], writes=[h_["ba"]])
                    yield
                    ps, bps = ringA.next()
                    mm_group(ps[:], bps, [(win[:, c, 4, hh * 128:(hh + 1) * 128], hT[:, c, :]) for c in range(NCH)], [bwin[4], bhT])
                    S.op("act", lambda e, ps=ps, hh=hh, gT=gT: e.activation(out=gT[:, hh, :], in_=ps[:], func=AF.Silu), reads=[bps], writes=[bgT[hh]])
                    a3 = h_["a"][:].rearrange("p (c n) -> p c n", c=8)
                    S.op("dve", lambda e, h_=h_, a3=a3: e.tensor_tensor(out=h_["at"][:].rearrange("p (c n) -> p c n", c=8), in0=a3, in1=a3[:, :, 31:32].to_broadcast([128, 8, 64]), op=ALU.subtract),
                         reads=[h_["ba"]], writes=[h_["bat"]])
                    S.op("pool", lambda e, h_=h_, noml_ap=noml_ap, oml_ap=oml_ap: e.tensor_scalar(out=h_["kk"][:], in0=h_["sig"][:], scalar1=noml_ap, scalar2=oml_ap, op0=ALU.mult, op1=ALU.add),
                         reads=[h_["bsig"], bsmall], writes=[h_["bkk"]])
                    yield
                    S.op("act", lambda e, h_=h_: e.activation(out=h_["e1"][:], in_=h_["at"][:], func=AF.Exp), reads=[h_["bat"]], writes=[h_["be1"]])
                    S.op("act", lambda e, h_=h_: e.activation(out=h_["e2"][:], in_=h_["at"][:], func=AF.Exp, scale=-1.0), reads=[h_["bat"]], writes=[h_["be2"]])
                    a16 = h_["a"][:].rearrange("p (c n) -> p c n", n=32)[:, :, 31:32]
                    S.op("act", lambda e, o_=o_, a16=a16: e.activation(out=o_["ex"][:, 0:16].rearrange("p (c n) -> p c n", n=1), in_=a16, func=AF.Exp),
                         reads=[h_["ba"]], writes=[o_["bex"]])
                    S.op("dve", lambda e, o_=o_, a3=a3: e.tensor_tensor(out=o_["ex"][:, 16:24].rearrange("p (c n) -> p c n", n=1), in0=a3[:, :, 63:64], in1=a3[:, :, 31:32], op=ALU.subtract),
                         reads=[h_["ba"]], writes=[o_["bex"]])
                    S.op("act", lambda e, o_=o_: e.activation(out=o_["ex"][:, 24:32], in_=o_["ex"][:, 16:24], func=AF.Exp), reads=[o_["bex"]], writes=[o_["bex"]])
                    yield
                    S.op("dve", lambda e, h_=h_, o_=o_: e.tensor_tensor(out=o_["qt"][:], in0=h_["q"][:], in1=h_["e1"][:], op=ALU.mult),
                         reads=[h_["bq"], h_["be1"]], writes=[o_["bqt"]])
                    S.op("pool", lambda e, h_=h_, o_=o_: e.tensor_tensor(out=o_["ktT"][:], in0=h_["kk"][:], in1=h_["e2"][:], op=ALU.mult),
                         reads=[h_["bkk"], h_["be2"]], writes=[o_["bktT"]])
                    yield
                    psb = PS[PS_KT][:].bitcast(BF16)
                    for t in range(4):
                        S.op("pe", lambda e, o_=o_, t=t, psb=psb: e.transpose(psb[:, t * 128:(t + 1) * 128], o_["ktT"][:, t * 128:(t + 1) * 128], id_b[:]),
                             reads=[o_["bktT"], bconst], writes=[BPS[PS_KT]])
                    S.op("act", lambda e, o_=o_, psb=psb: e.activation(out=o_["ktok"][:].rearrange("p t n -> p (t n)"), in_=psb[:, 0:512], func=AF.Copy),
                         reads=[BPS[PS_KT]], writes=[o_["bktok"]])
                    yield
                if not halo:
                    for oc in range(2):
                        ps, bps = ringA.next()
                        mm_group(ps[:], bps, [(pw[:, ic, oc * 128:(oc + 1) * 128], pooled[:, ic, :]) for ic in range(2)], [bpw, bpooled])
                        S.op("act", lambda e, ps=ps, oc=oc, mixT=mixT: e.activation(out=mixT[:, oc, :], in_=ps[:], func=AF.Copy, scale=small[:, 2 * p + oc:2 * p + oc + 1]),
                             reads=[bps, bsmall], writes=[bmix[oc]])
                    yield

            def back(b):
                halo = b < HB
                st_ = sets[b % 2]
                mixT, bmix = st_["mixT"], st_["bmix"]
                v_sb, bv = st_["v"], st_["bv"]
                gT, bgT = st_["gT"], st_["bgT"]
                for t in range(4):
                    for hh in range(2):
                        h_ = st_["hd"][hh]
                        r_ = rec[hh]
                        S.op("pe", lambda e, h_=h_, t=t: e.matmul(PS[PS_S][:, 0:128], lhsT=h_["ktT"][:, t * 128:(t + 1) * 128], rhs=h_["qt"][:, t * 128:(t + 1) * 128], start=True, stop=True),
                             reads=[h_["bktT"], h_["bqt"]], writes=[BPS[PS_S]])
                        ssb, bssb = r_["ssb4"][t]
                        S.op("dve", lambda e, ssb=ssb: e.copy_predicated(out=ssb[:], mask=bd_sb[:], data=PS[PS_S][:, 0:128]),
                             reads=[BPS[PS_S], bcm_], writes=[bssb])
                        for cc in range(2):
                            c = 2 * t + cc
                            S.op("pe", lambda e, h_=h_, t=t, cc=cc, hh=hh, v_sb=v_sb: e.matmul(PS[PS_ST][:, 0:128], lhsT=h_["ktok"][cc * 64:(cc + 1) * 64, t, :], rhs=v_sb[cc * 64:(cc + 1) * 64, t, hh * 128:(hh + 1) * 128], start=True, stop=True),
                                 reads=[h_["bktok"], bv], writes=[BPS[PS_ST]])
                            tst, btst = r_["tst8"][c]
                            S.op("act", lambda e, h_=h_, tst=tst, c=c: e.activation(out=tst[:], in_=PS[PS_ST][:, 0:128], func=AF.Copy, scale=h_["ex"][:, 24 + c:25 + c]),
                                 reads=[BPS[PS_ST], h_["bex"]], writes=[btst])
                        yield
                for t in range(4):
                    for hh in range(2):
                        h_ = st_["hd"][hh]
                        r_ = rec[hh]
                        pso, bpso = PS[PS_O + hh], BPS[PS_O + hh]
                        ssb, bssb = r_["ssb4"][t]
                        S.op("pe", lambda e, ssb=ssb, t=t, hh=hh, pso=pso, v_sb=v_sb: e.matmul(pso[:, t * 128:(t + 1) * 128], lhsT=v_sb[:, t, hh * 128:(hh + 1) * 128], rhs=ssb[:], start=True, stop=False),
                             reads=[bssb, bv], writes=[bpso])
                        for cc in range(2):
                            c = 2 * t + cc
                            sref, bsref = r_["sref"].next()
                            S.op("pool", lambda e, h_=h_, r_=r_, sref=sref, c=c: e.tensor_scalar(out=sref[:], in0=r_["S"][:], scalar1=h_["ex"][:, 2 * c:2 * c + 1], scalar2=0.0, op0=ALU.mult, op1=ALU.add),
                                 reads=[r_["bS"], h_["bex"]], writes=[bsref])
                            tst, btst = r_["tst8"][c]
                            S.op("pool", lambda e, h_=h_, r_=r_, c=c: e.tensor_scalar(out=r_["S"][:], in0=r_["S"][:], scalar1=h_["ex"][:, 2 * c + 1:2 * c + 2], scalar2=0.0, op0=ALU.mult, op1=ALU.add),
                                 reads=[r_["bS"], h_["bex"]], writes=[r_["bS"]])
                            S.op("pool", lambda e, r_=r_, tst=tst: e.tensor_tensor(out=r_["S"][:], in0=r_["S"][:], in1=tst[:], op=ALU.add),
                                 reads=[r_["bS"], btst], writes=[r_["bS"]])
                            c0 = t * 128 + cc * 64
                            S.op("pe", lambda e, h_=h_, sref=sref, c0=c0, pso=pso, cc=cc: e.matmul(pso[:, c0:c0 + 64], lhsT=sref[:], rhs=h_["qt"][:, c0:c0 + 64], start=False, stop=(cc == 1)),
                                 reads=[bsref, h_["bqt"]], writes=[bpso])
                        yield
                if halo:
                    return
                for hh in range(2):
                    pso, bpso = PS[PS_O + hh], BPS[PS_O + hh]
                    S.op("act", lambda e, pso=pso: e.activation(out=o_sb[:], in_=pso[:], func=AF.Copy), reads=[bpso], writes=[bo])
                    S.op("act", lambda e, pso=pso: e.activation(out=sq_sb[:], in_=pso[:], func=AF.Square), reads=[bpso], writes=[bsq])
                    yield
                    S.op("pe", lambda e: e.matmul(PS[PS_SS][:], lhsT=ones_f[:], rhs=sq_sb[:], start=True, stop=True), reads=[bsq, bconst], writes=[BPS[PS_SS]])
                    S.op("act", lambda e: e.activation(out=rs_sb[:], in_=PS[PS_SS][:], func=AF.Ln, scale=1.0 / 128, bias=eps_t[:]), reads=[BPS[PS_SS], bconst], writes=[brs])
                    S.op("act", lambda e: e.activation(out=rs_sb[:], in_=rs_sb[:], func=AF.Exp, scale=-0.5), reads=[brs], writes=[brs])
                    S.op("dve", lambda e: e.scalar_tensor_tensor(out=o_sb[:], in0=o_sb[:], scalar=small[:, 24:25], in1=rs_sb[:], op0=ALU.mult, op1=ALU.mult),
                         reads=[bo, brs, bsmall], writes=[bo])
                    S.op("dve", lambda e, hh=hh, mixT=mixT, gT=gT: e.tensor_tensor(out=mixT[:, 2 + hh, :], in0=o_sb[:], in1=gT[:, hh, :], op=ALU.mult),
                         reads=[bo, bgT[hh]], writes=[bmix[2 + hh]])
                    yield
                for t in range(4):
                    acc, bacc = accr.next()
                    ti = (b - HB) * 4 + t
                    if p == 0:
                        r0 = b * 512 + t * 128
                        S.dma("sp", lambda e, acc=acc, r0=r0: e.dma_start(out=acc[:], in_=xseg[r0:r0 + 128, :]), writes=[bacc])
                    else:
                        S.dma("sp", lambda e, acc=acc, ti=ti: e.dma_start(out=acc[:], in_=x1s[ti * 128:(ti + 1) * 128, :]), reads=[b_x1[ti]], writes=[bacc])
                    for n in range(4):
                        ps, bps = ringA.next()
                        mm_group(ps[:], bps, [(mixT[:, c, t * 128:(t + 1) * 128], wout[:, c, n * 512:(n + 1) * 512]) for c in range(4)], [bwout[n]] + bmix)
                        S.op("dve", lambda e, acc=acc, ps=ps, n=n: e.tensor_tensor(out=acc[:, n * 512:(n + 1) * 512], in0=ps[:], in1=acc[:, n * 512:(n + 1) * 512], op=ALU.add),
                             reads=[bps, bacc], writes=[bacc])
                        if n % 2 == 1 and not os.environ.get("NOYIELD_OP"):
                            yield
                    S.dma("sp", lambda e, acc=acc, ti=ti: e.dma_start(out=x1s[ti * 128:(ti + 1) * 128, :], in_=acc[:]), reads=[bacc], writes=[b_x1[ti]])
                    if dbg and p == 3:
                        bo_ = Buf()
                        S.dma("sp", lambda e, acc=acc, ti=ti: e.dma_start(out=x1d[ti * 128:(ti + 1) * 128, :], in_=acc[:]), reads=[bacc], writes=[bo_])
                        outs.append(bo_)

            interleave([front(0)])
            for b in range(1, NBM):
                interleave([back(b - 1), front(b)])
            interleave([back(NBM - 1)])
            S.barrier()
            S.emit_all()

    S.mute = limit < 6
    with nc.reset_on_exit():
        KT = sb("KT", [128, NCH, 256], BF16)
        V = sb("V", [128, 2, D], BF16)
        bKT, bV = Buf(), Buf()
        nt = NormT("x1n", [0, 1, 2, 3])
        ringB = psring([4, 5, 6, 7])
        with nc.reset_on_exit():
            nt.load_gain(4)
            mT = sb("mT", [128, NCH, 256], BF16)
            bmT = Buf()
            mts = Ring([(sb("memt%d" % i, [128, D], F32), Buf()) for i in range(2)])
            for mt in range(2):
                xt, bxt = mts.next()
                S.dma("sp", lambda e, xt=xt, mt=mt: e.dma_start(out=xt[:], in_=mem[mt * 128:(mt + 1) * 128, :]), writes=[bxt])
                nt.run(xt[:], bxt, mT[:, :, mt * 128:(mt + 1) * 128], bmT)
            wkr = Ring([(sb("wkv%d" % i, [128, NCH, 512], BF16), Buf()) for i in range(2)])
            for n in range(4):
                wt, bwt = wkr.next()
                S.dma("pool", lambda e, wt=wt, n=n: e.dma_start(out=wt[:], in_=wslice(wk, 0, D, n * 512, (n + 1) * 512)), writes=[bwt])
                for jj in range(4):
                    ps, bps = ringB.next()
                    mm_group(ps[:, 0:256], bps, [(wt[:, c, jj * 128:(jj + 1) * 128], mT[:, c, :]) for c in range(NCH)], [bwt, bmT])
                    evac_copy(KT[:, n * 4 + jj, :], ps[:, 0:256], [bps], [bKT])
            for n in range(4):
                wt, bwt = wkr.next()
                S.dma("pool", lambda e, wt=wt, n=n: e.dma_start(out=wt[:], in_=wslice(wv, 0, D, n * 512, (n + 1) * 512)), writes=[bwt])
                for mt in range(2):
                    ps, bps = ringB.next()
                    mm_group(ps[:], bps, [(mT[:, c, mt * 128:(mt + 1) * 128], wt[:, c, :]) for c in range(NCH)], [bwt, bmT])
                    evac_copy(V[:, mt, n * 512:(n + 1) * 512], ps[:], [bps], [bV])
            S.barrier()
            S.emit_all()
        nt.load_gain(1)
        wq_sb = sb("wq_sb", [128, NCH, D], BF16)
        bwq4 = [Buf() for _ in range(4)]
        for n in range(4):
            S.dma("pool", lambda e, n=n: e.dma_start(out=wq_sb[:, :, n * 512:(n + 1) * 512], in_=wslice(wq, 0, D, n * 512, (n + 1) * 512)), writes=[bwq4[n]])
        xts = Ring([(sb("x1x%d" % i, [128, D], F32), Buf()) for i in range(2)])
        h2T = sb("h2T", [128, NCH, 512], BF16)
        bh2 = Buf()
        qTs = [(sb("qT%d" % i, [128, NCH, 512], BF16), Buf()) for i in range(2)]
        aT, baT = sb("aT0", [128, NCH, 512], BF16), Buf()
        attr = Ring([(sb("pex%d" % i, [128, 256], F32), sb("pn%d" % i, [128, 256], BF16), sb("PT%d" % i, [128, 2, 128], BF16), sb("sst%d" % i, [128, 8], F32),
                      Buf(), Buf(), Buf(), Buf()) for i in range(3)])
        SC = 512 ** -0.5

        def x1_norm(b):
            for t in range(4):
                xt, bxt = xts.next()
                ti = b * 4 + t
                S.dma("sp", lambda e, xt=xt, ti=ti: e.dma_start(out=xt[:], in_=x1s[ti * 128:(ti + 1) * 128, :]), reads=[b_x1[ti]], writes=[bxt])
                nt.run(xt[:], bxt, h2T[:, :, t * 128:(t + 1) * 128], bh2)

        def x1_qgroup(b, j):
            qT, bqT = qTs[b % 2]
            ps, bps = ringB.next()
            mm_group(ps[:], bps, [(wq_sb[:, c, j * 128:(j + 1) * 128], h2T[:, c, :]) for c in range(NCH)], [bwq4[j // 4], bh2])
            evac_copy(qT[:, j, :], ps[:], [bps], [bqT])

        def x1_attn(b, t, h):
            qT, bqT = qTs[b % 2]
            pex, pn, PT, sst, bpex, bpn, bPT, bsst = attr.next()
            ps, bps = ringB.next()
            mm_group(ps[:, 0:256], bps, [(qT[:, 4 * h + c4, t * 128:(t + 1) * 128], KT[:, 4 * h + c4, :]) for c4 in range(4)], [bqT, bKT])
            S.op("dve", lambda e, ps=ps, sst=sst: e.tensor_reduce(out=sst[:, 0:1], in_=ps[:, 0:256], axis=AX.X, op=ALU.max), reads=[bps], writes=[bsst])
            S.op("dve", lambda e, sst=sst: e.tensor_scalar(out=sst[:, 1:2], in0=sst[:, 0:1], scalar1=-SC, scalar2=None, op0=ALU.mult), reads=[bsst], writes=[bsst])
            S.op("act", lambda e, ps=ps, sst=sst, pex=pex: e.activation(out=pex[:], in_=ps[:, 0:256], func=AF.Exp, scale=SC, bias=sst[:, 1:2], accum_out=sst[:, 2:3]),
                 reads=[bps, bsst], writes=[bpex, bsst])
            S.op("dve", lambda e, sst=sst: e.reciprocal(out=sst[:, 3:4], in_=sst[:, 2:3]), reads=[bsst], writes=[bsst])
            S.op("dve", lambda e, sst=sst, pex=pex, pn=pn: e.tensor_scalar(out=pn[:], in0=pex[:], scalar1=sst[:, 3:4], scalar2=None, op0=ALU.mult), reads=[bpex, bsst], writes=[bpn])
            ps2, bps2 = ringB.next()
            psb = ps2[:].bitcast(BF16)
            for mc in range(2):
                S.op("pe", lambda e, psb=psb, mc=mc, pn=pn: e.transpose(psb[:, mc * 128:(mc + 1) * 128], pn[:, mc * 128:(mc + 1) * 128], id_b[:]),
                     reads=[bpn, bconst], writes=[bps2])
            S.op("act", lambda e, psb=psb, PT=PT: e.activation(out=PT[:].rearrange("p c n -> p (c n)"), in_=psb[:, 0:256], func=AF.Copy), reads=[bps2], writes=[bPT])
            ps3, bps3 = ringB.next()
            for j in range(4):
                mm_group(ps3[:, j * 128:(j + 1) * 128], bps3,
                         [(V[:, mc, h * 512 + j * 128:h * 512 + (j + 1) * 128], PT[:, mc, :]) for mc in range(2)], [bV, bPT])
            evac_copy(aT[:, 4 * h:4 * h + 4, t * 128:(t + 1) * 128], ps3[:].rearrange("p (c n) -> p c n", c=4), [bps3], [baT])

        x1_norm(0)
        for j in range(NCH):
            x1_qgroup(0, j)
        for b in range(NB):
            if b + 1 < NB:
                x1_norm(b + 1)
            for k in range(16):
                x1_attn(b, k // 4, k % 4)
                if b + 1 < NB:
                    x1_qgroup(b + 1, k)
            S.dma("sp", lambda e, b=b: e.dma_start(out=aTs[b], in_=aT[:].rearrange("p c n -> p (c n)")), reads=[baT], writes=[b_aT[b]])
        S.barrier()
        S.emit_all()

    S.mute = limit < 6.5
    with nc.reset_on_exit():
        nt = NormT("x2n", [0, 1, 2, 3])
        nt.load_gain(2)
        ringB = psring([4, 5, 6])
        wo_sb = sb("wo_sb", [128, NCH, D], BF16)
        bwo = Buf()
        for n in range(4):
            S.dma("pool", lambda e, n=n: e.dma_start(out=wo_sb[:, :, n * 512:(n + 1) * 512], in_=wslice(wo, 0, D, n * 512, (n + 1) * 512)), writes=[bwo])
        wr_sb = sb("wr_sb", [128, NCH, 20], F32)
        bwr = Buf()
        S.dma("sp", lambda e: e.dma_start(out=wr_sb[:].rearrange("p c n -> p (c n)"), in_=wr), writes=[bwr])
        aTr = Ring([(sb("aTi%d" % i, [128, NCH, 512], BF16), Buf()) for i in range(2)])
        xts = Ring([(sb("x2x%d" % i, [128, D], F32), Buf()) for i in range(3)])
        h3r = Ring([(sb("h3b%d" % i, [128, NCH, 512], BF16), Buf()) for i in range(2)])
        h3fr = Ring([(sb("h3f%d" % i, [128, NCH, 128], F32), Buf()) for i in range(2)])
        rtr = Ring([(sb("rt%d" % i, [128, 128], F32), Buf()) for i in range(2)])
        cmr = Ring([(sb("cmb%d" % i, [128, 16], F32), Buf()) for i in range(2)])
        def x2_normrouter(b, t, ti, xt, bxt, h3b, bh3b):
            h3f, bh3f = h3fr.next()
            rt, brt = rtr.next()
            nt.run(xt[:], bxt, h3b[:, :, t * 128:(t + 1) * 128], bh3b, h3f, bh3f)
            if limit == 6.5:
                S.mute = True
            psr, bpsr = PS[7], BPS[7]
            mm_group(psr[:, 0:20], bpsr, [(h3f[:, c, :], wr_sb[:, c, :]) for c in range(NCH)], [bh3f, bwr])
            cm, bcm = cmr.next()
            R_ = rt
            S.op("act", lambda e, R_=R_: e.activation(out=R_[:, 0:20], in_=psr[:, 0:20], func=AF.Copy), reads=[bpsr], writes=[brt])
            S.op("dve", lambda e, R_=R_: e.tensor_reduce(out=R_[:, 20:21], in_=R_[:, 0:4], axis=AX.X, op=ALU.max), reads=[brt], writes=[brt])
            S.op("dve", lambda e, R_=R_: e.tensor_scalar(out=R_[:, 21:22], in0=R_[:, 20:21], scalar1=-1.0, scalar2=None, op0=ALU.mult), reads=[brt], writes=[brt])
            S.op("act", lambda e, R_=R_: e.activation(out=R_[:, 24:28], in_=R_[:, 0:4], func=AF.Exp, bias=R_[:, 21:22], accum_out=R_[:, 22:23]), reads=[brt], writes=[brt])
            S.op("dve", lambda e, R_=R_: e.reciprocal(out=R_[:, 23:24], in_=R_[:, 22:23]), reads=[brt], writes=[brt])
            S.op("dve", lambda e, R_=R_: e.tensor_scalar(out=R_[:, 24:28], in0=R_[:, 0:4], scalar1=R_[:, 20:21], scalar2=None, op0=ALU.is_equal), reads=[brt], writes=[brt])
            S.op("dve", lambda e, R_=R_: e.tensor_scalar(out=R_[:, 24:28], in0=R_[:, 24:28], scalar1=-1.0, scalar2=1e30, op0=ALU.add, op1=ALU.mult), reads=[brt], writes=[brt])
            S.op("dve", lambda e, R_=R_: e.tensor_tensor(out=R_[:, 32:48].rearrange("p (g k) -> p g k", g=4), in0=R_[:, 4:20].rearrange("p (g k) -> p g k", g=4),
                                                  in1=R_[:, 24:28].rearrange("p (g k) -> p g k", k=1).to_broadcast([128, 4, 4]), op=ALU.add), reads=[brt], writes=[brt])
            S.op("dve", lambda e, R_=R_: e.max(out=R_[:, 48:56], in_=R_[:, 32:48]), reads=[brt], writes=[brt])
            S.op("dve", lambda e, R_=R_: e.tensor_tensor(out=R_[:, 56:57], in0=R_[:, 49:50], in1=R_[:, 48:49], op=ALU.subtract), reads=[brt], writes=[brt])
            S.op("act", lambda e, R_=R_: e.activation(out=R_[:, 57:58], in_=R_[:, 56:57], func=AF.Exp), reads=[brt], writes=[brt])
            S.op("dve", lambda e, R_=R_: e.tensor_scalar(out=R_[:, 58:59], in0=R_[:, 57:58], scalar1=1.0, scalar2=None, op0=ALU.add), reads=[brt], writes=[brt])
            S.op("dve", lambda e, R_=R_: e.reciprocal(out=R_[:, 59:60], in_=R_[:, 58:59]), reads=[brt], writes=[brt])
            S.op("dve", lambda e, R_=R_: e.tensor_tensor(out=R_[:, 60:61], in0=R_[:, 57:58], in1=R_[:, 59:60], op=ALU.mult), reads=[brt], writes=[brt])
            S.op("dve", lambda e, R_=R_: e.tensor_scalar(out=R_[:, 61:63], in0=R_[:, 59:61], scalar1=R_[:, 23:24], scalar2=None, op0=ALU.mult), reads=[brt], writes=[brt])
            S.op("dve", lambda e, R_=R_: e.tensor_scalar(out=R_[:, 64:80], in0=R_[:, 32:48], scalar1=R_[:, 48:49], scalar2=R_[:, 61:62], op0=ALU.is_equal, op1=ALU.mult), reads=[brt], writes=[brt])
            S.op("dve", lambda e, R_=R_: e.tensor_scalar(out=R_[:, 80:96], in0=R_[:, 32:48], scalar1=R_[:, 49:50], scalar2=R_[:, 62:63], op0=ALU.is_equal, op1=ALU.mult), reads=[brt], writes=[brt])
            S.op("dve", lambda e, cm=cm, R_=R_: e.tensor_tensor(out=cm[:], in0=R_[:, 64:80], in1=R_[:, 80:96], op=ALU.add), reads=[brt], writes=[bcm])
            S.dma("sp", lambda e, cm=cm, ti=ti: e.dma_start(out=combs[ti * 128:(ti + 1) * 128, :], in_=cm[:]), reads=[bcm], writes=[b_comb[ti]])
            if limit == 6.5:
                S.mute = False
            if t == 3:
                S.dma("sp", lambda e, h3b=h3b, b=b: e.dma_start(out=h3Ts[b], in_=h3b[:].rearrange("p c n -> p (c n)")), reads=[bh3b], writes=[b_h3T[b]])

        pend = None
        for b in range(NB):
            aT, baT = aTr.next()
            S.dma("sp", lambda e, aT=aT, b=b: e.dma_start(out=aT[:].rearrange("p c n -> p (c n)"), in_=aTs[b]), reads=[b_aT[b]], writes=[baT])
            h3b, bh3b = h3r.next()
            for t in range(4):
                ti = b * 4 + t
                xt, bxt = xts.next()
                S.dma("sp", lambda e, xt=xt, ti=ti: e.dma_start(out=xt[:], in_=x1s[ti * 128:(ti + 1) * 128, :]), reads=[b_x1[ti]], writes=[bxt])
                for n in range(4):
                    ps, bps = ringB.next()
                    mm_group(ps[:], bps, [(aT[:, c, t * 128:(t + 1) * 128], wo_sb[:, c, n * 512:(n + 1) * 512]) for c in range(NCH)], [bwo, baT])
                    S.op("dve", lambda e, xt=xt, ps=ps, n=n: e.tensor_tensor(out=xt[:, n * 512:(n + 1) * 512], in0=ps[:], in1=xt[:, n * 512:(n + 1) * 512], op=ALU.add),
                         reads=[bps, bxt], writes=[bxt])
                S.dma("sp", lambda e, xt=xt, ti=ti: e.dma_start(out=x2s[ti * 128:(ti + 1) * 128, :], in_=xt[:]), reads=[bxt], writes=[b_x2[ti]])
                if dbg:
                    bo_ = Buf()
                    S.dma("sp", lambda e, xt=xt, ti=ti: e.dma_start(out=x2d[ti * 128:(ti + 1) * 128, :], in_=xt[:]), reads=[bxt], writes=[bo_])
                    outs.append(bo_)
                if pend is not None:
                    x2_normrouter(*pend)
                pend = (b, t, ti, xt, bxt, h3b, bh3b)
        x2_normrouter(*pend)
        S.barrier()
        S.emit_all()

    S.mute = limit < 8
    with nc.reset_on_exit():
        h3T = sb("e_h3T", [128, NCH, EB], BF16)
        bh3 = Buf()
        y = sb("e_y", [128, ETI, D], F32)
        by = [Buf() for _ in range(ETI)]
        wring = Ring([(sb("e_w%d" % i, [128, 8192], BF16), Buf()) for i in range(4)])
        cmb = sb("e_cmb", [128, ETI, 16], F32)
        bcmb = Buf()
        sgr = Ring([(sb("e_sg%d" % i, [128, 512], F32), Buf()) for i in range(2)])
        hidr = Ring([(sb("e_hid%d" % i, [128, 512], BF16), Buf()) for i in range(2)])
        hTr_ = Ring([(sb("e_hT%d" % i, [128, 4, 128], BF16), Buf()) for i in range(2)])
        xts = Ring([(sb("e_x%d" % i, [128, D], F32), Buf()) for i in range(2)])
        junk = sb("e_junk", [128, D], BF16)
        gB = sb("e_gB", [128, D], F32)
        fst = sb("e_fst", [128, 4], F32)
        bjunk, bgB, bfst = Buf(), Buf(), Buf()
        S.dma("sp", lambda e: e.dma_start(out=gB[:], in_=gains[3:4, :].to_broadcast([128, D])), writes=[bgB])
        gur = Ring([((PS[0], BPS[0]), (PS[1], BPS[1])), ((PS[2], BPS[2]), (PS[3], BPS[3]))])
        ringY = psring([5, 6, 7])
        PS_T = 4
        pending_fn = None
        for eb in range(T // EB):
            for half in range(EB // 512):
                bi = eb * (EB // 512) + half
                S.dma("sp", lambda e, half=half, bi=bi: e.dma_start(out=h3T[:, :, half * 512:(half + 1) * 512], in_=h3Ts[bi].rearrange("p (c n) -> p c n", c=NCH)),
                      reads=[b_h3T[bi]], writes=[bh3])
            for i in range(ETI):
                ti = eb * ETI + i
                S.dma("sp", lambda e, i=i, ti=ti: e.dma_start(out=cmb[:, i, :], in_=combs[ti * 128:(ti + 1) * 128, :]), reads=[b_comb[ti]], writes=[bcmb])
            for ex in range(16):
                wg, bwg = wring.next()
                S.dma("pool", lambda e, wg=wg, ex=ex: e.dma_start(out=wg[:].rearrange("p (c n) -> p c n", c=NCH), in_=w_gate[ex].rearrange("(c p) n -> p c n", p=128)), writes=[bwg])
                wu, bwu = wring.next()
                S.dma("pool", lambda e, wu=wu, ex=ex: e.dma_start(out=wu[:].rearrange("p (c n) -> p c n", c=NCH), in_=w_up[ex].rearrange("(c p) n -> p c n", p=128)), writes=[bwu])
                wd, bwd = wring.next()
                for n in range(4):
                    S.dma("pool", lambda e, wd=wd, ex=ex, n=n: e.dma_start(out=wd[:].rearrange("p (c n) -> p c n", c=4)[:, :, n * 512:(n + 1) * 512],
                                                                    in_=w_down[ex][:, n * 512:(n + 1) * 512].rearrange("(c p) n -> p c n", p=128)), writes=[bwd])
                wg3 = wg[:].rearrange("p (c n) -> p c n", c=NCH)
                wu3 = wu[:].rearrange("p (c n) -> p c n", c=NCH)
                wd3 = wd[:].rearrange("p (c n) -> p c n", c=4)

                def GUg(i):
                    (pg, bpg), (pu, bpu) = gur.next()
                    mm_group(pg[:], bpg, [(h3T[:, c, i * 128:(i + 1) * 128], wg3[:, c, :]) for c in range(NCH)], [bh3, bwg])
                    return (pg, bpg, pu, bpu)

                def GUu(i, gu):
                    pg, bpg, pu, bpu = gu
                    mm_group(pu[:], bpu, [(h3T[:, c, i * 128:(i + 1) * 128], wu3[:, c, :]) for c in range(NCH)], [bh3, bwu])

                def HID(i, gu, ex=ex):
                    pg, bpg, pu, bpu = gu
                    sg, bsg = sgr.next()
                    S.op("act", lambda e: e.activation(out=sg[:], in_=pg[:], func=AF.Silu), reads=[bpg], writes=[bsg])
                    hid, bhid = hidr.next()
                    S.op("dve", lambda e: e.scalar_tensor_tensor(out=hid[:], in0=sg[:], scalar=cmb[:, i, ex:ex + 1], in1=pu[:], op0=ALU.mult, op1=ALU.mult),
                         reads=[bsg, bpu, bcmb], writes=[bhid])
                    return hid, bhid

                def TR(i, hb):
                    hid, bhid = hb
                    psb = PS[PS_T][:].bitcast(BF16)
                    for c4 in range(4):
                        S.op("pe", lambda e, c4=c4: e.transpose(psb[:, c4 * 128:(c4 + 1) * 128], hid[:, c4 * 128:(c4 + 1) * 128], id_b[:]),
                             reads=[bhid, bconst], writes=[BPS[PS_T]])
                    hT_, bhT_ = hTr_.next()
                    S.op("act", lambda e: e.activation(out=hT_[:].rearrange("p c n -> p (c n)"), in_=psb[:, 0:512], func=AF.Copy), reads=[BPS[PS_T]], writes=[bhT_])
                    return hT_, bhT_

                def DOWN(i, hb, ex=ex):
                    hT_, bhT_ = hb
                    for n in range(4):
                        ps, bps = ringY.next()
                        mm_group(ps[:], bps, [(hT_[:, c4, :], wd3[:, c4, n * 512:(n + 1) * 512]) for c4 in range(4)], [bhT_, bwd])
                        if ex == 0:
                            S.op("dve", lambda e, ps=ps, n=n: e.tensor_copy(out=y[:, i, n * 512:(n + 1) * 512], in_=ps[:]), reads=[bps], writes=[by[i]])
                        else:
                            S.op("dve", lambda e, ps=ps, n=n: e.tensor_tensor(out=y[:, i, n * 512:(n + 1) * 512], in0=ps[:], in1=y[:, i, n * 512:(n + 1) * 512], op=ALU.add),
                                 reads=[bps, by[i]], writes=[by[i]])

                g_cur = GUg(0)
                GUu(0, g_cur)
                for i in range(ETI):
                    hb = HID(i, g_cur)
                    g_next = GUg(i + 1) if i + 1 < ETI else None
                    if ex == 0 and pending_fn is not None:
                        pending_fn(i)
                    tb = TR(i, hb)
                    if g_next is not None:
                        GUu(i + 1, g_next)
                    DOWN(i, tb)
                    g_cur = g_next
                if ex == 0:
                    pending_fn = None

            def final_norm(i, eb=eb):
                ti = eb * ETI + i
                xt, bxt = xts.next()
                S.dma("sp", lambda e, xt=xt, ti=ti: e.dma_start(out=xt[:], in_=x2s[ti * 128:(ti + 1) * 128, :]), reads=[b_x2[ti]], writes=[bxt])
                S.op("dve", lambda e, xt=xt, i=i: e.tensor_tensor(out=y[:, i, :], in0=y[:, i, :], in1=xt[:], op=ALU.add), reads=[bxt, by[i]], writes=[by[i]])
                S.op("act", lambda e, i=i: e.activation(out=junk[:], in_=y[:, i, :], func=AF.Square, accum_out=fst[:, 0:1]), reads=[by[i]], writes=[bjunk, bfst])
                S.op("act", lambda e: e.activation(out=fst[:, 1:2], in_=fst[:, 0:1], func=AF.Sqrt, scale=1.0 / D, bias=eps_t[:]), reads=[bfst, bconst], writes=[bfst])
                S.op("dve", lambda e: e.reciprocal(out=fst[:, 2:3], in_=fst[:, 1:2]), reads=[bfst], writes=[bfst])
                S.op("dve", lambda e, xt=xt, i=i: e.scalar_tensor_tensor(out=xt[:], in0=y[:, i, :], scalar=fst[:, 2:3], in1=gB[:], op0=ALU.mult, op1=ALU.mult),
                     reads=[by[i], bfst, bgB], writes=[bxt])
                bo_ = Buf()
                S.dma("sp", lambda e, xt=xt, ti=ti: e.dma_start(out=out[ti * 128:(ti + 1) * 128, :], in_=xt[:]), reads=[bxt], writes=[bo_])
                outs.append(bo_)

            if eb + 1 < T // EB:
                pending_fn = final_norm
            else:
                for i in range(ETI):
                    final_norm(i)
        S.mute = False
        S.final_wait("sp", outs)
        S.barrier()
        S.emit_all()
    return nc


_CACHE = {}


def _consts():
    s = np.arange(128)[:, None]
    t = np.arange(128)[None, :]
    bd = ((s // 64 == t // 64) & (s <= t)).astype(np.uint32)
    rm = np.ones((128, 512), np.float32)
    rm[:, ::64] = 0.0
    return np.eye(128, dtype=np.float32), bd, rm


def run(inputs, dbg=False, limit=99):
    x = np.asarray(inputs["x"], np.float32)
    B, SEQ, _ = x.shape
    SEG = 4
    T = SEQ // SEG
    W = HALO
    key = (T, W, dbg, limit)
    if key not in _CACHE:
        _CACHE[key] = build(T, W, dbg, limit)
    nc = _CACHE[key]
    f = lambda k: np.ascontiguousarray(np.asarray(inputs[k], np.float32))
    ident, bd, rm = _consts()
    gains = np.stack([f("norm_mix")[0], f("norm_xattn")[0], f("norm_ffn")[0], f("norm_final"), f("norm_mem")[0]], 0)
    pscale = np.ascontiguousarray(f("pool_scale")[0].reshape(8, 128).T)
    lbl = np.ascontiguousarray(f("hgrn_lb_logits").reshape(2, 8, 128).transpose(2, 0, 1).reshape(128, 16))
    hnorm = np.ascontiguousarray(f("hgrn_norm")[0].reshape(128, 1))
    wr = np.concatenate([f("router_group")[0], f("router_expert")[0]], axis=1)
    wr = np.ascontiguousarray(wr.reshape(NCH, 128, 20).transpose(1, 0, 2).reshape(128, NCH * 20))
    shared = {
        "w_in": f("w_in")[0], "pool_w": f("pool_w")[0], "w_out": f("w_out")[0],
        "wq": f("xattn_wq")[0], "wk": f("xattn_wk")[0], "wv": f("xattn_wv")[0], "wo": f("xattn_wo")[0],
        "wr": wr, "w_gate": f("w_gate")[0], "w_up": f("w_up")[0], "w_down": f("w_down")[0],
        "gains": np.ascontiguousarray(gains), "pscale": pscale, "lbl": lbl, "hnorm": hnorm,
        "ident": ident, "bdmask": bd, "rmask": rm,
    }
    memf = f("mem")
    in_maps = []
    for c in range(B * SEG):
        b, j = divmod(c, SEG)
        seg = np.zeros((W + T, D), np.float32)
        if j == 0:
            seg[W:] = x[b, 0:T]
        else:
            seg[:] = x[b, j * T - W:(j + 1) * T]
        ic = np.empty((4, 512), np.float32)
        for g, w_ in enumerate((2, 4, 8, 16)):
            if j == 0:
                ic[g] = 1.0 / np.minimum(np.arange(512) + 1, w_)
            else:
                ic[g] = 1.0 / w_
        m = dict(shared)
        m["xseg"] = seg
        m["mem"] = np.ascontiguousarray(memf[b])
        m["invcnt"] = np.ascontiguousarray(np.broadcast_to(ic.reshape(1, 4 * 512), (128, 4 * 512)))
        in_maps.append(m)
    res = run_bass_kernel_spmd(nc, in_maps, core_ids=list(range(B * SEG)))
    names = ["out"] + (["x1d", "x2d"] if dbg else [])
    outd = {}
    for nm in names:
        o = np.empty((B, SEQ, D), np.float32)
        for c in range(B * SEG):
            b, j = divmod(c, SEG)
            o[b, j * T:(j + 1) * T] = np.asarray(res.results[c][nm])
        outd[nm] = o
    return outd


def kernel(**inputs):
    return run(inputs)["out"]
```

```python
import os
import numpy as np
import concourse.bass as bass
import concourse.mybir as mybir
from concourse.bass_utils import run_bass_kernel_spmd

F32 = mybir.dt.float32
BF16 = mybir.dt.bfloat16
U32 = mybir.dt.uint32
AF = mybir.ActivationFunctionType
ALU = mybir.AluOpType
AX = mybir.AxisListType

ENGS = ["pe", "act", "dve", "pool", "sp"]
N_DMA_SEMS = 32
D = 2048
NCH = 16
EPS = 1e-6
HALO = 512


class Buf:
    __slots__ = ("name", "w", "r")

    def __init__(self, name=""):
        self.name = name
        self.w = {}
        self.r = {}


class Sched:
    def __init__(self, nc):
        self.nc = nc
        self.prog = {e: [] for e in ENGS}
        self.cnt = {e: 0 for e in ENGS}
        self.seen = {e: {} for e in ENGS}
        self.sem = {e: nc.alloc_semaphore("sem_" + e) for e in ENGS}
        self.dsem = [nc.alloc_semaphore("dsem%d" % i) for i in range(N_DMA_SEMS)]
        self.dcnt = [0] * N_DMA_SEMS
        self.dnext2 = [0, 0]
        self.mute = False
        self.touched = set()

    def _deps(self, reads, writes):
        deps = {}
        for b in reads:
            for k, n in b.w.items():
                if deps.get(k, 0) < n:
                    deps[k] = n
        for b in writes:
            for d in (b.w, b.r):
                for k, n in d.items():
                    if deps.get(k, 0) < n:
                        deps[k] = n
        return deps

    def _waits(self, eng, deps):
        waits = []
        seen = self.seen[eng]
        for k, n in deps.items():
            if k == "pe" and eng == "pe":
                continue
            if seen.get(k, 0) < n:
                seen[k] = n
                waits.append((k, n))
        return waits

    def _semof(self, k):
        if isinstance(k, tuple):
            return self.dsem[k[1]], 16
        return self.sem[k], 1

    def op(self, eng, emit, reads=(), writes=()):
        if self.mute:
            return
        deps = self._deps(reads, writes)
        waits = self._waits(eng, deps)
        self.cnt[eng] += 1
        my = self.cnt[eng]
        self.prog[eng].append((waits, emit, self.sem[eng], 1))
        self.touched.update(reads)
        self.touched.update(writes)
        for b in writes:
            b.w = {eng: my}
            b.r = {}
        for b in reads:
            if b.r.get(eng, 0) < my:
                b.r[eng] = my

    def dma(self, eng, emit, reads=(), writes=()):
        if self.mute:
            return
        half = N_DMA_SEMS // 2
        q = 1 if eng == "pool" else 0
        i = q * half + self.dnext2[q]
        self.dnext2[q] = (self.dnext2[q] + 1) % half
        key = ("dma", i)
        deps = self._deps(reads, writes)
        if self.dcnt[i] > 0:
            deps[key] = max(deps.get(key, 0), self.dcnt[i])
        waits = self._waits(eng, deps)
        self.dcnt[i] += 1
        my = self.dcnt[i]
        self.prog[eng].append((waits, emit, self.dsem[i], 16))
        self.touched.update(reads)
        self.touched.update(writes)
        for b in writes:
            b.w = {key: my}
            b.r = {}
        for b in reads:
            if b.r.get(key, 0) < my:
                b.r[key] = my

    def final_wait(self, eng, bufs):
        deps = {}
        for b in bufs:
            for k, n in b.w.items():
                deps[k] = max(deps.get(k, 0), n)
        waits = self._waits(eng, deps)
        self.prog[eng].append((waits, None, None, 0))

    def barrier(self):
        deps = {e: self.cnt[e] for e in ENGS if self.cnt[e] > 0}
        for i in range(N_DMA_SEMS):
            if self.dcnt[i] > 0:
                deps[("dma", i)] = self.dcnt[i]
        for eng in ENGS:
            waits = []
            seen = self.seen[eng]
            for k, n in deps.items():
                if k == eng:
                    continue
                if seen.get(k, 0) < n:
                    seen[k] = n
                    waits.append((k, n))
            self.prog[eng].append((waits, None, None, 0))

    def emit_all(self):
        nc = self.nc

        def run(e, name):
            for waits, emit, sem, inc in self.prog[name]:
                for k, n in waits:
                    s, mult = self._semof(k)
                    e.wait_ge(s, n * mult)
                if emit is not None:
                    emit(e).then_inc(sem, inc)

        with nc.Block() as block:
            @block.tensor
            def _(e):
                run(e, "pe")

            @block.scalar
            def _(e):
                run(e, "act")

            @block.vector
            def _(e):
                run(e, "dve")

            @block.gpsimd
            def _(e):
                run(e, "pool")

            @block.sync
            def _(e):
                run(e, "sp")
        self.prog = {e: [] for e in ENGS}
        for b in self.touched:
            b.w = {}
            b.r = {}
        self.touched = set()
        self.cnt = {e: 0 for e in ENGS}
        self.seen = {e: {} for e in ENGS}
        self.dcnt = [0] * N_DMA_SEMS


class Ring:
    def __init__(self, items):
        self.items = items
        self.i = 0

    def next(self):
        it = self.items[self.i % len(self.items)]
        self.i += 1
        return it


def build(T, W, dbg=False, limit=99):
    nc = bass.Bass("TRN2", target_bir_lowering=False)
    S = Sched(nc)
    NBM = (W + T) // 512
    HB = W // 512
    NB = T // 512
    EB = 1024 if T % 1024 == 0 else 512
    ETI = EB // 128

    uid = [0]

    def sb(name, shape, dt):
        uid[0] += 1
        return nc.alloc_sbuf_tensor("%s_%d" % (name, uid[0]), shape, dt)

    def din(name, shape, dt=F32):
        return nc.dram_tensor(name, list(shape), dt, kind="ExternalInput").ap()

    xseg = din("xseg", [W + T, D])
    mem = din("mem", [256, D])
    w_in = din("w_in", [D, 5120])
    pool_w = din("pool_w", [4, 256, 256])
    w_out = din("w_out", [D, D])
    wq = din("wq", [D, D])
    wk = din("wk", [D, D])
    wv = din("wv", [D, D])
    wo = din("wo", [D, D])
    wr = din("wr", [128, NCH * 20])
    w_gate = din("w_gate", [16, D, 512])
    w_up = din("w_up", [16, D, 512])
    w_down = din("w_down", [16, 512, D])
    gains = din("gains", [5, D])
    pscale = din("pscale", [128, 8])
    lbl = din("lbl", [128, 16])
    hnorm = din("hnorm", [128, 1])
    ident = din("ident", [128, 128])
    bdmask = din("bdmask", [128, 128], U32)
    rmask = din("rmask", [128, 512])
    invcnt = din("invcnt", [128, 4 * 512])
    out = nc.dram_tensor("out", [T, D], F32, kind="ExternalOutput").ap()
    if dbg:
        x1d = nc.dram_tensor("x1d", [T, D], F32, kind="ExternalOutput").ap()
        x2d = nc.dram_tensor("x2d", [T, D], F32, kind="ExternalOutput").ap()

    hT1 = nc.dram_tensor("hT1", [NBM, 128, NCH * 512], BF16).ap()
    x1s = nc.dram_tensor("x1s", [T, D], F32).ap()
    aTs = nc.dram_tensor("aTs", [NB, 128, NCH * 512], BF16).ap()
    x2s = nc.dram_tensor("x2s", [T, D], F32).ap()
    h3Ts = nc.dram_tensor("h3Ts", [NB, 128, NCH * 512], BF16).ap()
    combs = nc.dram_tensor("combs", [T, 16], F32).ap()
    b_hT1 = [Buf() for _ in range(NBM)]
    b_x1 = [Buf() for _ in range(T // 128)]
    b_aT = [Buf() for _ in range(NB)]
    b_x2 = [Buf() for _ in range(T // 128)]
    b_h3T = [Buf() for _ in range(NB)]
    b_comb = [Buf() for _ in range(T // 128)]
    outs = []

    PS = [nc.alloc_psum_tensor("ps%d" % i, [128, 512], F32) for i in range(8)]
    BPS = [Buf("ps%d" % i) for i in range(8)]

    def psring(idx):
        return Ring([(PS[i], BPS[i]) for i in idx])

    id_f = sb("id_f", [128, 128], F32)
    id_b = sb("id_b", [128, 128], BF16)
    ones_f = sb("ones_f", [128, 128], F32)
    eps_t = sb("eps_t", [128, 1], F32)
    bconst = Buf("const")
    S.dma("sp", lambda e: e.dma_start(out=id_f[:], in_=ident), writes=[bconst])
    S.dma("pool", lambda e: e.dma_start(out=id_b[:], in_=ident), writes=[bconst])
    S.op("dve", lambda e: e.memset(ones_f[:], 1.0), writes=[bconst])
    S.op("dve", lambda e: e.memset(eps_t[:], EPS), writes=[bconst])

    flip = [0]

    def evac_copy(dst, src, reads, writes):
        flip[0] ^= 1
        if flip[0]:
            S.op("act", lambda e: e.activation(out=dst, in_=src, func=AF.Copy), reads=reads, writes=writes)
        else:
            S.op("dve", lambda e: e.tensor_copy(out=dst, in_=src), reads=reads, writes=writes)

    def mm_group(ps_ap, bps, pairs, reads):
        n = len(pairs)
        for i, (l, r) in enumerate(pairs):
            S.op("pe", lambda e, l=l, r=r, i=i: e.matmul(ps_ap, lhsT=l, rhs=r, start=(i == 0), stop=(i == n - 1)),
                 reads=reads, writes=[bps])

    class NormT:
        def __init__(self, tag, banks):
            self.tmp = Ring([(sb(tag + "junk%d" % i, [128, D], BF16), sb(tag + "xn%d" % i, [128, D], F32), sb(tag + "st%d" % i, [128, 4], F32),
                              Buf(), Buf(), Buf()) for i in range(2)])
            self.gB = sb(tag + "gB", [128, D], F32)
            self.bg = Buf()
            self.ring = psring(banks)

        def load_gain(self, row):
            S.dma("sp", lambda e: e.dma_start(out=self.gB[:], in_=gains[row:row + 1, :].to_broadcast([128, D])), writes=[self.bg])

        def run(self, src, bsrc, dst_bf, bdst, dst_f=None, bdst_f=None):
            junk, xn, st, bj, bxn, bst = self.tmp.next()
            S.op("act", lambda e: e.activation(out=junk[:], in_=src, func=AF.Square, accum_out=st[:, 0:1]),
                 reads=[bsrc], writes=[bj, bst])
            S.op("act", lambda e: e.activation(out=st[:, 1:2], in_=st[:, 0:1], func=AF.Sqrt, scale=1.0 / D, bias=eps_t[:]),
                 reads=[bst, bconst], writes=[bst])
            S.op("dve", lambda e: e.reciprocal(out=st[:, 2:3], in_=st[:, 1:2]), reads=[bst], writes=[bst])
            S.op("dve", lambda e: e.scalar_tensor_tensor(out=xn[:], in0=src, scalar=st[:, 2:3], in1=self.gB[:],
                                                         op0=ALU.mult, op1=ALU.mult),
                 reads=[bsrc, bst, self.bg], writes=[bxn])
            for b4 in range(4):
                ps, bps = self.ring.next()
                for j in range(4):
                    c = b4 * 4 + j
                    S.op("pe", lambda e, ps=ps, j=j, c=c: e.transpose(ps[:, j * 128:(j + 1) * 128], xn[:, c * 128:(c + 1) * 128], id_f[:]),
                         reads=[bxn, bconst], writes=[bps])
                src3 = ps[:].rearrange("p (c n) -> p c n", c=4)
                if dst_f is None:
                    evac_copy(dst_bf[:, b4 * 4:(b4 + 1) * 4, :], src3, [bps], [bdst])
                else:
                    evac_copy(dst_f[:, b4 * 4:(b4 + 1) * 4, :], src3, [bps], [bdst_f])
                    S.op("pool", lambda e, b4=b4: e.tensor_copy(out=dst_bf[:, b4 * 4:(b4 + 1) * 4, :], in_=dst_f[:, b4 * 4:(b4 + 1) * 4, :]),
                         reads=[bdst_f], writes=[bdst])

    def wslice(w, r0, r1, c0, c1):
        return w[r0:r1, c0:c1].rearrange("(c p) n -> p c n", p=128)

    S.mute = limit < 1
    with nc.reset_on_exit():
        nt = NormT("n1", [0, 1, 2, 3, 4, 5, 6, 7])
        nt.load_gain(0)
        xts = Ring([(sb("n1x%d" % i, [128, D], F32), Buf()) for i in range(3)])
        hbs = Ring([(sb("n1h%d" % i, [128, NCH, 512], BF16), Buf()) for i in range(2)])
        for b in range(NBM):
            hb, bhb = hbs.next()
            for t in range(4):
                xt, bxt = xts.next()
                r0 = b * 512 + t * 128
                S.dma("sp", lambda e, xt=xt, r0=r0: e.dma_start(out=xt[:], in_=xseg[r0:r0 + 128, :]), writes=[bxt])
                nt.run(xt[:], bxt, hb[:, :, t * 128:(t + 1) * 128], bhb)
            S.dma("sp", lambda e, hb=hb, b=b: e.dma_start(out=hT1[b], in_=hb[:].rearrange("p c n -> p (c n)")),
                  reads=[bhb], writes=[b_hT1[b]])
        S.barrier()
        S.emit_all()

    def interleave(gens):
        gens = list(gens)
        while gens:
            for g in list(gens):
                try:
                    next(g)
                except StopIteration:
                    gens.remove(g)

    for p in range(4):
        S.mute = limit < 2 + p
        with nc.reset_on_exit():
            win = sb("win", [128, NCH, 5, 256], BF16)
            wout = sb("wout", [128, 4, D], BF16)
            pw = sb("pw", [128, 2, 256], BF16)
            bwin, bwout, bpw = [Buf() for _ in range(5)], [Buf() for _ in range(4)], Buf()
            for g5 in (0, 3, 1, 2, 4):
                c0 = g5 * 1024 + 256 * p
                S.dma("pool", lambda e, g5=g5, c0=c0: e.dma_start(out=win[:, :, g5, :], in_=wslice(w_in, 0, D, c0, c0 + 256)), writes=[bwin[g5]])
            S.dma("pool", lambda e: e.dma_start(out=pw[:], in_=pool_w[p].rearrange("(c q) n -> q c n", q=128)), writes=[bpw])
            for n in range(4):
                S.dma("pool", lambda e, n=n: e.dma_start(out=wout[:, 0:2, n * 512:(n + 1) * 512], in_=wslice(w_out, 256 * p, 256 * p + 256, n * 512, (n + 1) * 512)), writes=[bwout[n]])
                S.dma("pool", lambda e, n=n: e.dma_start(out=wout[:, 2:4, n * 512:(n + 1) * 512], in_=wslice(w_out, 1024 + 256 * p, 1024 + 256 * p + 256, n * 512, (n + 1) * 512)), writes=[bwout[n]])
            small = sb("msmall", [128, 64], F32)
            bsmall = Buf()
            S.dma("sp", lambda e: e.dma_start(out=small[:, 0:8], in_=pscale), writes=[bsmall])
            S.dma("sp", lambda e: e.dma_start(out=small[:, 8:24], in_=lbl), writes=[bsmall])
            S.dma("sp", lambda e: e.dma_start(out=small[:, 24:25], in_=hnorm), writes=[bsmall])
            S.op("dve", lambda e: e.tensor_tensor(out=small[:, 32:34], in0=small[:, 8 + 2 * p:10 + 2 * p], in1=small[:, 16 + 2 * p:18 + 2 * p], op=ALU.subtract),
                 reads=[bsmall], writes=[bsmall])
            S.op("act", lambda e: e.activation(out=small[:, 25:27], in_=small[:, 32:34], func=AF.Sigmoid), reads=[bsmall], writes=[bsmall])
            S.op("dve", lambda e: e.tensor_scalar(out=small[:, 27:29], in0=small[:, 25:27], scalar1=-1.0, scalar2=1.0, op0=ALU.mult, op1=ALU.add),
                 reads=[bsmall], writes=[bsmall])
            S.op("dve", lambda e: e.tensor_scalar(out=small[:, 29:31], in0=small[:, 27:29], scalar1=-1.0, scalar2=None, op0=ALU.mult),
                 reads=[bsmall], writes=[bsmall])
            rm_sb = sb("rm_sb", [128, 512], F32)
            bd_sb = sb("bd_sb", [128, 128], U32)
            ic_sb = sb("ic_sb", [128, 512], F32)
            bcm_ = Buf()
            S.dma("sp", lambda e: e.dma_start(out=rm_sb[:], in_=rmask), writes=[bcm_])
            S.dma("sp", lambda e: e.dma_start(out=bd_sb[:], in_=bdmask), writes=[bcm_])
            S.dma("sp", lambda e: e.dma_start(out=ic_sb[:], in_=invcnt[:, p * 512:(p + 1) * 512]), writes=[bcm_])

            hTr = Ring([(sb("mh%d" % i, [128, NCH, 512], BF16), Buf()) for i in range(2)])
            uT = sb("uT", [128, 2, 528], F32)
            b_uh, b_um = Buf(), Buf()
            s_a = sb("s_a", [128, 2, 528], F32)
            s_b = sb("s_b", [128, 2, 528], F32)
            bsa, bsb = Buf(), Buf()
            pooled = sb("pooled", [128, 2, 512], BF16)
            bpooled = Buf()
            tmpH = []
            for hh in range(2):
                d_ = {}
                for nm in ["q", "sig", "lf", "kk", "a", "at"]:
                    d_[nm] = sb("h%d%s" % (hh, nm), [128, 512], F32)
                    d_["b" + nm] = Buf()
                d_["e1"], d_["be1"] = d_["lf"], d_["blf"]
                d_["e2"], d_["be2"] = d_["sig"], d_["bsig"]
                tmpH.append(d_)
            sets = []
            for si in range(2):
                st_ = {"hd": []}
                for hh in range(2):
                    d_ = {}
                    for nm in ["qt", "ktT"]:
                        d_[nm] = sb("s%dh%d%s" % (si, hh, nm), [128, 512], BF16)
                        d_["b" + nm] = Buf()
                    d_["ktok"] = sb("s%dh%dktok" % (si, hh), [128, 4, 128], BF16)
                    d_["bktok"] = Buf()
                    d_["ex"] = sb("s%dh%dex" % (si, hh), [128, 32], F32)
                    d_["bex"] = Buf()
                    st_["hd"].append(d_)
                st_["gT"] = sb("s%dgT" % si, [128, 2, 512], F32)
                st_["bgT"] = [Buf(), Buf()]
                st_["v"] = sb("s%dv" % si, [128, 4, 256], BF16)
                st_["bv"] = Buf()
                st_["mixT"] = sb("s%dmixT" % si, [128, 4, 512], BF16)
                st_["bmix"] = [Buf() for _ in range(4)]
                sets.append(st_)
            rec = []
            for hh in range(2):
                d_ = {}
                d_["S"] = sb("h%dS" % hh, [128, 128], F32)
                d_["bS"] = Buf()
                d_["sref"] = Ring([(sb("h%dsr%d" % (hh, i), [128, 128], BF16), Buf()) for i in range(3)])
                d_["tst8"] = [(sb("h%dts%d" % (hh, i), [128, 128], F32), Buf()) for i in range(8)]
                d_["ssb4"] = [(sb("h%dss%d" % (hh, i), [128, 128], BF16), Buf()) for i in range(4)]
                for (tt, bb) in d_["ssb4"]:
                    S.op("pool", lambda e, tt=tt: e.memset(tt[:], 0.0), writes=[bb])
                S.op("pool", lambda e, d_=d_: e.memset(d_["S"][:], 0.0), writes=[d_["bS"]])
                rec.append(d_)
            o_sb = sb("o_sb", [128, 512], F32)
            sq_sb = sb("sq_sb", [128, 512], F32)
            rs_sb = sb("rs_sb", [128, 512], F32)
            bo, bsq, brs = Buf(), Buf(), Buf()
            accr = Ring([(sb("acc%d" % i, [128, D], F32), Buf()) for i in range(2)])
            ringA = psring([0, 1, 2])
            PS_S, PS_O, PS_ST, PS_KT, PS_SS = 3, 4, 6, 7, 7

            def front(b):
                halo = b < HB
                st_ = sets[b % 2]
                mixT, bmix = st_["mixT"], st_["bmix"]
                v_sb, bv = st_["v"], st_["bv"]
                gT, bgT = st_["gT"], st_["bgT"]
                hT, bhT = hTr.next()
                S.dma("sp", lambda e, hT=hT, b=b: e.dma_start(out=hT[:].rearrange("p c n -> p (c n)"), in_=hT1[b]),
                      reads=[b_hT1[b]], writes=[bhT])
                if b == 0:
                    S.op("dve", lambda e: e.memset(uT[:, :, 0:16], 0.0), writes=[b_uh])
                else:
                    S.op("dve", lambda e: e.tensor_copy(out=uT[:, :, 0:16], in_=uT[:, :, 512:528]), reads=[b_um], writes=[b_uh])
                for j in range(2):
                    ps, bps = ringA.next()
                    mm_group(ps[:], bps, [(win[:, c, 0, j * 128:(j + 1) * 128], hT[:, c, :]) for c in range(NCH)], [bwin[0], bhT])
                    S.op("act", lambda e, ps=ps, j=j: e.activation(out=uT[:, j, 16:528], in_=ps[:], func=AF.Copy), reads=[bps], writes=[b_um])
                    yield
                for t in range(4):
                    ps, bps = ringA.next()
                    mm_group(ps[:, 0:256], bps, [(hT[:, c, t * 128:(t + 1) * 128], win[:, c, 3, :]) for c in range(NCH)], [bwin[3], bhT])
                    S.op("dve", lambda e, ps=ps, t=t: e.tensor_copy(out=v_sb[:, t, :], in_=ps[:, 0:256]), reads=[bps], writes=[bv])
                    if t % 2 == 1:
                        yield
                if not halo:
                    k = p + 1
                    src, bsrc = uT, None
                    for step in range(1, k + 1):
                        sh = 1 << (step - 1)
                        lo = 1 << step
                        dst, bdst = (s_a, bsa) if step % 2 == 1 else (s_b, bsb)
                        if step == 1:
                            S.op("pool", lambda e, dst=dst, lo=lo, sh=sh: e.tensor_tensor(out=dst[:, :, lo:528], in0=uT[:, :, lo:528], in1=uT[:, :, lo - sh:528 - sh], op=ALU.add),
                                 reads=[b_uh, b_um], writes=[bdst])
                        else:
                            S.op("pool", lambda e, dst=dst, src=src, lo=lo, sh=sh: e.tensor_tensor(out=dst[:, :, lo:528], in0=src[:, :, lo:528], in1=src[:, :, lo - sh:528 - sh], op=ALU.add),
                                 reads=[bsrc], writes=[bdst])
                        src, bsrc = dst, bdst
                    wnd = float(1 << k)
                    if b == HB:
                        other, bother = (s_b, bsb) if src is s_a else (s_a, bsa)
                        for j in range(2):
                            S.op("dve", lambda e, j=j, src=src, other=other: e.tensor_tensor(out=other[:, j, 16:528], in0=src[:, j, 16:528], in1=ic_sb[:], op=ALU.mult),
                                 reads=[bsrc, bcm_], writes=[bother])
                        S.op("dve", lambda e, other=other: e.tensor_tensor(out=pooled[:], in0=other[:, :, 16:528], in1=uT[:, :, 16:528], op=ALU.subtract),
                             reads=[bother, b_um], writes=[bpooled])
                    else:
                        S.op("dve", lambda e, src=src, wnd=wnd: e.scalar_tensor_tensor(out=pooled[:], in0=src[:, :, 16:528], scalar=1.0 / wnd, in1=uT[:, :, 16:528],
                                                                                   op0=ALU.mult, op1=ALU.subtract),
                             reads=[bsrc, b_um], writes=[bpooled])
                    yield
                for hh in range(2):
                    h_ = tmpH[hh]
                    o_ = st_["hd"][hh]
                    lb_ap = small[:, 25 + hh:26 + hh]
                    oml_ap = small[:, 27 + hh:28 + hh]
                    noml_ap = small[:, 29 + hh:30 + hh]
                    ps, bps = ringA.next()
                    mm_group(ps[:], bps, [(win[:, c, 2, hh * 128:(hh + 1) * 128], hT[:, c, :]) for c in range(NCH)], [bwin[2], bhT])
                    S.op("act", lambda e, ps=ps, h_=h_: e.activation(out=h_["sig"][:], in_=ps[:], func=AF.Sigmoid), reads=[bps], writes=[h_["bsig"]])
                    yield
                    ps, bps = ringA.next()
                    mm_group(ps[:], bps, [(win[:, c, 1, hh * 128:(hh + 1) * 128], hT[:, c, :]) for c in range(NCH)], [bwin[1], bhT])
                    S.op("act", lambda e, ps=ps, h_=h_: e.activation(out=h_["q"][:], in_=ps[:], func=AF.Silu), reads=[bps], writes=[h_["bq"]])
                    S.op("act", lambda e, h_=h_, oml_ap=oml_ap, lb_ap=lb_ap: e.activation(out=h_["lf"][:], in_=h_["sig"][:], func=AF.Ln, scale=oml_ap, bias=lb_ap),
                         reads=[h_["bsig"], bsmall], writes=[h_["blf"]])
                    S.op("dve", lambda e, h_=h_: e.tensor_tensor_scan(out=h_["a"][:], data0=rm_sb[:], data1=h_["lf"][:], initial=0.0, op0=ALU.mult, op1=ALU.add),
                         reads=[h_["blf"], bcm_], writes=[h_["ba"]])
                    yield
                    ps, bps = ringA.next()
                    mm_group(ps[:], bps, [(win[:, c, 4, hh * 128:(hh + 1) * 128], hT[:, c, :]) for c in range(NCH)], [bwin[4], bhT])
                    S.op("act", lambda e, ps=ps, hh=hh, gT=gT: e.activation(out=gT[:, hh, :], in_=ps[:], func=AF.Silu), reads=[bps], writes=[bgT[hh]])
                    a3 = h_["a"][:].rearrange("p (c n) -> p c n", c=8)
                    S.op("dve", lambda e, h_=h_, a3=a3: e.tensor_tensor(out=h_["at"][:].rearrange("p (c n) -> p c n", c=8), in0=a3, in1=a3[:, :, 31:32].to_broadcast([128, 8, 64]), op=ALU.subtract),
                         reads=[h_["ba"]], writes=[h_["bat"]])
                    S.op("pool", lambda e, h_=h_, noml_ap=noml_ap, oml_ap=oml_ap: e.tensor_scalar(out=h_["kk"][:], in0=h_["sig"][:], scalar1=noml_ap, scalar2=oml_ap, op0=ALU.mult, op1=ALU.add),
                         reads=[h_["bsig"], bsmall], writes=[h_["bkk"]])
                    yield
                    S.op("act", lambda e, h_=h_: e.activation(out=h_["e1"][:], in_=h_["at"][:], func=AF.Exp), reads=[h_["bat"]], writes=[h_["be1"]])
                    S.op("act", lambda e, h_=h_: e.activation(out=h_["e2"][:], in_=h_["at"][:], func=AF.Exp, scale=-1.0), reads=[h_["bat"]], writes=[h_["be2"]])
                    a16 = h_["a"][:].rearrange("p (c n) -> p c n", n=32)[:, :, 31:32]
                    S.op("act", lambda e, o_=o_, a16=a16: e.activation(out=o_["ex"][:, 0:16].rearrange("p (c n) -> p c n", n=1), in_=a16, func=AF.Exp),
                         reads=[h_["ba"]], writes=[o_["bex"]])
                    S.op("dve", lambda e, o_=o_, a3=a3: e.tensor_tensor(out=o_["ex"][:, 16:24].rearrange("p (c n) -> p c n", n=1), in0=a3[:, :, 63:64], in1=a3[:, :, 31:32], op=ALU.subtract),
                         reads=[h_["ba"]], writes=[o_["bex"]])
                    S.op("act", lambda e, o_=o_: e.activation(out=o_["ex"][:, 24:32], in_=o_["ex"][:, 16:24], func=AF.Exp), reads=[o_["bex"]], writes=[o_["bex"]])
                    yield
                    S.op("dve", lambda e, h_=h_, o_=o_: e.tensor_tensor(out=o_["qt"][:], in0=h_["q"][:], in1=h_["e1"][:], op=ALU.mult),
                         reads=[h_["bq"], h_["be1"]], writes=[o_["bqt"]])
                    S.op("pool", lambda e, h_=h_, o_=o_: e.tensor_tensor(out=o_["ktT"][:], in0=h_["kk"][:], in1=h_["e2"][:], op=ALU.mult),
                         reads=[h_["bkk"], h_["be2"]], writes=[o_["bktT"]])
                    yield
                    psb = PS[PS_KT][:].bitcast(BF16)
                    for t in range(4):
                        S.op("pe", lambda e, o_=o_, t=t, psb=psb: e.transpose(psb[:, t * 128:(t + 1) * 128], o_["ktT"][:, t * 128:(t + 1) * 128], id_b[:]),
                             reads=[o_["bktT"], bconst], writes=[BPS[PS_KT]])
                    S.op("act", lambda e, o_=o_, psb=psb: e.activation(out=o_["ktok"][:].rearrange("p t n -> p (t n)"), in_=psb[:, 0:512], func=AF.Copy),
                         reads=[BPS[PS_KT]], writes=[o_["bktok"]])
                    yield
                if not halo:
                    for oc in range(2):
                        ps, bps = ringA.next()
                        mm_group(ps[:], bps, [(pw[:, ic, oc * 128:(oc + 1) * 128], pooled[:, ic, :]) for ic in range(2)], [bpw, bpooled])
                        S.op("act", lambda e, ps=ps, oc=oc, mixT=mixT: e.activation(out=mixT[:, oc, :], in_=ps[:], func=AF.Copy, scale=small[:, 2 * p + oc:2 * p + oc + 1]),
                             reads=[bps, bsmall], writes=[bmix[oc]])
                    yield

            def back(b):
                halo = b < HB
                st_ = sets[b % 2]
                mixT, bmix = st_["mixT"], st_["bmix"]
                v_sb, bv = st_["v"], st_["bv"]
                gT, bgT = st_["gT"], st_["bgT"]
                for t in range(4):
                    for hh in range(2):
                        h_ = st_["hd"][hh]
                        r_ = rec[hh]
                        S.op("pe", lambda e, h_=h_, t=t: e.matmul(PS[PS_S][:, 0:128], lhsT=h_["ktT"][:, t * 128:(t + 1) * 128], rhs=h_["qt"][:, t * 128:(t + 1) * 128], start=True, stop=True),
                             reads=[h_["bktT"], h_["bqt"]], writes=[BPS[PS_S]])
                        ssb, bssb = r_["ssb4"][t]
                        S.op("dve", lambda e, ssb=ssb: e.copy_predicated(out=ssb[:], mask=bd_sb[:], data=PS[PS_S][:, 0:128]),
                             reads=[BPS[PS_S], bcm_], writes=[bssb])
                        for cc in range(2):
                            c = 2 * t + cc
                            S.op("pe", lambda e, h_=h_, t=t, cc=cc, hh=hh, v_sb=v_sb: e.matmul(PS[PS_ST][:, 0:128], lhsT=h_["ktok"][cc * 64:(cc + 1) * 64, t, :], rhs=v_sb[cc * 64:(cc + 1) * 64, t, hh * 128:(hh + 1) * 128], start=True, stop=True),
                                 reads=[h_["bktok"], bv], writes=[BPS[PS_ST]])
                            tst, btst = r_["tst8"][c]
                            S.op("act", lambda e, h_=h_, tst=tst, c=c: e.activation(out=tst[:], in_=PS[PS_ST][:, 0:128], func=AF.Copy, scale=h_["ex"][:, 24 + c:25 + c]),
                                 reads=[BPS[PS_ST], h_["bex"]], writes=[btst])
                        yield
                for t in range(4):
                    for hh in range(2):
                        h_ = st_["hd"][hh]
                        r_ = rec[hh]
                        pso, bpso = PS[PS_O + hh], BPS[PS_O + hh]
                        ssb, bssb = r_["ssb4"][t]
                        S.op("pe", lambda e, ssb=ssb, t=t, hh=hh, pso=pso, v_sb=v_sb: e.matmul(pso[:, t * 128:(t + 1) * 128], lhsT=v_sb[:, t, hh * 128:(hh + 1) * 128], rhs=ssb[:], start=True, stop=False),
                             reads=[bssb, bv], writes=[bpso])
                        for cc in range(2):
                            c = 2 * t + cc
                            sref, bsref = r_["sref"].next()
                            S.op("pool", lambda e, h_=h_, r_=r_, sref=sref, c=c: e.tensor_scalar(out=sref[:], in0=r_["S"][:], scalar1=h_["ex"][:, 2 * c:2 * c + 1], scalar2=0.0, op0=ALU.mult, op1=ALU.add),
                                 reads=[r_["bS"], h_["bex"]], writes=[bsref])
                            tst, btst = r_["tst8"][c]
                            S.op("pool", lambda e, h_=h_, r_=r_, c=c: e.tensor_scalar(out=r_["S"][:], in0=r_["S"][:], scalar1=h_["ex"][:, 2 * c + 1:2 * c + 2], scalar2=0.0, op0=ALU.mult, op1=ALU.add),
                                 reads=[r_["bS"], h_["bex"]], writes=[r_["bS"]])
                            S.op("pool", lambda e, r_=r_, tst=tst: e.tensor_tensor(out=r_["S"][:], in0=r_["S"][:], in1=tst[:], op=ALU.add),
                                 reads=[r_["bS"], btst], writes=[r_["bS"]])
                            c0 = t * 128 + cc * 64
                            S.op("pe", lambda e, h_=h_, sref=sref, c0=c0, pso=pso, cc=cc: e.matmul(pso[:, c0:c0 + 64], lhsT=sref[:], rhs=h_["qt"][:, c0:c0 + 64], start=False, stop=(cc == 1)),
                                 reads=[bsref, h_["bqt"]], writes=[bpso])
                        yield
                if halo:
                    return
                for hh in range(2):
                    pso, bpso = PS[PS_O + hh], BPS[PS_O + hh]
                    S.op("act", lambda e, pso=pso: e.activation(out=o_sb[:], in_=pso[:], func=AF.Copy), reads=[bpso], writes=[bo])
                    S.op("act", lambda e, pso=pso: e.activation(out=sq_sb[:], in_=pso[:], func=AF.Square), reads=[bpso], writes=[bsq])
                    yield
                    S.op("pe", lambda e: e.matmul(PS[PS_SS][:], lhsT=ones_f[:], rhs=sq_sb[:], start=True, stop=True), reads=[bsq, bconst], writes=[BPS[PS_SS]])
                    S.op("act", lambda e: e.activation(out=rs_sb[:], in_=PS[PS_SS][:], func=AF.Ln, scale=1.0 / 128, bias=eps_t[:]), reads=[BPS[PS_SS], bconst], writes=[brs])
                    S.op("act", lambda e: e.activation(out=rs_sb[:], in_=rs_sb[:], func=AF.Exp, scale=-0.5), reads=[brs], writes=[brs])
                    S.op("dve", lambda e: e.scalar_tensor_tensor(out=o_sb[:], in0=o_sb[:], scalar=small[:, 24:25], in1=rs_sb[:], op0=ALU.mult, op1=ALU.mult),
                         reads=[bo, brs, bsmall], writes=[bo])
                    S.op("dve", lambda e, hh=hh, mixT=mixT, gT=gT: e.tensor_tensor(out=mixT[:, 2 + hh, :], in0=o_sb[:], in1=gT[:, hh, :], op=ALU.mult),
                         reads=[bo, bgT[hh]], writes=[bmix[2 + hh]])
                    yield
                for t in range(4):
                    acc, bacc = accr.next()
                    ti = (b - HB) * 4 + t
                    if p == 0:
                        r0 = b * 512 + t * 128
                        S.dma("sp", lambda e, acc=acc, r0=r0: e.dma_start(out=acc[:], in_=xseg[r0:r0 + 128, :]), writes=[bacc])
                    else:
                        S.dma("sp", lambda e, acc=acc, ti=ti: e.dma_start(out=acc[:], in_=x1s[ti * 128:(ti + 1) * 128, :]), reads=[b_x1[ti]], writes=[bacc])
                    for n in range(4):
                        ps, bps = ringA.next()
                        mm_group(ps[:], bps, [(mixT[:, c, t * 128:(t + 1) * 128], wout[:, c, n * 512:(n + 1) * 512]) for c in range(4)], [bwout[n]] + bmix)
                        S.op("dve", lambda e, acc=acc, ps=ps, n=n: e.tensor_tensor(out=acc[:, n * 512:(n + 1) * 512], in0=ps[:], in1=acc[:, n * 512:(n + 1) * 512], op=ALU.add),
                             reads=[bps, bacc], writes=[bacc])
                        if n % 2 == 1 and not os.environ.get("NOYIELD_OP"):
                            yield
                    S.dma("sp", lambda e, acc=acc, ti=ti: e.dma_start(out=x1s[ti * 128:(ti + 1) * 128, :], in_=acc[:]), reads=[bacc], writes=[b_x1[ti]])
                    if dbg and p == 3:
                        bo_ = Buf()
                        S.dma("sp", lambda e, acc=acc, ti=ti: e.dma_start(out=x1d[ti * 128:(ti + 1) * 128, :], in_=acc[:]), reads=[bacc], writes=[bo_])
                        outs.append(bo_)

            interleave([front(0)])
            for b in range(1, NBM):
                interleave([back(b - 1), front(b)])
            interleave([back(NBM - 1)])
            S.barrier()
            S.emit_all()

    S.mute = limit < 6
    with nc.reset_on_exit():
        KT = sb("KT", [128, NCH, 256], BF16)
        V = sb("V", [128, 2, D], BF16)
        bKT, bV = Buf(), Buf()
        nt = NormT("x1n", [0, 1])
        ringB = psring([2, 3, 4, 5, 6, 7])
        with nc.reset_on_exit():
            nt.load_gain(4)
            mT = sb("mT", [128, NCH, 256], BF16)
            bmT = Buf()
            mts = Ring([(sb("memt%d" % i, [128, D], F32), Buf()) for i in range(2)])
            for mt in range(2):
                xt, bxt = mts.next()
                S.dma("sp", lambda e, xt=xt, mt=mt: e.dma_start(out=xt[:], in_=mem[mt * 128:(mt + 1) * 128, :]), writes=[bxt])
                nt.run(xt[:], bxt, mT[:, :, mt * 128:(mt + 1) * 128], bmT)
            wkr = Ring([(sb("wkv%d" % i, [128, NCH, 512], BF16), Buf()) for i in range(2)])
            for n in range(4):
                wt, bwt = wkr.next()
                S.dma("pool", lambda e, wt=wt, n=n: e.dma_start(out=wt[:], in_=wslice(wk, 0, D, n * 512, (n + 1) * 512)), writes=[bwt])
                for jj in range(4):
                    ps, bps = ringB.next()
                    mm_group(ps[:, 0:256], bps, [(wt[:, c, jj * 128:(jj + 1) * 128], mT[:, c, :]) for c in range(NCH)], [bwt, bmT])
                    evac_copy(KT[:, n * 4 + jj, :], ps[:, 0:256], [bps], [bKT])
            for n in range(4):
                wt, bwt = wkr.next()
                S.dma("pool", lambda e, wt=wt, n=n: e.dma_start(out=wt[:], in_=wslice(wv, 0, D, n * 512, (n + 1) * 512)), writes=[bwt])
                for mt in range(2):
                    ps, bps = ringB.next()
                    mm_group(ps[:], bps, [(mT[:, c, mt * 128:(mt + 1) * 128], wt[:, c, :]) for c in range(NCH)], [bwt, bmT])
                    evac_copy(V[:, mt, n * 512:(n + 1) * 512], ps[:], [bps], [bV])
            S.barrier()
            S.emit_all()
        nt.load_gain(1)
        wq_sb = sb("wq_sb", [128, NCH, D], BF16)
        bwq4 = [Buf() for _ in range(4)]
        for n in range(4):
            S.dma("pool", lambda e, n=n: e.dma_start(out=wq_sb[:, :, n * 512:(n + 1) * 512], in_=wslice(wq, 0, D, n * 512, (n + 1) * 512)), writes=[bwq4[n]])
        xts = Ring([(sb("x1x%d" % i, [128, D], F32), Buf()) for i in range(2)])
        h2T = sb("h2T", [128, NCH, 512], BF16)
        bh2 = Buf()
        qTs = [(sb("qT%d" % i, [128, NCH, 512], BF16), Buf()) for i in range(2)]
        aT, baT = sb("aT0", [128, NCH, 512], BF16), Buf()
        attr = Ring([(sb("pex%d" % i, [128, 256], F32), sb("pn%d" % i, [128, 256], BF16), sb("PT%d" % i, [128, 2, 128], BF16), sb("sst%d" % i, [128, 8], F32),
                      Buf(), Buf(), Buf(), Buf()) for i in range(3)])
        SC = 512 ** -0.5

        def x1_norm(b):
            for t in range(4):
                xt, bxt = xts.next()
                ti = b * 4 + t
                S.dma("sp", lambda e, xt=xt, ti=ti: e.dma_start(out=xt[:], in_=x1s[ti * 128:(ti + 1) * 128, :]), reads=[b_x1[ti]], writes=[bxt])
                nt.run(xt[:], bxt, h2T[:, :, t * 128:(t + 1) * 128], bh2)

        def x1_qgroup(b, j):
            qT, bqT = qTs[b % 2]
            ps, bps = ringB.next()
            mm_group(ps[:], bps, [(wq_sb[:, c, j * 128:(j + 1) * 128], h2T[:, c, :]) for c in range(NCH)], [bwq4[j // 4], bh2])
            evac_copy(qT[:, j, :], ps[:], [bps], [bqT])

        def x1_attn(b, t, h):
            qT, bqT = qTs[b % 2]
            pex, pn, PT, sst, bpex, bpn, bPT, bsst = attr.next()
            ps, bps = ringB.next()
            mm_group(ps[:, 0:256], bps, [(qT[:, 4 * h + c4, t * 128:(t + 1) * 128], KT[:, 4 * h + c4, :]) for c4 in range(4)], [bqT, bKT])
            S.op("dve", lambda e, ps=ps, sst=sst: e.tensor_reduce(out=sst[:, 0:1], in_=ps[:, 0:256], axis=AX.X, op=ALU.max), reads=[bps], writes=[bsst])
            S.op("dve", lambda e, sst=sst: e.tensor_scalar(out=sst[:, 1:2], in0=sst[:, 0:1], scalar1=-SC, scalar2=None, op0=ALU.mult), reads=[bsst], writes=[bsst])
            S.op("act", lambda e, ps=ps, sst=sst, pex=pex: e.activation(out=pex[:], in_=ps[:, 0:256], func=AF.Exp, scale=SC, bias=sst[:, 1:2], accum_out=sst[:, 2:3]),
                 reads=[bps, bsst], writes=[bpex, bsst])
            S.op("dve", lambda e, sst=sst: e.reciprocal(out=sst[:, 3:4], in_=sst[:, 2:3]), reads=[bsst], writes=[bsst])
            S.op("dve", lambda e, sst=sst, pex=pex, pn=pn: e.tensor_scalar(out=pn[:], in0=pex[:], scalar1=sst[:, 3:4], scalar2=None, op0=ALU.mult), reads=[bpex, bsst], writes=[bpn])
            ps2, bps2 = ringB.next()
            psb = ps2[:].bitcast(BF16)
            for mc in range(2):
                S.op("pe", lambda e, psb=psb, mc=mc, pn=pn: e.transpose(psb[:, mc * 128:(mc + 1) * 128], pn[:, mc * 128:(mc + 1) * 128], id_b[:]),
                     reads=[bpn, bconst], writes=[bps2])
            S.op("act", lambda e, psb=psb, PT=PT: e.activation(out=PT[:].rearrange("p c n -> p (c n)"), in_=psb[:, 0:256], func=AF.Copy), reads=[bps2], writes=[bPT])
            ps3, bps3 = ringB.next()
            for j in range(4):
                mm_group(ps3[:, j * 128:(j + 1) * 128], bps3,
                         [(V[:, mc, h * 512 + j * 128:h * 512 + (j + 1) * 128], PT[:, mc, :]) for mc in range(2)], [bV, bPT])
            evac_copy(aT[:, 4 * h:4 * h + 4, t * 128:(t + 1) * 128], ps3[:].rearrange("p (c n) -> p c n", c=4), [bps3], [baT])

        x1_norm(0)
        for j in range(NCH):
            x1_qgroup(0, j)
        for b in range(NB):
            if b + 1 < NB:
                x1_norm(b + 1)
            for k in range(16):
                x1_attn(b, k // 4, k % 4)
                if b + 1 < NB:
                    x1_qgroup(b + 1, k)
            S.dma("sp", lambda e, b=b: e.dma_start(out=aTs[b], in_=aT[:].rearrange("p c n -> p (c n)")), reads=[baT], writes=[b_aT[b]])
        S.barrier()
        S.emit_all()

    S.mute = limit < 6.5
    with nc.reset_on_exit():
        nt = NormT("x2n", [0, 1, 2, 3])
        nt.load_gain(2)
        ringB = psring([4, 5, 6])
        wo_sb = sb("wo_sb", [128, NCH, D], BF16)
        bwo = Buf()
        for n in range(4):
            S.dma("pool", lambda e, n=n: e.dma_start(out=wo_sb[:, :, n * 512:(n + 1) * 512], in_=wslice(wo, 0, D, n * 512, (n + 1) * 512)), writes=[bwo])
        wr_sb = sb("wr_sb", [128, NCH, 20], F32)
        bwr = Buf()
        S.dma("sp", lambda e: e.dma_start(out=wr_sb[:].rearrange("p c n -> p (c n)"), in_=wr), writes=[bwr])
        aTr = Ring([(sb("aTi%d" % i, [128, NCH, 512], BF16), Buf()) for i in range(2)])
        xts = Ring([(sb("x2x%d" % i, [128, D], F32), Buf()) for i in range(3)])
        h3r = Ring([(sb("h3b%d" % i, [128, NCH, 512], BF16), Buf()) for i in range(2)])
        h3fr = Ring([(sb("h3f%d" % i, [128, NCH, 128], F32), Buf()) for i in range(2)])
        rtr = Ring([(sb("rt%d" % i, [128, 128], F32), Buf()) for i in range(2)])
        cmr = Ring([(sb("cmb%d" % i, [128, 16], F32), Buf()) for i in range(2)])
        def x2_normrouter(b, t, ti, xt, bxt, h3b, bh3b):
            h3f, bh3f = h3fr.next()
            rt, brt = rtr.next()
            nt.run(xt[:], bxt, h3b[:, :, t * 128:(t + 1) * 128], bh3b, h3f, bh3f)
            if limit == 6.5:
                S.mute = True
            psr, bpsr = PS[7], BPS[7]
            mm_group(psr[:, 0:20], bpsr, [(h3f[:, c, :], wr_sb[:, c, :]) for c in range(NCH)], [bh3f, bwr])
            cm, bcm = cmr.next()
            R_ = rt
            S.op("act", lambda e, R_=R_: e.activation(out=R_[:, 0:20], in_=psr[:, 0:20], func=AF.Copy), reads=[bpsr], writes=[brt])
            S.op("dve", lambda e, R_=R_: e.tensor_reduce(out=R_[:, 20:21], in_=R_[:, 0:4], axis=AX.X, op=ALU.max), reads=[brt], writes=[brt])
            S.op("dve", lambda e, R_=R_: e.tensor_scalar(out=R_[:, 21:22], in0=R_[:, 20:21], scalar1=-1.0, scalar2=None, op0=ALU.mult), reads=[brt], writes=[brt])
            S.op("act", lambda e, R_=R_: e.activation(out=R_[:, 24:28], in_=R_[:, 0:4], func=AF.Exp, bias=R_[:, 21:22], accum_out=R_[:, 22:23]), reads=[brt], writes=[brt])
            S.op("dve", lambda e, R_=R_: e.reciprocal(out=R_[:, 23:24], in_=R_[:, 22:23]), reads=[brt], writes=[brt])
            S.op("dve", lambda e, R_=R_: e.tensor_scalar(out=R_[:, 24:28], in0=R_[:, 0:4], scalar1=R_[:, 20:21], scalar2=None, op0=ALU.is_equal), reads=[brt], writes=[brt])
            S.op("dve", lambda e, R_=R_: e.tensor_scalar(out=R_[:, 24:28], in0=R_[:, 24:28], scalar1=-1.0, scalar2=1e30, op0=ALU.add, op1=ALU.mult), reads=[brt], writes=[brt])
            S.op("dve", lambda e, R_=R_: e.tensor_tensor(out=R_[:, 32:48].rearrange("p (g k) -> p g k", g=4), in0=R_[:, 4:20].rearrange("p (g k) -> p g k", g=4),
                                                  in1=R_[:, 24:28].rearrange("p (g k) -> p g k", k=1).to_broadcast([128, 4, 4]), op=ALU.add), reads=[brt], writes=[brt])
            S.op("dve", lambda e, R_=R_: e.max(out=R_[:, 48:56], in_=R_[:, 32:48]), reads=[brt], writes=[brt])
            S.op("dve", lambda e, R_=R_: e.tensor_tensor(out=R_[:, 56:57], in0=R_[:, 49:50], in1=R_[:, 48:49], op=ALU.subtract), reads=[brt], writes=[brt])
            S.op("act", lambda e, R_=R_: e.activation(out=R_[:, 57:58], in_=R_[:, 56:57], func=AF.Exp), reads=[brt], writes=[brt])
            S.op("dve", lambda e, R_=R_: e.tensor_scalar(out=R_[:, 58:59], in0=R_[:, 57:58], scalar1=1.0, scalar2=None, op0=ALU.add), reads=[brt], writes=[brt])
            S.op("dve", lambda e, R_=R_: e.reciprocal(out=R_[:, 59:60], in_=R_[:, 58:59]), reads=[brt], writes=[brt])
            S.op("dve", lambda e, R_=R_: e.tensor_tensor(out=R_[:, 60:61], in0=R_[:, 57:58], in1=R_[:, 59:60], op=ALU.mult), reads=[brt], writes=[brt])
            S.op("dve", lambda e, R_=R_: e.tensor_scalar(out=R_[:, 61:63], in0=R_[:, 59:61], scalar1=R_[:, 23:24], scalar2=None, op0=ALU.mult), reads=[brt], writes=[brt])
            S.op("dve", lambda e, R_=R_: e.tensor_scalar(out=R_[:, 64:80], in0=R_[:, 32:48], scalar1=R_[:, 48:49], scalar2=R_[:, 61:62], op0=ALU.is_equal, op1=ALU.mult), reads=[brt], writes=[brt])
            S.op("dve", lambda e, R_=R_: e.tensor_scalar(out=R_[:, 80:96], in0=R_[:, 32:48], scalar1=R_[:, 49:50], scalar2=R_[:, 62:63], op0=ALU.is_equal, op1=ALU.mult), reads=[brt], writes=[brt])
            S.op("dve", lambda e, cm=cm, R_=R_: e.tensor_tensor(out=cm[:], in0=R_[:, 64:80], in1=R_[:, 80:96], op=ALU.add), reads=[brt], writes=[bcm])
            S.dma("sp", lambda e, cm=cm, ti=ti: e.dma_start(out=combs[ti * 128:(ti + 1) * 128, :], in_=cm[:]), reads=[bcm], writes=[b_comb[ti]])
            if limit == 6.5:
                S.mute = False
            if t == 3:
                S.dma("sp", lambda e, h3b=h3b, b=b: e.dma_start(out=h3Ts[b], in_=h3b[:].rearrange("p c n -> p (c n)")), reads=[bh3b], writes=[b_h3T[b]])

        pend = None
        for b in range(NB):
            aT, baT = aTr.next()
            S.dma("sp", lambda e, aT=aT, b=b: e.dma_start(out=aT[:].rearrange("p c n -> p (c n)"), in_=aTs[b]), reads=[b_aT[b]], writes=[baT])
            h3b, bh3b = h3r.next()
            for t in range(4):
                ti = b * 4 + t
                xt, bxt = xts.next()
                S.dma("sp", lambda e, xt=xt, ti=ti: e.dma_start(out=xt[:], in_=x1s[ti * 128:(ti + 1) * 128, :]), reads=[b_x1[ti]], writes=[bxt])
                for n in range(4):
                    ps, bps = ringB.next()
                    mm_group(ps[:], bps, [(aT[:, c, t * 128:(t + 1) * 128], wo_sb[:, c, n * 512:(n + 1) * 512]) for c in range(NCH)], [bwo, baT])
                    S.op("dve", lambda e, xt=xt, ps=ps, n=n: e.tensor_tensor(out=xt[:, n * 512:(n + 1) * 512], in0=ps[:], in1=xt[:, n * 512:(n + 1) * 512], op=ALU.add),
                         reads=[bps, bxt], writes=[bxt])
                S.dma("sp", lambda e, xt=xt, ti=ti: e.dma_start(out=x2s[ti * 128:(ti + 1) * 128, :], in_=xt[:]), reads=[bxt], writes=[b_x2[ti]])
                if dbg:
                    bo_ = Buf()
                    S.dma("sp", lambda e, xt=xt, ti=ti: e.dma_start(out=x2d[ti * 128:(ti + 1) * 128, :], in_=xt[:]), reads=[bxt], writes=[bo_])
                    outs.append(bo_)
                if pend is not None:
                    x2_normrouter(*pend)
                pend = (b, t, ti, xt, bxt, h3b, bh3b)
        x2_normrouter(*pend)
        S.barrier()
        S.emit_all()

    S.mute = limit < 8
    with nc.reset_on_exit():
        h3T = sb("e_h3T", [128, NCH, EB], BF16)
        bh3 = Buf()
        y = sb("e_y", [128, ETI, D], F32)
        by = [Buf() for _ in range(ETI)]
        wring = Ring([(sb("e_w%d" % i, [128, 8192], BF16), Buf()) for i in range(5)])
        cmb = sb("e_cmb", [128, ETI, 16], F32)
        bcmb = Buf()
        sgr = Ring([(sb("e_sg%d" % i, [128, 512], F32), Buf()) for i in range(2)])
        hidr = Ring([(sb("e_hid%d" % i, [128, 512], BF16), Buf()) for i in range(2)])
        hTr_ = Ring([(sb("e_hT%d" % i, [128, 4, 128], BF16), Buf()) for i in range(2)])
        xts = Ring([(sb("e_x%d" % i, [128, D], F32), Buf()) for i in range(1)])
        gB = sb("e_gB", [128, D], F32)
        fst = sb("e_fst", [128, 4], F32)
        bjunk, bgB, bfst = Buf(), Buf(), Buf()
        S.dma("sp", lambda e: e.dma_start(out=gB[:], in_=gains[3:4, :].to_broadcast([128, D])), writes=[bgB])
        gur = Ring([((PS[0], BPS[0]), (PS[1], BPS[1])), ((PS[2], BPS[2]), (PS[3], BPS[3]))])
        ringY = psring([5, 6, 7])
        PS_T = 4
        pending_fn = None
        for eb in range(T // EB):
            for half in range(EB // 512):
                bi = eb * (EB // 512) + half
                S.dma("sp", lambda e, half=half, bi=bi: e.dma_start(out=h3T[:, :, half * 512:(half + 1) * 512], in_=h3Ts[bi].rearrange("p (c n) -> p c n", c=NCH)),
                      reads=[b_h3T[bi]], writes=[bh3])
            for i in range(ETI):
                ti = eb * ETI + i
                S.dma("sp", lambda e, i=i, ti=ti: e.dma_start(out=cmb[:, i, :], in_=combs[ti * 128:(ti + 1) * 128, :]), reads=[b_comb[ti]], writes=[bcmb])
            for ex in range(16):
                wg, bwg = wring.next()
                S.dma("pool", lambda e, wg=wg, ex=ex: e.dma_start(out=wg[:].rearrange("p (c n) -> p c n", c=NCH), in_=w_gate[ex].rearrange("(c p) n -> p c n", p=128)), writes=[bwg])
                wu, bwu = wring.next()
                S.dma("pool", lambda e, wu=wu, ex=ex: e.dma_start(out=wu[:].rearrange("p (c n) -> p c n", c=NCH), in_=w_up[ex].rearrange("(c p) n -> p c n", p=128)), writes=[bwu])
                wd, bwd = wring.next()
                for n in range(4):
                    S.dma("pool", lambda e, wd=wd, ex=ex, n=n: e.dma_start(out=wd[:].rearrange("p (c n) -> p c n", c=4)[:, :, n * 512:(n + 1) * 512],
                                                                    in_=w_down[ex][:, n * 512:(n + 1) * 512].rearrange("(c p) n -> p c n", p=128)), writes=[bwd])
                wg3 = wg[:].rearrange("p (c n) -> p c n", c=NCH)
                wu3 = wu[:].rearrange("p (c n) -> p c n", c=NCH)
                wd3 = wd[:].rearrange("p (c n) -> p c n", c=4)

                def GUg(i):
                    (pg, bpg), (pu, bpu) = gur.next()
                    mm_group(pg[:], bpg, [(h3T[:, c, i * 128:(i + 1) * 128], wg3[:, c, :]) for c in range(NCH)], [bh3, bwg])
                    return (pg, bpg, pu, bpu)

                def GUu(i, gu):
                    pg, bpg, pu, bpu = gu
                    mm_group(pu[:], bpu, [(h3T[:, c, i * 128:(i + 1) * 128], wu3[:, c, :]) for c in range(NCH)], [bh3, bwu])

                def HID(i, gu, ex=ex):
                    pg, bpg, pu, bpu = gu
                    sg, bsg = sgr.next()
                    S.op("act", lambda e: e.activation(out=sg[:], in_=pg[:], func=AF.Silu), reads=[bpg], writes=[bsg])
                    hid, bhid = hidr.next()
                    S.op("dve", lambda e: e.scalar_tensor_tensor(out=hid[:], in0=sg[:], scalar=cmb[:, i, ex:ex + 1], in1=pu[:], op0=ALU.mult, op1=ALU.mult),
                         reads=[bsg, bpu, bcmb], writes=[bhid])
                    return hid, bhid

                def TR(i, hb):
                    hid, bhid = hb
                    psb = PS[PS_T][:].bitcast(BF16)
                    for c4 in range(4):
                        S.op("pe", lambda e, c4=c4: e.transpose(psb[:, c4 * 128:(c4 + 1) * 128], hid[:, c4 * 128:(c4 + 1) * 128], id_b[:]),
                             reads=[bhid, bconst], writes=[BPS[PS_T]])
                    hT_, bhT_ = hTr_.next()
                    S.op("act", lambda e: e.activation(out=hT_[:].rearrange("p c n -> p (c n)"), in_=psb[:, 0:512], func=AF.Copy), reads=[BPS[PS_T]], writes=[bhT_])
                    return hT_, bhT_

                def DOWN(i, hb, ex=ex):
                    hT_, bhT_ = hb
                    for n in range(4):
                        ps, bps = ringY.next()
                        mm_group(ps[:], bps, [(hT_[:, c4, :], wd3[:, c4, n * 512:(n + 1) * 512]) for c4 in range(4)], [bhT_, bwd])
                        if ex == 0:
                            S.op("dve", lambda e, ps=ps, n=n: e.tensor_copy(out=y[:, i, n * 512:(n + 1) * 512], in_=ps[:]), reads=[bps], writes=[by[i]])
                        else:
                            S.op("dve", lambda e, ps=ps, n=n: e.tensor_tensor(out=y[:, i, n * 512:(n + 1) * 512], in0=ps[:], in1=y[:, i, n * 512:(n + 1) * 512], op=ALU.add),
                                 reads=[bps, by[i]], writes=[by[i]])

                g_cur = GUg(0)
                GUu(0, g_cur)
                for i in range(ETI):
                    hb = HID(i, g_cur)
                    g_next = GUg(i + 1) if i + 1 < ETI else None
                    if ex == 0 and pending_fn is not None:
                        pending_fn(i)
                    tb = TR(i, hb)
                    if g_next is not None:
                        GUu(i + 1, g_next)
                    DOWN(i, tb)
                    g_cur = g_next
                if ex == 0:
                    pending_fn = None

            def final_norm(i, eb=eb):
                ti = eb * ETI + i
                xt, bxt = xts.next()
                S.dma("sp", lambda e, xt=xt, ti=ti: e.dma_start(out=xt[:], in_=x2s[ti * 128:(ti + 1) * 128, :]), reads=[b_x2[ti]], writes=[bxt])
                S.op("dve", lambda e, xt=xt, i=i: e.tensor_tensor(out=y[:, i, :], in0=y[:, i, :], in1=xt[:], op=ALU.add), reads=[bxt, by[i]], writes=[by[i]])
                S.op("act", lambda e, i=i, xt=xt: e.activation(out=xt[:], in_=y[:, i, :], func=AF.Square, accum_out=fst[:, 0:1]), reads=[by[i]], writes=[bxt, bfst])
                S.op("act", lambda e: e.activation(out=fst[:, 1:2], in_=fst[:, 0:1], func=AF.Sqrt, scale=1.0 / D, bias=eps_t[:]), reads=[bfst, bconst], writes=[bfst])
                S.op("dve", lambda e: e.reciprocal(out=fst[:, 2:3], in_=fst[:, 1:2]), reads=[bfst], writes=[bfst])
                S.op("dve", lambda e, xt=xt, i=i: e.scalar_tensor_tensor(out=xt[:], in0=y[:, i, :], scalar=fst[:, 2:3], in1=gB[:], op0=ALU.mult, op1=ALU.mult),
                     reads=[by[i], bfst, bgB], writes=[bxt])
                bo_ = Buf()
                S.dma("sp", lambda e, xt=xt, ti=ti: e.dma_start(out=out[ti * 128:(ti + 1) * 128, :], in_=xt[:]), reads=[bxt], writes=[bo_])
                outs.append(bo_)

            if eb + 1 < T // EB:
                pending_fn = final_norm
            else:
                for i in range(ETI):
                    final_norm(i)
        S.mute = False
        S.final_wait("sp", outs)
        S.barrier()
        S.emit_all()
    return nc


_CACHE = {}


def _consts():
    s = np.arange(128)[:, None]
    t = np.arange(128)[None, :]
    bd = ((s // 64 == t // 64) & (s <= t)).astype(np.uint32)
    rm = np.ones((128, 512), np.float32)
    rm[:, ::64] = 0.0
    return np.eye(128, dtype=np.float32), bd, rm


def run(inputs, dbg=False, limit=99):
    x = np.asarray(inputs["x"], np.float32)
    B, SEQ, _ = x.shape
    SEG = 4
    T = SEQ // SEG
    W = HALO
    key = (T, W, dbg, limit)
    if key not in _CACHE:
        _CACHE[key] = build(T, W, dbg, limit)
    nc = _CACHE[key]
    f = lambda k: np.ascontiguousarray(np.asarray(inputs[k], np.float32))
    ident, bd, rm = _consts()
    gains = np.stack([f("norm_mix")[0], f("norm_xattn")[0], f("norm_ffn")[0], f("norm_final"), f("norm_mem")[0]], 0)
    pscale = np.ascontiguousarray(f("pool_scale")[0].reshape(8, 128).T)
    lbl = np.ascontiguousarray(f("hgrn_lb_logits").reshape(2, 8, 128).transpose(2, 0, 1).reshape(128, 16))
    hnorm = np.ascontiguousarray(f("hgrn_norm")[0].reshape(128, 1))
    wr = np.concatenate([f("router_group")[0], f("router_expert")[0]], axis=1)
    wr = np.ascontiguousarray(wr.reshape(NCH, 128, 20).transpose(1, 0, 2).reshape(128, NCH * 20))
    shared = {
        "w_in": f("w_in")[0], "pool_w": f("pool_w")[0], "w_out": f("w_out")[0],
        "wq": f("xattn_wq")[0], "wk": f("xattn_wk")[0], "wv": f("xattn_wv")[0], "wo": f("xattn_wo")[0],
        "wr": wr, "w_gate": f("w_gate")[0], "w_up": f("w_up")[0], "w_down": f("w_down")[0],
        "gains": np.ascontiguousarray(gains), "pscale": pscale, "lbl": lbl, "hnorm": hnorm,
        "ident": ident, "bdmask": bd, "rmask": rm,
    }
    memf = f("mem")
    in_maps = []
    for c in range(B * SEG):
        b, j = divmod(c, SEG)
        seg = np.zeros((W + T, D), np.float32)
        if j == 0:
            seg[W:] = x[b, 0:T]
        else:
            seg[:] = x[b, j * T - W:(j + 1) * T]
        ic = np.empty((4, 512), np.float32)
        for g, w_ in enumerate((2, 4, 8, 16)):
            if j == 0:
                ic[g] = 1.0 / np.minimum(np.arange(512) + 1, w_)
            else:
                ic[g] = 1.0 / w_
        m = dict(shared)
        m["xseg"] = seg
        m["mem"] = np.ascontiguousarray(memf[b])
        m["invcnt"] = np.ascontiguousarray(np.broadcast_to(ic.reshape(1, 4 * 512), (128, 4 * 512)))
        in_maps.append(m)
    res = run_bass_kernel_spmd(nc, in_maps, core_ids=list(range(B * SEG)))
    names = ["out"] + (["x1d", "x2d"] if dbg else [])
    outd = {}
    for nm in names:
        o = np.empty((B, SEQ, D), np.float32)
        for c in range(B * SEG):
            b, j = divmod(c, SEG)
            o[b, j * T:(j + 1) * T] = np.asarray(res.results[c][nm])
        outd[nm] = o
    return outd


def kernel(**inputs):
    return run(inputs)["out"]
```

```python
import os
import numpy as np
import concourse.bass as bass
import concourse.mybir as mybir
from concourse.bass_utils import run_bass_kernel_spmd

F32 = mybir.dt.float32
BF16 = mybir.dt.bfloat16
U32 = mybir.dt.uint32
AF = mybir.ActivationFunctionType
ALU = mybir.AluOpType
AX = mybir.AxisListType

ENGS = ["pe", "act", "dve", "pool", "sp"]
N_DMA_SEMS = 32
D = 2048
NCH = 16
EPS = 1e-6
HALO = 512


class Buf:
    __slots__ = ("name", "w", "r")

    def __init__(self, name=""):
        self.name = name
        self.w = {}
        self.r = {}


class Sched:
    def __init__(self, nc):
        self.nc = nc
        self.prog = {e: [] for e in ENGS}
        self.cnt = {e: 0 for e in ENGS}
        self.seen = {e: {} for e in ENGS}
        self.sem = {e: nc.alloc_semaphore("sem_" + e) for e in ENGS}
        self.dsem = [nc.alloc_semaphore("dsem%d" % i) for i in range(N_DMA_SEMS)]
        self.dcnt = [0] * N_DMA_SEMS
        self.dnext2 = [0, 0]
        self.mute = False
        self.touched = set()

    def _deps(self, reads, writes):
        deps = {}
        for b in reads:
            for k, n in b.w.items():
                if deps.get(k, 0) < n:
                    deps[k] = n
        for b in writes:
            for d in (b.w, b.r):
                for k, n in d.items():
                    if deps.get(k, 0) < n:
                        deps[k] = n
        return deps

    def _waits(self, eng, deps):
        waits = []
        seen = self.seen[eng]
        for k, n in deps.items():
            if k == "pe" and eng == "pe":
                continue
            if seen.get(k, 0) < n:
                seen[k] = n
                waits.append((k, n))
        return waits

    def _semof(self, k):
        if isinstance(k, tuple):
            return self.dsem[k[1]], 16
        return self.sem[k], 1

    def op(self, eng, emit, reads=(), writes=()):
        if self.mute:
            return
        deps = self._deps(reads, writes)
        waits = self._waits(eng, deps)
        self.cnt[eng] += 1
        my = self.cnt[eng]
        self.prog[eng].append((waits, emit, self.sem[eng], 1))
        self.touched.update(reads)
        self.touched.update(writes)
        for b in writes:
            b.w = {eng: my}
            b.r = {}
        for b in reads:
            if b.r.get(eng, 0) < my:
                b.r[eng] = my

    def dma(self, eng, emit, reads=(), writes=()):
        if self.mute:
            return
        half = N_DMA_SEMS // 2
        q = 1 if eng == "pool" else 0
        i = q * half + self.dnext2[q]
        self.dnext2[q] = (self.dnext2[q] + 1) % half
        key = ("dma", i)
        deps = self._deps(reads, writes)
        if self.dcnt[i] > 0:
            deps[key] = max(deps.get(key, 0), self.dcnt[i])
        waits = self._waits(eng, deps)
        self.dcnt[i] += 1
        my = self.dcnt[i]
        self.prog[eng].append((waits, emit, self.dsem[i], 16))
        self.touched.update(reads)
        self.touched.update(writes)
        for b in writes:
            b.w = {key: my}
            b.r = {}
        for b in reads:
            if b.r.get(key, 0) < my:
                b.r[key] = my

    def final_wait(self, eng, bufs):
        deps = {}
        for b in bufs:
            for k, n in b.w.items():
                deps[k] = max(deps.get(k, 0), n)
        waits = self._waits(eng, deps)
        self.prog[eng].append((waits, None, None, 0))

    def barrier(self):
        deps = {e: self.cnt[e] for e in ENGS if self.cnt[e] > 0}
        for i in range(N_DMA_SEMS):
            if self.dcnt[i] > 0:
                deps[("dma", i)] = self.dcnt[i]
        for eng in ENGS:
            waits = []
            seen = self.seen[eng]
            for k, n in deps.items():
                if k == eng:
                    continue
                if seen.get(k, 0) < n:
                    seen[k] = n
                    waits.append((k, n))
            self.prog[eng].append((waits, None, None, 0))

    def emit_all(self):
        nc = self.nc

        def run(e, name):
            for waits, emit, sem, inc in self.prog[name]:
                for k, n in waits:
                    s, mult = self._semof(k)
                    e.wait_ge(s, n * mult)
                if emit is not None:
                    emit(e).then_inc(sem, inc)

        with nc.Block() as block:
            @block.tensor
            def _(e):
                run(e, "pe")

            @block.scalar
            def _(e):
                run(e, "act")

            @block.vector
            def _(e):
                run(e, "dve")

            @block.gpsimd
            def _(e):
                run(e, "pool")

            @block.sync
            def _(e):
                run(e, "sp")
        self.prog = {e: [] for e in ENGS}
        for b in self.touched:
            b.w = {}
            b.r = {}
        self.touched = set()
        self.cnt = {e: 0 for e in ENGS}
        self.seen = {e: {} for e in ENGS}
        self.dcnt = [0] * N_DMA_SEMS


class Ring:
    def __init__(self, items):
        self.items = items
        self.i = 0

    def next(self):
        it = self.items[self.i % len(self.items)]
        self.i += 1
        return it


def build(T, W, dbg=False, limit=99):
    nc = bass.Bass("TRN2", target_bir_lowering=False)
    S = Sched(nc)
    NBM = (W + T) // 512
    HB = W // 512
    NB = T // 512
    EB = 1024 if T % 1024 == 0 else 512
    ETI = EB // 128

    uid = [0]

    def sb(name, shape, dt):
        uid[0] += 1
        return nc.alloc_sbuf_tensor("%s_%d" % (name, uid[0]), shape, dt)

    def din(name, shape, dt=F32):
        return nc.dram_tensor(name, list(shape), dt, kind="ExternalInput").ap()

    xseg = din("xseg", [W + T, D])
    mem = din("mem", [256, D])
    w_in = din("w_in", [D, 5120])
    pool_w = din("pool_w", [4, 256, 256])
    w_out = din("w_out", [D, D])
    wq = din("wq", [D, D])
    wk = din("wk", [D, D])
    wv = din("wv", [D, D])
    wo = din("wo", [D, D])
    wr = din("wr", [128, NCH * 20])
    w_gate = din("w_gate", [16, D, 512])
    w_up = din("w_up", [16, D, 512])
    w_down = din("w_down", [16, 512, D])
    gains = din("gains", [5, D])
    pscale = din("pscale", [128, 8])
    lbl = din("lbl", [128, 16])
    hnorm = din("hnorm", [128, 1])
    ident = din("ident", [128, 128])
    bdmask = din("bdmask", [128, 128], U32)
    rmask = din("rmask", [128, 512])
    invcnt = din("invcnt", [128, 4 * 512])
    out = nc.dram_tensor("out", [T, D], F32, kind="ExternalOutput").ap()
    if dbg:
        x1d = nc.dram_tensor("x1d", [T, D], F32, kind="ExternalOutput").ap()
        x2d = nc.dram_tensor("x2d", [T, D], F32, kind="ExternalOutput").ap()

    hT1 = nc.dram_tensor("hT1", [NBM, 128, NCH * 512], BF16).ap()
    x1s = nc.dram_tensor("x1s", [T, D], F32).ap()
    aTs = nc.dram_tensor("aTs", [NB, 128, NCH * 512], BF16).ap()
    x2s = nc.dram_tensor("x2s", [T, D], F32).ap()
    h3Ts = nc.dram_tensor("h3Ts", [NB, 128, NCH * 512], BF16).ap()
    combs = nc.dram_tensor("combs", [T, 16], F32).ap()
    b_hT1 = [Buf() for _ in range(NBM)]
    b_x1 = [Buf() for _ in range(T // 128)]
    b_aT = [Buf() for _ in range(NB)]
    b_x2 = [Buf() for _ in range(T // 128)]
    b_h3T = [Buf() for _ in range(NB)]
    b_comb = [Buf() for _ in range(T // 128)]
    outs = []

    PS = [nc.alloc_psum_tensor("ps%d" % i, [128, 512], F32) for i in range(8)]
    BPS = [Buf("ps%d" % i) for i in range(8)]

    def psring(idx):
        return Ring([(PS[i], BPS[i]) for i in idx])

    id_f = sb("id_f", [128, 128], F32)
    id_b = sb("id_b", [128, 128], BF16)
    ones_f = sb("ones_f", [128, 128], F32)
    eps_t = sb("eps_t", [128, 1], F32)
    bconst = Buf("const")
    S.dma("sp", lambda e: e.dma_start(out=id_f[:], in_=ident), writes=[bconst])
    S.dma("pool", lambda e: e.dma_start(out=id_b[:], in_=ident), writes=[bconst])
    S.op("dve", lambda e: e.memset(ones_f[:], 1.0), writes=[bconst])
    S.op("dve", lambda e: e.memset(eps_t[:], EPS), writes=[bconst])

    flip = [0]

    def evac_copy(dst, src, reads, writes):
        flip[0] ^= 1
        if flip[0]:
            S.op("act", lambda e: e.activation(out=dst, in_=src, func=AF.Copy), reads=reads, writes=writes)
        else:
            S.op("dve", lambda e: e.tensor_copy(out=dst, in_=src), reads=reads, writes=writes)

    def mm_group(ps_ap, bps, pairs, reads):
        n = len(pairs)
        for i, (l, r) in enumerate(pairs):
            S.op("pe", lambda e, l=l, r=r, i=i: e.matmul(ps_ap, lhsT=l, rhs=r, start=(i == 0), stop=(i == n - 1)),
                 reads=reads, writes=[bps])

    class NormT:
        def __init__(self, tag, banks):
            self.tmp = Ring([(sb(tag + "junk%d" % i, [128, D], BF16), sb(tag + "xn%d" % i, [128, D], F32), sb(tag + "st%d" % i, [128, 4], F32),
                              Buf(), Buf(), Buf()) for i in range(2)])
            self.gB = sb(tag + "gB", [128, D], F32)
            self.bg = Buf()
            self.ring = psring(banks)

        def load_gain(self, row):
            S.dma("sp", lambda e: e.dma_start(out=self.gB[:], in_=gains[row:row + 1, :].to_broadcast([128, D])), writes=[self.bg])

        def run(self, src, bsrc, dst_bf, bdst, dst_f=None, bdst_f=None):
            junk, xn, st, bj, bxn, bst = self.tmp.next()
            S.op("act", lambda e: e.activation(out=junk[:], in_=src, func=AF.Square, accum_out=st[:, 0:1]),
                 reads=[bsrc], writes=[bj, bst])
            S.op("act", lambda e: e.activation(out=st[:, 1:2], in_=st[:, 0:1], func=AF.Sqrt, scale=1.0 / D, bias=eps_t[:]),
                 reads=[bst, bconst], writes=[bst])
            S.op("dve", lambda e: e.reciprocal(out=st[:, 2:3], in_=st[:, 1:2]), reads=[bst], writes=[bst])
            S.op("dve", lambda e: e.scalar_tensor_tensor(out=xn[:], in0=src, scalar=st[:, 2:3], in1=self.gB[:],
                                                         op0=ALU.mult, op1=ALU.mult),
                 reads=[bsrc, bst, self.bg], writes=[bxn])
            for b4 in range(4):
                ps, bps = self.ring.next()
                for j in range(4):
                    c = b4 * 4 + j
                    S.op("pe", lambda e, ps=ps, j=j, c=c: e.transpose(ps[:, j * 128:(j + 1) * 128], xn[:, c * 128:(c + 1) * 128], id_f[:]),
                         reads=[bxn, bconst], writes=[bps])
                src3 = ps[:].rearrange("p (c n) -> p c n", c=4)
                if dst_f is None:
                    evac_copy(dst_bf[:, b4 * 4:(b4 + 1) * 4, :], src3, [bps], [bdst])
                else:
                    evac_copy(dst_f[:, b4 * 4:(b4 + 1) * 4, :], src3, [bps], [bdst_f])
                    S.op("pool", lambda e, b4=b4: e.tensor_copy(out=dst_bf[:, b4 * 4:(b4 + 1) * 4, :], in_=dst_f[:, b4 * 4:(b4 + 1) * 4, :]),
                         reads=[bdst_f], writes=[bdst])

    def wslice(w, r0, r1, c0, c1):
        return w[r0:r1, c0:c1].rearrange("(c p) n -> p c n", p=128)

    S.mute = limit < 1
    with nc.reset_on_exit():
        nt = NormT("n1", [0, 1, 2, 3, 4, 5, 6, 7])
        nt.load_gain(0)
        xts = Ring([(sb("n1x%d" % i, [128, D], F32), Buf()) for i in range(3)])
        hbs = Ring([(sb("n1h%d" % i, [128, NCH, 512], BF16), Buf()) for i in range(2)])
        for b in range(NBM):
            hb, bhb = hbs.next()
            for t in range(4):
                xt, bxt = xts.next()
                r0 = b * 512 + t * 128
                S.dma("sp", lambda e, xt=xt, r0=r0: e.dma_start(out=xt[:], in_=xseg[r0:r0 + 128, :]), writes=[bxt])
                nt.run(xt[:], bxt, hb[:, :, t * 128:(t + 1) * 128], bhb)
            S.dma("sp", lambda e, hb=hb, b=b: e.dma_start(out=hT1[b], in_=hb[:].rearrange("p c n -> p (c n)")),
                  reads=[bhb], writes=[b_hT1[b]])
        S.barrier()
        S.emit_all()

    def interleave(gens):
        gens = list(gens)
        while gens:
            for g in list(gens):
                try:
                    next(g)
                except StopIteration:
                    gens.remove(g)

    for p in range(4):
        S.mute = limit < 2 + p
        with nc.reset_on_exit():
            win = sb("win", [128, NCH, 5, 256], BF16)
            wout = sb("wout", [128, 4, D], BF16)
            pw = sb("pw", [128, 2, 256], BF16)
            bwin, bwout, bpw = [Buf() for _ in range(5)], [Buf() for _ in range(4)], Buf()
            for g5 in (0, 3, 1, 2, 4):
                c0 = g5 * 1024 + 256 * p
                S.dma("pool", lambda e, g5=g5, c0=c0: e.dma_start(out=win[:, :, g5, :], in_=wslice(w_in, 0, D, c0, c0 + 256)), writes=[bwin[g5]])
            S.dma("pool", lambda e: e.dma_start(out=pw[:], in_=pool_w[p].rearrange("(c q) n -> q c n", q=128)), writes=[bpw])
            for n in range(4):
                S.dma("pool", lambda e, n=n: e.dma_start(out=wout[:, 0:2, n * 512:(n + 1) * 512], in_=wslice(w_out, 256 * p, 256 * p + 256, n * 512, (n + 1) * 512)), writes=[bwout[n]])
                S.dma("pool", lambda e, n=n: e.dma_start(out=wout[:, 2:4, n * 512:(n + 1) * 512], in_=wslice(w_out, 1024 + 256 * p, 1024 + 256 * p + 256, n * 512, (n + 1) * 512)), writes=[bwout[n]])
            small = sb("msmall", [128, 64], F32)
            bsmall = Buf()
            S.dma("sp", lambda e: e.dma_start(out=small[:, 0:8], in_=pscale), writes=[bsmall])
            S.dma("sp", lambda e: e.dma_start(out=small[:, 8:24], in_=lbl), writes=[bsmall])
            S.dma("sp", lambda e: e.dma_start(out=small[:, 24:25], in_=hnorm), writes=[bsmall])
            S.op("dve", lambda e: e.tensor_tensor(out=small[:, 32:34], in0=small[:, 8 + 2 * p:10 + 2 * p], in1=small[:, 16 + 2 * p:18 + 2 * p], op=ALU.subtract),
                 reads=[bsmall], writes=[bsmall])
            S.op("act", lambda e: e.activation(out=small[:, 25:27], in_=small[:, 32:34], func=AF.Sigmoid), reads=[bsmall], writes=[bsmall])
            S.op("dve", lambda e: e.tensor_scalar(out=small[:, 27:29], in0=small[:, 25:27], scalar1=-1.0, scalar2=1.0, op0=ALU.mult, op1=ALU.add),
                 reads=[bsmall], writes=[bsmall])
            S.op("dve", lambda e: e.tensor_scalar(out=small[:, 29:31], in0=small[:, 27:29], scalar1=-1.0, scalar2=None, op0=ALU.mult),
                 reads=[bsmall], writes=[bsmall])
            rm_sb = sb("rm_sb", [128, 512], F32)
            bd_sb = sb("bd_sb", [128, 128], U32)
            ic_sb = sb("ic_sb", [128, 512], F32)
            bcm_ = Buf()
            S.dma("sp", lambda e: e.dma_start(out=rm_sb[:], in_=rmask), writes=[bcm_])
            S.dma("sp", lambda e: e.dma_start(out=bd_sb[:], in_=bdmask), writes=[bcm_])
            S.dma("sp", lambda e: e.dma_start(out=ic_sb[:], in_=invcnt[:, p * 512:(p + 1) * 512]), writes=[bcm_])

            hTr = Ring([(sb("mh%d" % i, [128, NCH, 512], BF16), Buf()) for i in range(2)])
            uT = sb("uT", [128, 2, 528], F32)
            b_uh, b_um = Buf(), Buf()
            s_a = sb("s_a", [128, 2, 528], F32)
            s_b = sb("s_b", [128, 2, 528], F32)
            bsa, bsb = Buf(), Buf()
            pooled = sb("pooled", [128, 2, 512], BF16)
            bpooled = Buf()
            tmpH = []
            for hh in range(2):
                d_ = {}
                for nm in ["q", "sig", "lf", "kk", "a", "at"]:
                    d_[nm] = sb("h%d%s" % (hh, nm), [128, 512], F32)
                    d_["b" + nm] = Buf()
                d_["e1"], d_["be1"] = d_["lf"], d_["blf"]
                d_["e2"], d_["be2"] = d_["sig"], d_["bsig"]
                tmpH.append(d_)
            sets = []
            for si in range(2):
                st_ = {"hd": []}
                for hh in range(2):
                    d_ = {}
                    for nm in ["qt", "ktT"]:
                        d_[nm] = sb("s%dh%d%s" % (si, hh, nm), [128, 512], BF16)
                        d_["b" + nm] = Buf()
                    d_["ktok"] = sb("s%dh%dktok" % (si, hh), [128, 4, 128], BF16)
                    d_["bktok"] = Buf()
                    d_["ex"] = sb("s%dh%dex" % (si, hh), [128, 32], F32)
                    d_["bex"] = Buf()
                    st_["hd"].append(d_)
                st_["gT"] = sb("s%dgT" % si, [128, 2, 512], F32)
                st_["bgT"] = [Buf(), Buf()]
                st_["v"] = sb("s%dv" % si, [128, 4, 256], BF16)
                st_["bv"] = Buf()
                st_["mixT"] = sb("s%dmixT" % si, [128, 4, 512], BF16)
                st_["bmix"] = [Buf() for _ in range(4)]
                sets.append(st_)
            rec = []
            for hh in range(2):
                d_ = {}
                d_["S"] = sb("h%dS" % hh, [128, 128], F32)
                d_["bS"] = Buf()
                d_["sref"] = Ring([(sb("h%dsr%d" % (hh, i), [128, 128], BF16), Buf()) for i in range(3)])
                d_["tst8"] = [(sb("h%dts%d" % (hh, i), [128, 128], F32), Buf()) for i in range(8)]
                d_["ssb4"] = [(sb("h%dss%d" % (hh, i), [128, 128], BF16), Buf()) for i in range(4)]
                for (tt, bb) in d_["ssb4"]:
                    S.op("pool", lambda e, tt=tt: e.memset(tt[:], 0.0), writes=[bb])
                S.op("pool", lambda e, d_=d_: e.memset(d_["S"][:], 0.0), writes=[d_["bS"]])
                rec.append(d_)
            o_sb = sb("o_sb", [128, 512], F32)
            sq_sb = sb("sq_sb", [128, 512], F32)
            rs_sb = sb("rs_sb", [128, 512], F32)
            bo, bsq, brs = Buf(), Buf(), Buf()
            accr = Ring([(sb("acc%d" % i, [128, D], F32), Buf()) for i in range(2)])
            ringA = psring([0, 1, 2])
            PS_S, PS_O, PS_ST, PS_KT, PS_SS = 3, 4, 6, 7, 7

            def front(b):
                halo = b < HB
                st_ = sets[b % 2]
                mixT, bmix = st_["mixT"], st_["bmix"]
                v_sb, bv = st_["v"], st_["bv"]
                gT, bgT = st_["gT"], st_["bgT"]
                hT, bhT = hTr.next()
                S.dma("sp", lambda e, hT=hT, b=b: e.dma_start(out=hT[:].rearrange("p c n -> p (c n)"), in_=hT1[b]),
                      reads=[b_hT1[b]], writes=[bhT])
                if b == 0:
                    S.op("dve", lambda e: e.memset(uT[:, :, 0:16], 0.0), writes=[b_uh])
                else:
                    S.op("dve", lambda e: e.tensor_copy(out=uT[:, :, 0:16], in_=uT[:, :, 512:528]), reads=[b_um], writes=[b_uh])
                for j in range(2):
                    ps, bps = ringA.next()
                    mm_group(ps[:], bps, [(win[:, c, 0, j * 128:(j + 1) * 128], hT[:, c, :]) for c in range(NCH)], [bwin[0], bhT])
                    S.op("act", lambda e, ps=ps, j=j: e.activation(out=uT[:, j, 16:528], in_=ps[:], func=AF.Copy), reads=[bps], writes=[b_um])
                    yield
                for t in range(4):
                    ps, bps = ringA.next()
                    mm_group(ps[:, 0:256], bps, [(hT[:, c, t * 128:(t + 1) * 128], win[:, c, 3, :]) for c in range(NCH)], [bwin[3], bhT])
                    S.op("dve", lambda e, ps=ps, t=t: e.tensor_copy(out=v_sb[:, t, :], in_=ps[:, 0:256]), reads=[bps], writes=[bv])
                    if t % 2 == 1:
                        yield
                if not halo:
                    k = p + 1
                    src, bsrc = uT, None
                    for step in range(1, k + 1):
                        sh = 1 << (step - 1)
                        lo = 1 << step
                        dst, bdst = (s_a, bsa) if step % 2 == 1 else (s_b, bsb)
                        if step == 1:
                            S.op("pool", lambda e, dst=dst, lo=lo, sh=sh: e.tensor_tensor(out=dst[:, :, lo:528], in0=uT[:, :, lo:528], in1=uT[:, :, lo - sh:528 - sh], op=ALU.add),
                                 reads=[b_uh, b_um], writes=[bdst])
                        else:
                            S.op("pool", lambda e, dst=dst, src=src, lo=lo, sh=sh: e.tensor_tensor(out=dst[:, :, lo:528], in0=src[:, :, lo:528], in1=src[:, :, lo - sh:528 - sh], op=ALU.add),
                                 reads=[bsrc], writes=[bdst])
                        src, bsrc = dst, bdst
                    wnd = float(1 << k)
                    if b == HB:
                        other, bother = (s_b, bsb) if src is s_a else (s_a, bsa)
                        for j in range(2):
                            S.op("dve", lambda e, j=j, src=src, other=other: e.tensor_tensor(out=other[:, j, 16:528], in0=src[:, j, 16:528], in1=ic_sb[:], op=ALU.mult),
                                 reads=[bsrc, bcm_], writes=[bother])
                        S.op("dve", lambda e, other=other: e.tensor_tensor(out=pooled[:], in0=other[:, :, 16:528], in1=uT[:, :, 16:528], op=ALU.subtract),
                             reads=[bother, b_um], writes=[bpooled])
                    else:
                        S.op("dve", lambda e, src=src, wnd=wnd: e.scalar_tensor_tensor(out=pooled[:], in0=src[:, :, 16:528], scalar=1.0 / wnd, in1=uT[:, :, 16:528],
                                                                                   op0=ALU.mult, op1=ALU.subtract),
                             reads=[bsrc, b_um], writes=[bpooled])
                    yield
                for hh in range(2):
                    h_ = tmpH[hh]
                    o_ = st_["hd"][hh]
                    lb_ap = small[:, 25 + hh:26 + hh]
                    oml_ap = small[:, 27 + hh:28 + hh]
                    noml_ap = small[:, 29 + hh:30 + hh]
                    ps, bps = ringA.next()
                    mm_group(ps[:], bps, [(win[:, c, 2, hh * 128:(hh + 1) * 128], hT[:, c, :]) for c in range(NCH)], [bwin[2], bhT])
                    S.op("act", lambda e, ps=ps, h_=h_: e.activation(out=h_["sig"][:], in_=ps[:], func=AF.Sigmoid), reads=[bps], writes=[h_["bsig"]])
                    yield
                    ps, bps = ringA.next()
                    mm_group(ps[:], bps, [(win[:, c, 1, hh * 128:(hh + 1) * 128], hT[:, c, :]) for c in range(NCH)], [bwin[1], bhT])
                    S.op("act", lambda e, ps=ps, h_=h_: e.activation(out=h_["q"][:], in_=ps[:], func=AF.Silu), reads=[bps], writes=[h_["bq"]])
                    S.op("act", lambda e, h_=h_, oml_ap=oml_ap, lb_ap=lb_ap: e.activation(out=h_["lf"][:], in_=h_["sig"][:], func=AF.Ln, scale=oml_ap, bias=lb_ap),
                         reads=[h_["bsig"], bsmall], writes=[h_["blf"]])
                    S.op("dve", lambda e, h_=h_: e.tensor_tensor_scan(out=h_["a"][:], data0=rm_sb[:], data1=h_["lf"][:], initial=0.0, op0=ALU.mult, op1=ALU.add),
                         reads=[h_["blf"], bcm_], writes=[h_["ba"]])
                    yield
                    ps, bps = ringA.next()
                    mm_group(ps[:], bps, [(win[:, c, 4, hh * 128:(hh + 1) * 128], hT[:, c, :]) for c in range(NCH)], [bwin[4], bhT])
                    S.op("act", lambda e, ps=ps, hh=hh, gT=gT: e.activation(out=gT[:, hh, :], in_=ps[:], func=AF.Silu), reads=[bps], writes=[bgT[hh]])
                    a3 = h_["a"][:].rearrange("p (c n) -> p c n", c=8)
                    S.op("dve", lambda e, h_=h_, a3=a3: e.tensor_tensor(out=h_["at"][:].rearrange("p (c n) -> p c n", c=8), in0=a3, in1=a3[:, :, 31:32].to_broadcast([128, 8, 64]), op=ALU.subtract),
                         reads=[h_["ba"]], writes=[h_["bat"]])
                    S.op("pool", lambda e, h_=h_, noml_ap=noml_ap, oml_ap=oml_ap: e.tensor_scalar(out=h_["kk"][:], in0=h_["sig"][:], scalar1=noml_ap, scalar2=oml_ap, op0=ALU.mult, op1=ALU.add),
                         reads=[h_["bsig"], bsmall], writes=[h_["bkk"]])
                    yield
                    S.op("act", lambda e, h_=h_: e.activation(out=h_["e1"][:], in_=h_["at"][:], func=AF.Exp), reads=[h_["bat"]], writes=[h_["be1"]])
                    S.op("act", lambda e, h_=h_: e.activation(out=h_["e2"][:], in_=h_["at"][:], func=AF.Exp, scale=-1.0), reads=[h_["bat"]], writes=[h_["be2"]])
                    a16 = h_["a"][:].rearrange("p (c n) -> p c n", n=32)[:, :, 31:32]
                    S.op("act", lambda e, o_=o_, a16=a16: e.activation(out=o_["ex"][:, 0:16].rearrange("p (c n) -> p c n", n=1), in_=a16, func=AF.Exp),
                         reads=[h_["ba"]], writes=[o_["bex"]])
                    S.op("dve", lambda e, o_=o_, a3=a3: e.tensor_tensor(out=o_["ex"][:, 16:24].rearrange("p (c n) -> p c n", n=1), in0=a3[:, :, 63:64], in1=a3[:, :, 31:32], op=ALU.subtract),
                         reads=[h_["ba"]], writes=[o_["bex"]])
                    S.op("act", lambda e, o_=o_: e.activation(out=o_["ex"][:, 24:32], in_=o_["ex"][:, 16:24], func=AF.Exp), reads=[o_["bex"]], writes=[o_["bex"]])
                    yield
                    S.op("dve", lambda e, h_=h_, o_=o_: e.tensor_tensor(out=o_["qt"][:], in0=h_["q"][:], in1=h_["e1"][:], op=ALU.mult),
                         reads=[h_["bq"], h_["be1"]], writes=[o_["bqt"]])
                    S.op("pool", lambda e, h_=h_, o_=o_: e.tensor_tensor(out=o_["ktT"][:], in0=h_["kk"][:], in1=h_["e2"][:], op=ALU.mult),
                         reads=[h_["bkk"], h_["be2"]], writes=[o_["bktT"]])
                    yield
                    psb = PS[PS_KT][:].bitcast(BF16)
                    for t in range(4):
                        S.op("pe", lambda e, o_=o_, t=t, psb=psb: e.transpose(psb[:, t * 128:(t + 1) * 128], o_["ktT"][:, t * 128:(t + 1) * 128], id_b[:]),
                             reads=[o_["bktT"], bconst], writes=[BPS[PS_KT]])
                    S.op("act", lambda e, o_=o_, psb=psb: e.activation(out=o_["ktok"][:].rearrange("p t n -> p (t n)"), in_=psb[:, 0:512], func=AF.Copy),
                         reads=[BPS[PS_KT]], writes=[o_["bktok"]])
                    yield
                if not halo:
                    for oc in range(2):
                        ps, bps = ringA.next()
                        mm_group(ps[:], bps, [(pw[:, ic, oc * 128:(oc + 1) * 128], pooled[:, ic, :]) for ic in range(2)], [bpw, bpooled])
                        S.op("act", lambda e, ps=ps, oc=oc, mixT=mixT: e.activation(out=mixT[:, oc, :], in_=ps[:], func=AF.Copy, scale=small[:, 2 * p + oc:2 * p + oc + 1]),
                             reads=[bps, bsmall], writes=[bmix[oc]])
                    yield

            def back(b):
                halo = b < HB
                st_ = sets[b % 2]
                mixT, bmix = st_["mixT"], st_["bmix"]
                v_sb, bv = st_["v"], st_["bv"]
                gT, bgT = st_["gT"], st_["bgT"]
                for t in range(4):
                    for hh in range(2):
                        h_ = st_["hd"][hh]
                        r_ = rec[hh]
                        S.op("pe", lambda e, h_=h_, t=t: e.matmul(PS[PS_S][:, 0:128], lhsT=h_["ktT"][:, t * 128:(t + 1) * 128], rhs=h_["qt"][:, t * 128:(t + 1) * 128], start=True, stop=True),
                             reads=[h_["bktT"], h_["bqt"]], writes=[BPS[PS_S]])
                        ssb, bssb = r_["ssb4"][t]
                        S.op("dve", lambda e, ssb=ssb: e.copy_predicated(out=ssb[:], mask=bd_sb[:], data=PS[PS_S][:, 0:128]),
                             reads=[BPS[PS_S], bcm_], writes=[bssb])
                        for cc in range(2):
                            c = 2 * t + cc
                            S.op("pe", lambda e, h_=h_, t=t, cc=cc, hh=hh, v_sb=v_sb: e.matmul(PS[PS_ST][:, 0:128], lhsT=h_["ktok"][cc * 64:(cc + 1) * 64, t, :], rhs=v_sb[cc * 64:(cc + 1) * 64, t, hh * 128:(hh + 1) * 128], start=True, stop=True),
                                 reads=[h_["bktok"], bv], writes=[BPS[PS_ST]])
                            tst, btst = r_["tst8"][c]
                            S.op("act", lambda e, h_=h_, tst=tst, c=c: e.activation(out=tst[:], in_=PS[PS_ST][:, 0:128], func=AF.Copy, scale=h_["ex"][:, 24 + c:25 + c]),
                                 reads=[BPS[PS_ST], h_["bex"]], writes=[btst])
                        yield
                for t in range(4):
                    for hh in range(2):
                        h_ = st_["hd"][hh]
                        r_ = rec[hh]
                        pso, bpso = PS[PS_O + hh], BPS[PS_O + hh]
                        ssb, bssb = r_["ssb4"][t]
                        S.op("pe", lambda e, ssb=ssb, t=t, hh=hh, pso=pso, v_sb=v_sb: e.matmul(pso[:, t * 128:(t + 1) * 128], lhsT=v_sb[:, t, hh * 128:(hh + 1) * 128], rhs=ssb[:], start=True, stop=False),
                             reads=[bssb, bv], writes=[bpso])
                        for cc in range(2):
                            c = 2 * t + cc
                            sref, bsref = r_["sref"].next()
                            S.op("pool", lambda e, h_=h_, r_=r_, sref=sref, c=c: e.tensor_scalar(out=sref[:], in0=r_["S"][:], scalar1=h_["ex"][:, 2 * c:2 * c + 1], scalar2=0.0, op0=ALU.mult, op1=ALU.add),
                                 reads=[r_["bS"], h_["bex"]], writes=[bsref])
                            tst, btst = r_["tst8"][c]
                            S.op("pool", lambda e, h_=h_, r_=r_, c=c: e.tensor_scalar(out=r_["S"][:], in0=r_["S"][:], scalar1=h_["ex"][:, 2 * c + 1:2 * c + 2], scalar2=0.0, op0=ALU.mult, op1=ALU.add),
                                 reads=[r_["bS"], h_["bex"]], writes=[r_["bS"]])
                            S.op("pool", lambda e, r_=r_, tst=tst: e.tensor_tensor(out=r_["S"][:], in0=r_["S"][:], in1=tst[:], op=ALU.add),
                                 reads=[r_["bS"], btst], writes=[r_["bS"]])
                            c0 = t * 128 + cc * 64
                            S.op("pe", lambda e, h_=h_, sref=sref, c0=c0, pso=pso, cc=cc: e.matmul(pso[:, c0:c0 + 64], lhsT=sref[:], rhs=h_["qt"][:, c0:c0 + 64], start=False, stop=(cc == 1)),
                                 reads=[bsref, h_["bqt"]], writes=[bpso])
                        yield
                if halo:
                    return
                for hh in range(2):
                    pso, bpso = PS[PS_O + hh], BPS[PS_O + hh]
                    S.op("act", lambda e, pso=pso: e.activation(out=o_sb[:], in_=pso[:], func=AF.Copy), reads=[bpso], writes=[bo])
                    S.op("act", lambda e, pso=pso: e.activation(out=sq_sb[:], in_=pso[:], func=AF.Square), reads=[bpso], writes=[bsq])
                    yield
                    S.op("pe", lambda e: e.matmul(PS[PS_SS][:], lhsT=ones_f[:], rhs=sq_sb[:], start=True, stop=True), reads=[bsq, bconst], writes=[BPS[PS_SS]])
                    S.op("act", lambda e: e.activation(out=rs_sb[:], in_=PS[PS_SS][:], func=AF.Ln, scale=1.0 / 128, bias=eps_t[:]), reads=[BPS[PS_SS], bconst], writes=[brs])
                    S.op("act", lambda e: e.activation(out=rs_sb[:], in_=rs_sb[:], func=AF.Exp, scale=-0.5), reads=[brs], writes=[brs])
                    S.op("dve", lambda e: e.scalar_tensor_tensor(out=o_sb[:], in0=o_sb[:], scalar=small[:, 24:25], in1=rs_sb[:], op0=ALU.mult, op1=ALU.mult),
                         reads=[bo, brs, bsmall], writes=[bo])
                    S.op("dve", lambda e, hh=hh, mixT=mixT, gT=gT: e.tensor_tensor(out=mixT[:, 2 + hh, :], in0=o_sb[:], in1=gT[:, hh, :], op=ALU.mult),
                         reads=[bo, bgT[hh]], writes=[bmix[2 + hh]])
                    yield
                for t in range(4):
                    acc, bacc = accr.next()
                    ti = (b - HB) * 4 + t
                    if p == 0:
                        r0 = b * 512 + t * 128
                        S.dma("sp", lambda e, acc=acc, r0=r0: e.dma_start(out=acc[:], in_=xseg[r0:r0 + 128, :]), writes=[bacc])
                    else:
                        S.dma("sp", lambda e, acc=acc, ti=ti: e.dma_start(out=acc[:], in_=x1s[ti * 128:(ti + 1) * 128, :]), reads=[b_x1[ti]], writes=[bacc])
                    for n in range(4):
                        ps, bps = ringA.next()
                        mm_group(ps[:], bps, [(mixT[:, c, t * 128:(t + 1) * 128], wout[:, c, n * 512:(n + 1) * 512]) for c in range(4)], [bwout[n]] + bmix)
                        S.op("dve", lambda e, acc=acc, ps=ps, n=n: e.tensor_tensor(out=acc[:, n * 512:(n + 1) * 512], in0=ps[:], in1=acc[:, n * 512:(n + 1) * 512], op=ALU.add),
                             reads=[bps, bacc], writes=[bacc])
                        if n % 2 == 1 and not os.environ.get("NOYIELD_OP"):
                            yield
                    S.dma("sp", lambda e, acc=acc, ti=ti: e.dma_start(out=x1s[ti * 128:(ti + 1) * 128, :], in_=acc[:]), reads=[bacc], writes=[b_x1[ti]])
                    if dbg and p == 3:
                        bo_ = Buf()
                        S.dma("sp", lambda e, acc=acc, ti=ti: e.dma_start(out=x1d[ti * 128:(ti + 1) * 128, :], in_=acc[:]), reads=[bacc], writes=[bo_])
                        outs.append(bo_)

            interleave([front(0)])
            for b in range(1, NBM):
                interleave([back(b - 1), front(b)])
            interleave([back(NBM - 1)])
            S.barrier()
            S.emit_all()

    S.mute = limit < 6
    with nc.reset_on_exit():
        KT = sb("KT", [128, NCH, 256], BF16)
        V = sb("V", [128, 2, D], BF16)
        bKT, bV = Buf(), Buf()
        nt = NormT("x1n", [0, 1])
        ringB = psring([2, 3, 4, 5, 6, 7])
        with nc.reset_on_exit():
            nt.load_gain(4)
            mT = sb("mT", [128, NCH, 256], BF16)
            bmT = Buf()
            mts = Ring([(sb("memt%d" % i, [128, D], F32), Buf()) for i in range(2)])
            for mt in range(2):
                xt, bxt = mts.next()
                S.dma("sp", lambda e, xt=xt, mt=mt: e.dma_start(out=xt[:], in_=mem[mt * 128:(mt + 1) * 128, :]), writes=[bxt])
                nt.run(xt[:], bxt, mT[:, :, mt * 128:(mt + 1) * 128], bmT)
            wkr = Ring([(sb("wkv%d" % i, [128, NCH, 512], BF16), Buf()) for i in range(2)])
            for n in range(4):
                wt, bwt = wkr.next()
                S.dma("pool", lambda e, wt=wt, n=n: e.dma_start(out=wt[:], in_=wslice(wk, 0, D, n * 512, (n + 1) * 512)), writes=[bwt])
                for jj in range(4):
                    ps, bps = ringB.next()
                    mm_group(ps[:, 0:256], bps, [(wt[:, c, jj * 128:(jj + 1) * 128], mT[:, c, :]) for c in range(NCH)], [bwt, bmT])
                    evac_copy(KT[:, n * 4 + jj, :], ps[:, 0:256], [bps], [bKT])
            for n in range(4):
                wt, bwt = wkr.next()
                S.dma("pool", lambda e, wt=wt, n=n: e.dma_start(out=wt[:], in_=wslice(wv, 0, D, n * 512, (n + 1) * 512)), writes=[bwt])
                for mt in range(2):
                    ps, bps = ringB.next()
                    mm_group(ps[:], bps, [(mT[:, c, mt * 128:(mt + 1) * 128], wt[:, c, :]) for c in range(NCH)], [bwt, bmT])
                    evac_copy(V[:, mt, n * 512:(n + 1) * 512], ps[:], [bps], [bV])
            S.barrier()
            S.emit_all()
        nt.load_gain(1)
        wq_sb = sb("wq_sb", [128, NCH, D], BF16)
        bwq4 = [Buf() for _ in range(4)]
        for n in range(4):
            S.dma("pool", lambda e, n=n: e.dma_start(out=wq_sb[:, :, n * 512:(n + 1) * 512], in_=wslice(wq, 0, D, n * 512, (n + 1) * 512)), writes=[bwq4[n]])
        xts = Ring([(sb("x1x%d" % i, [128, D], F32), Buf()) for i in range(2)])
        h2T = sb("h2T", [128, NCH, 512], BF16)
        bh2 = Buf()
        qTs = [(sb("qT%d" % i, [128, NCH, 512], BF16), Buf()) for i in range(2)]
        aT, baT = sb("aT0", [128, NCH, 512], BF16), Buf()
        attr = Ring([(sb("pex%d" % i, [128, 256], F32), sb("pn%d" % i, [128, 256], BF16), sb("PT%d" % i, [128, 2, 128], BF16), sb("sst%d" % i, [128, 8], F32),
                      Buf(), Buf(), Buf(), Buf()) for i in range(3)])
        SC = 512 ** -0.5

        def x1_norm(b):
            for t in range(4):
                xt, bxt = xts.next()
                ti = b * 4 + t
                S.dma("sp", lambda e, xt=xt, ti=ti: e.dma_start(out=xt[:], in_=x1s[ti * 128:(ti + 1) * 128, :]), reads=[b_x1[ti]], writes=[bxt])
                nt.run(xt[:], bxt, h2T[:, :, t * 128:(t + 1) * 128], bh2)

        def x1_qgroup(b, j):
            qT, bqT = qTs[b % 2]
            ps, bps = ringB.next()
            mm_group(ps[:], bps, [(wq_sb[:, c, j * 128:(j + 1) * 128], h2T[:, c, :]) for c in range(NCH)], [bwq4[j // 4], bh2])
            evac_copy(qT[:, j, :], ps[:], [bps], [bqT])

        def x1_attnA(b, t, h):
            qT, bqT = qTs[b % 2]
            pex, pn, PT, sst, bpex, bpn, bPT, bsst = attr.next()
            ps, bps = ringB.next()
            mm_group(ps[:, 0:256], bps, [(qT[:, 4 * h + c4, t * 128:(t + 1) * 128], KT[:, 4 * h + c4, :]) for c4 in range(4)], [bqT, bKT])
            S.op("dve", lambda e, ps=ps, sst=sst: e.tensor_reduce(out=sst[:, 0:1], in_=ps[:, 0:256], axis=AX.X, op=ALU.max), reads=[bps], writes=[bsst])
            S.op("dve", lambda e, sst=sst: e.tensor_scalar(out=sst[:, 1:2], in0=sst[:, 0:1], scalar1=-SC, scalar2=None, op0=ALU.mult), reads=[bsst], writes=[bsst])
            S.op("act", lambda e, ps=ps, sst=sst, pex=pex: e.activation(out=pex[:], in_=ps[:, 0:256], func=AF.Exp, scale=SC, bias=sst[:, 1:2], accum_out=sst[:, 2:3]),
                 reads=[bps, bsst], writes=[bpex, bsst])
            S.op("dve", lambda e, sst=sst: e.reciprocal(out=sst[:, 3:4], in_=sst[:, 2:3]), reads=[bsst], writes=[bsst])
            S.op("dve", lambda e, sst=sst, pex=pex, pn=pn: e.tensor_scalar(out=pn[:], in0=pex[:], scalar1=sst[:, 3:4], scalar2=None, op0=ALU.mult), reads=[bpex, bsst], writes=[bpn])
            return (t, h, pn, bpn, PT, bPT)

        def x1_attnB(st):
            t, h, pn, bpn, PT, bPT = st
            ps2, bps2 = ringB.next()
            psb = ps2[:].bitcast(BF16)
            for mc in range(2):
                S.op("pe", lambda e, psb=psb, mc=mc, pn=pn: e.transpose(psb[:, mc * 128:(mc + 1) * 128], pn[:, mc * 128:(mc + 1) * 128], id_b[:]),
                     reads=[bpn, bconst], writes=[bps2])
            S.op("act", lambda e, psb=psb, PT=PT: e.activation(out=PT[:].rearrange("p c n -> p (c n)"), in_=psb[:, 0:256], func=AF.Copy), reads=[bps2], writes=[bPT])
            ps3, bps3 = ringB.next()
            for j in range(4):
                mm_group(ps3[:, j * 128:(j + 1) * 128], bps3,
                         [(V[:, mc, h * 512 + j * 128:h * 512 + (j + 1) * 128], PT[:, mc, :]) for mc in range(2)], [bV, bPT])
            evac_copy(aT[:, 4 * h:4 * h + 4, t * 128:(t + 1) * 128], ps3[:].rearrange("p (c n) -> p c n", c=4), [bps3], [baT])

        x1_norm(0)
        for j in range(NCH):
            x1_qgroup(0, j)
        for b in range(NB):
            if b + 1 < NB:
                x1_norm(b + 1)
            stA = x1_attnA(b, 0, 0)
            for k in range(16):
                stN = x1_attnA(b, (k + 1) // 4, (k + 1) % 4) if k + 1 < 16 else None
                if b + 1 < NB:
                    x1_qgroup(b + 1, k)
                x1_attnB(stA)
                stA = stN
            S.dma("sp", lambda e, b=b: e.dma_start(out=aTs[b], in_=aT[:].rearrange("p c n -> p (c n)")), reads=[baT], writes=[b_aT[b]])
        S.barrier()
        S.emit_all()

    S.mute = limit < 6.5
    with nc.reset_on_exit():
        nt = NormT("x2n", [0, 1, 2, 3])
        nt.load_gain(2)
        ringB = psring([4, 5, 6])
        wo_sb = sb("wo_sb", [128, NCH, D], BF16)
        bwo = Buf()
        for n in range(4):
            S.dma("pool", lambda e, n=n: e.dma_start(out=wo_sb[:, :, n * 512:(n + 1) * 512], in_=wslice(wo, 0, D, n * 512, (n + 1) * 512)), writes=[bwo])
        wr_sb = sb("wr_sb", [128, NCH, 20], F32)
        bwr = Buf()
        S.dma("sp", lambda e: e.dma_start(out=wr_sb[:].rearrange("p c n -> p (c n)"), in_=wr), writes=[bwr])
        aTr = Ring([(sb("aTi%d" % i, [128, NCH, 512], BF16), Buf()) for i in range(2)])
        xts = Ring([(sb("x2x%d" % i, [128, D], F32), Buf()) for i in range(3)])
        h3r = Ring([(sb("h3b%d" % i, [128, NCH, 512], BF16), Buf()) for i in range(2)])
        h3fr = Ring([(sb("h3f%d" % i, [128, NCH, 128], F32), Buf()) for i in range(2)])
        rtr = Ring([(sb("rt%d" % i, [128, 128], F32), Buf()) for i in range(2)])
        cmr = Ring([(sb("cmb%d" % i, [128, 16], F32), Buf()) for i in range(2)])
        def x2_normrouter(b, t, ti, xt, bxt, h3b, bh3b):
            h3f, bh3f = h3fr.next()
            rt, brt = rtr.next()
            nt.run(xt[:], bxt, h3b[:, :, t * 128:(t + 1) * 128], bh3b, h3f, bh3f)
            if limit == 6.5:
                S.mute = True
            psr, bpsr = PS[7], BPS[7]
            mm_group(psr[:, 0:20], bpsr, [(h3f[:, c, :], wr_sb[:, c, :]) for c in range(NCH)], [bh3f, bwr])
            cm, bcm = cmr.next()
            R_ = rt
            S.op("act", lambda e, R_=R_: e.activation(out=R_[:, 0:20], in_=psr[:, 0:20], func=AF.Copy), reads=[bpsr], writes=[brt])
            S.op("dve", lambda e, R_=R_: e.tensor_reduce(out=R_[:, 20:21], in_=R_[:, 0:4], axis=AX.X, op=ALU.max), reads=[brt], writes=[brt])
            S.op("dve", lambda e, R_=R_: e.tensor_scalar(out=R_[:, 21:22], in0=R_[:, 20:21], scalar1=-1.0, scalar2=None, op0=ALU.mult), reads=[brt], writes=[brt])
            S.op("act", lambda e, R_=R_: e.activation(out=R_[:, 24:28], in_=R_[:, 0:4], func=AF.Exp, bias=R_[:, 21:22], accum_out=R_[:, 22:23]), reads=[brt], writes=[brt])
            S.op("dve", lambda e, R_=R_: e.reciprocal(out=R_[:, 23:24], in_=R_[:, 22:23]), reads=[brt], writes=[brt])
            S.op("dve", lambda e, R_=R_: e.tensor_scalar(out=R_[:, 24:28], in0=R_[:, 0:4], scalar1=R_[:, 20:21], scalar2=None, op0=ALU.is_equal), reads=[brt], writes=[brt])
            S.op("dve", lambda e, R_=R_: e.tensor_scalar(out=R_[:, 24:28], in0=R_[:, 24:28], scalar1=-1.0, scalar2=1e30, op0=ALU.add, op1=ALU.mult), reads=[brt], writes=[brt])
            S.op("dve", lambda e, R_=R_: e.tensor_tensor(out=R_[:, 32:48].rearrange("p (g k) -> p g k", g=4), in0=R_[:, 4:20].rearrange("p (g k) -> p g k", g=4),
                                                  in1=R_[:, 24:28].rearrange("p (g k) -> p g k", k=1).to_broadcast([128, 4, 4]), op=ALU.add), reads=[brt], writes=[brt])
            S.op("dve", lambda e, R_=R_: e.max(out=R_[:, 48:56], in_=R_[:, 32:48]), reads=[brt], writes=[brt])
            S.op("dve", lambda e, R_=R_: e.tensor_tensor(out=R_[:, 56:57], in0=R_[:, 49:50], in1=R_[:, 48:49], op=ALU.subtract), reads=[brt], writes=[brt])
            S.op("act", lambda e, R_=R_: e.activation(out=R_[:, 57:58], in_=R_[:, 56:57], func=AF.Exp), reads=[brt], writes=[brt])
            S.op("dve", lambda e, R_=R_: e.tensor_scalar(out=R_[:, 58:59], in0=R_[:, 57:58], scalar1=1.0, scalar2=None, op0=ALU.add), reads=[brt], writes=[brt])
            S.op("dve", lambda e, R_=R_: e.reciprocal(out=R_[:, 59:60], in_=R_[:, 58:59]), reads=[brt], writes=[brt])
            S.op("dve", lambda e, R_=R_: e.tensor_tensor(out=R_[:, 60:61], in0=R_[:, 57:58], in1=R_[:, 59:60], op=ALU.mult), reads=[brt], writes=[brt])
            S.op("dve", lambda e, R_=R_: e.tensor_scalar(out=R_[:, 61:63], in0=R_[:, 59:61], scalar1=R_[:, 23:24], scalar2=None, op0=ALU.mult), reads=[brt], writes=[brt])
            S.op("dve", lambda e, R_=R_: e.tensor_scalar(out=R_[:, 64:80], in0=R_[:, 32:48], scalar1=R_[:, 48:49], scalar2=R_[:, 61:62], op0=ALU.is_equal, op1=ALU.mult), reads=[brt], writes=[brt])
            S.op("dve", lambda e, R_=R_: e.tensor_scalar(out=R_[:, 80:96], in0=R_[:, 32:48], scalar1=R_[:, 49:50], scalar2=R_[:, 62:63], op0=ALU.is_equal, op1=ALU.mult), reads=[brt], writes=[brt])
            S.op("dve", lambda e, cm=cm, R_=R_: e.tensor_tensor(out=cm[:], in0=R_[:, 64:80], in1=R_[:, 80:96], op=ALU.add), reads=[brt], writes=[bcm])
            S.dma("sp", lambda e, cm=cm, ti=ti: e.dma_start(out=combs[ti * 128:(ti + 1) * 128, :], in_=cm[:]), reads=[bcm], writes=[b_comb[ti]])
            if limit == 6.5:
                S.mute = False
            if t == 3:
                S.dma("sp", lambda e, h3b=h3b, b=b: e.dma_start(out=h3Ts[b], in_=h3b[:].rearrange("p c n -> p (c n)")), reads=[bh3b], writes=[b_h3T[b]])

        pend = None
        for b in range(NB):
            aT, baT = aTr.next()
            S.dma("sp", lambda e, aT=aT, b=b: e.dma_start(out=aT[:].rearrange("p c n -> p (c n)"), in_=aTs[b]), reads=[b_aT[b]], writes=[baT])
            h3b, bh3b = h3r.next()
            for t in range(4):
                ti = b * 4 + t
                xt, bxt = xts.next()
                S.dma("sp", lambda e, xt=xt, ti=ti: e.dma_start(out=xt[:], in_=x1s[ti * 128:(ti + 1) * 128, :]), reads=[b_x1[ti]], writes=[bxt])
                for n in range(4):
                    ps, bps = ringB.next()
                    mm_group(ps[:], bps, [(aT[:, c, t * 128:(t + 1) * 128], wo_sb[:, c, n * 512:(n + 1) * 512]) for c in range(NCH)], [bwo, baT])
                    S.op("dve", lambda e, xt=xt, ps=ps, n=n: e.tensor_tensor(out=xt[:, n * 512:(n + 1) * 512], in0=ps[:], in1=xt[:, n * 512:(n + 1) * 512], op=ALU.add),
                         reads=[bps, bxt], writes=[bxt])
                S.dma("sp", lambda e, xt=xt, ti=ti: e.dma_start(out=x2s[ti * 128:(ti + 1) * 128, :], in_=xt[:]), reads=[bxt], writes=[b_x2[ti]])
                if dbg:
                    bo_ = Buf()
                    S.dma("sp", lambda e, xt=xt, ti=ti: e.dma_start(out=x2d[ti * 128:(ti + 1) * 128, :], in_=xt[:]), reads=[bxt], writes=[bo_])
                    outs.append(bo_)
                if pend is not None:
                    x2_normrouter(*pend)
                pend = (b, t, ti, xt, bxt, h3b, bh3b)
        x2_normrouter(*pend)
        S.barrier()
        S.emit_all()

    S.mute = limit < 8
    with nc.reset_on_exit():
        h3T = sb("e_h3T", [128, NCH, EB], BF16)
        bh3 = Buf()
        y = sb("e_y", [128, ETI, D], F32)
        by = [Buf() for _ in range(ETI)]
        wring = Ring([(sb("e_w%d" % i, [128, 8192], BF16), Buf()) for i in range(5)])
        cmb = sb("e_cmb", [128, ETI, 16], F32)
        bcmb = Buf()
        sgr = Ring([(sb("e_sg%d" % i, [128, 512], F32), Buf()) for i in range(2)])
        hidr = Ring([(sb("e_hid%d" % i, [128, 512], BF16), Buf()) for i in range(2)])
        hTr_ = Ring([(sb("e_hT%d" % i, [128, 4, 128], BF16), Buf()) for i in range(2)])
        xts = Ring([(sb("e_x%d" % i, [128, D], F32), Buf()) for i in range(1)])
        gB = sb("e_gB", [128, D], F32)
        fst = sb("e_fst", [128, 4], F32)
        bjunk, bgB, bfst = Buf(), Buf(), Buf()
        S.dma("sp", lambda e: e.dma_start(out=gB[:], in_=gains[3:4, :].to_broadcast([128, D])), writes=[bgB])
        gur = Ring([((PS[0], BPS[0]), (PS[1], BPS[1])), ((PS[2], BPS[2]), (PS[3], BPS[3]))])
        ringY = psring([5, 6, 7])
        PS_T = 4
        pending_fn = None
        for eb in range(T // EB):
            for half in range(EB // 512):
                bi = eb * (EB // 512) + half
                S.dma("sp", lambda e, half=half, bi=bi: e.dma_start(out=h3T[:, :, half * 512:(half + 1) * 512], in_=h3Ts[bi].rearrange("p (c n) -> p c n", c=NCH)),
                      reads=[b_h3T[bi]], writes=[bh3])
            for i in range(ETI):
                ti = eb * ETI + i
                S.dma("sp", lambda e, i=i, ti=ti: e.dma_start(out=cmb[:, i, :], in_=combs[ti * 128:(ti + 1) * 128, :]), reads=[b_comb[ti]], writes=[bcmb])
            for ex in range(16):
                wg, bwg = wring.next()
                S.dma("pool", lambda e, wg=wg, ex=ex: e.dma_start(out=wg[:].rearrange("p (c n) -> p c n", c=NCH), in_=w_gate[ex].rearrange("(c p) n -> p c n", p=128)), writes=[bwg])
                wu, bwu = wring.next()
                S.dma("pool", lambda e, wu=wu, ex=ex: e.dma_start(out=wu[:].rearrange("p (c n) -> p c n", c=NCH), in_=w_up[ex].rearrange("(c p) n -> p c n", p=128)), writes=[bwu])
                wd, bwd = wring.next()
                for n in range(4):
                    S.dma("pool", lambda e, wd=wd, ex=ex, n=n: e.dma_start(out=wd[:].rearrange("p (c n) -> p c n", c=4)[:, :, n * 512:(n + 1) * 512],
                                                                    in_=w_down[ex][:, n * 512:(n + 1) * 512].rearrange("(c p) n -> p c n", p=128)), writes=[bwd])
                wg3 = wg[:].rearrange("p (c n) -> p c n", c=NCH)
                wu3 = wu[:].rearrange("p (c n) -> p c n", c=NCH)
                wd3 = wd[:].rearrange("p (c n) -> p c n", c=4)

                def GUg(i):
                    (pg, bpg), (pu, bpu) = gur.next()
                    mm_group(pg[:], bpg, [(h3T[:, c, i * 128:(i + 1) * 128], wg3[:, c, :]) for c in range(NCH)], [bh3, bwg])
                    return (pg, bpg, pu, bpu)

                def GUu(i, gu):
                    pg, bpg, pu, bpu = gu
                    mm_group(pu[:], bpu, [(h3T[:, c, i * 128:(i + 1) * 128], wu3[:, c, :]) for c in range(NCH)], [bh3, bwu])

                def HID(i, gu, ex=ex):
                    pg, bpg, pu, bpu = gu
                    sg, bsg = sgr.next()
                    S.op("act", lambda e: e.activation(out=sg[:], in_=pg[:], func=AF.Silu), reads=[bpg], writes=[bsg])
                    hid, bhid = hidr.next()
                    S.op("dve", lambda e: e.scalar_tensor_tensor(out=hid[:], in0=sg[:], scalar=cmb[:, i, ex:ex + 1], in1=pu[:], op0=ALU.mult, op1=ALU.mult),
                         reads=[bsg, bpu, bcmb], writes=[bhid])
                    return hid, bhid

                def TR(i, hb):
                    hid, bhid = hb
                    psb = PS[PS_T][:].bitcast(BF16)
                    for c4 in range(4):
                        S.op("pe", lambda e, c4=c4: e.transpose(psb[:, c4 * 128:(c4 + 1) * 128], hid[:, c4 * 128:(c4 + 1) * 128], id_b[:]),
                             reads=[bhid, bconst], writes=[BPS[PS_T]])
                    hT_, bhT_ = hTr_.next()
                    S.op("act", lambda e: e.activation(out=hT_[:].rearrange("p c n -> p (c n)"), in_=psb[:, 0:512], func=AF.Copy), reads=[BPS[PS_T]], writes=[bhT_])
                    return hT_, bhT_

                def DOWN(i, hb, ex=ex):
                    hT_, bhT_ = hb
                    for n in range(4):
                        ps, bps = ringY.next()
                        mm_group(ps[:], bps, [(hT_[:, c4, :], wd3[:, c4, n * 512:(n + 1) * 512]) for c4 in range(4)], [bhT_, bwd])
                        if ex == 0:
                            S.op("dve", lambda e, ps=ps, n=n: e.tensor_copy(out=y[:, i, n * 512:(n + 1) * 512], in_=ps[:]), reads=[bps], writes=[by[i]])
                        else:
                            S.op("dve", lambda e, ps=ps, n=n: e.tensor_tensor(out=y[:, i, n * 512:(n + 1) * 512], in0=ps[:], in1=y[:, i, n * 512:(n + 1) * 512], op=ALU.add),
                                 reads=[bps, by[i]], writes=[by[i]])

                g_cur = GUg(0)
                GUu(0, g_cur)
                for i in range(ETI):
                    hb = HID(i, g_cur)
                    g_next = GUg(i + 1) if i + 1 < ETI else None
                    if ex == 0 and pending_fn is not None:
                        pending_fn(i)
                    tb = TR(i, hb)
                    if g_next is not None:
                        GUu(i + 1, g_next)
                    DOWN(i, tb)
                    g_cur = g_next
                if ex == 0:
                    pending_fn = None

            def final_norm(i, eb=eb):
                ti = eb * ETI + i
                xt, bxt = xts.next()
                S.dma("sp", lambda e, xt=xt, ti=ti: e.dma_start(out=xt[:], in_=x2s[ti * 128:(ti + 1) * 128, :]), reads=[b_x2[ti]], writes=[bxt])
                S.op("dve", lambda e, xt=xt, i=i: e.tensor_tensor(out=y[:, i, :], in0=y[:, i, :], in1=xt[:], op=ALU.add), reads=[bxt, by[i]], writes=[by[i]])
                S.op("act", lambda e, i=i, xt=xt: e.activation(out=xt[:], in_=y[:, i, :], func=AF.Square, accum_out=fst[:, 0:1]), reads=[by[i]], writes=[bxt, bfst])
                S.op("act", lambda e: e.activation(out=fst[:, 1:2], in_=fst[:, 0:1], func=AF.Sqrt, scale=1.0 / D, bias=eps_t[:]), reads=[bfst, bconst], writes=[bfst])
                S.op("dve", lambda e: e.reciprocal(out=fst[:, 2:3], in_=fst[:, 1:2]), reads=[bfst], writes=[bfst])
                S.op("dve", lambda e, xt=xt, i=i: e.scalar_tensor_tensor(out=xt[:], in0=y[:, i, :], scalar=fst[:, 2:3], in1=gB[:], op0=ALU.mult, op1=ALU.mult),
                     reads=[by[i], bfst, bgB], writes=[bxt])
                bo_ = Buf()
                S.dma("sp", lambda e, xt=xt, ti=ti: e.dma_start(out=out[ti * 128:(ti + 1) * 128, :], in_=xt[:]), reads=[bxt], writes=[bo_])
                outs.append(bo_)

            if eb + 1 < T // EB:
                pending_fn = final_norm
            else:
                for i in range(ETI):
                    final_norm(i)
        S.mute = False
        S.final_wait("sp", outs)
        S.barrier()
        S.emit_all()
    return nc


_CACHE = {}


def _consts():
    s = np.arange(128)[:, None]
    t = np.arange(128)[None, :]
    bd = ((s // 64 == t // 64) & (s <= t)).astype(np.uint32)
    rm = np.ones((128, 512), np.float32)
    rm[:, ::64] = 0.0
    return np.eye(128, dtype=np.float32), bd, rm


def run(inputs, dbg=False, limit=99):
    x = np.asarray(inputs["x"], np.float32)
    B, SEQ, _ = x.shape
    SEG = 4
    T = SEQ // SEG
    W = HALO
    key = (T, W, dbg, limit)
    if key not in _CACHE:
        _CACHE[key] = build(T, W, dbg, limit)
    nc = _CACHE[key]
    f = lambda k: np.ascontiguousarray(np.asarray(inputs[k], np.float32))
    ident, bd, rm = _consts()
    gains = np.stack([f("norm_mix")[0], f("norm_xattn")[0], f("norm_ffn")[0], f("norm_final"), f("norm_mem")[0]], 0)
    pscale = np.ascontiguousarray(f("pool_scale")[0].reshape(8, 128).T)
    lbl = np.ascontiguousarray(f("hgrn_lb_logits").reshape(2, 8, 128).transpose(2, 0, 1).reshape(128, 16))
    hnorm = np.ascontiguousarray(f("hgrn_norm")[0].reshape(128, 1))
    wr = np.concatenate([f("router_group")[0], f("router_expert")[0]], axis=1)
    wr = np.ascontiguousarray(wr.reshape(NCH, 128, 20).transpose(1, 0, 2).reshape(128, NCH * 20))
    shared = {
        "w_in": f("w_in")[0], "pool_w": f("pool_w")[0], "w_out": f("w_out")[0],
        "wq": f("xattn_wq")[0], "wk": f("xattn_wk")[0], "wv": f("xattn_wv")[0], "wo": f("xattn_wo")[0],
        "wr": wr, "w_gate": f("w_gate")[0], "w_up": f("w_up")[0], "w_down": f("w_down")[0],
        "gains": np.ascontiguousarray(gains), "pscale": pscale, "lbl": lbl, "hnorm": hnorm,
        "ident": ident, "bdmask": bd, "rmask": rm,
    }
    memf = f("mem")
    in_maps = []
    for c in range(B * SEG):
        b, j = divmod(c, SEG)
        seg = np.zeros((W + T, D), np.float32)
        if j == 0:
            seg[W:] = x[b, 0:T]
        else:
            seg[:] = x[b, j * T - W:(j + 1) * T]
        ic = np.empty((4, 512), np.float32)
        for g, w_ in enumerate((2, 4, 8, 16)):
            if j == 0:
                ic[g] = 1.0 / np.minimum(np.arange(512) + 1, w_)
            else:
                ic[g] = 1.0 / w_
        m = dict(shared)
        m["xseg"] = seg
        m["mem"] = np.ascontiguousarray(memf[b])
        m["invcnt"] = np.ascontiguousarray(np.broadcast_to(ic.reshape(1, 4 * 512), (128, 4 * 512)))
        in_maps.append(m)
    res = run_bass_kernel_spmd(nc, in_maps, core_ids=list(range(B * SEG)))
    names = ["out"] + (["x1d", "x2d"] if dbg else [])
    outd = {}
    for nm in names:
        o = np.empty((B, SEQ, D), np.float32)
        for c in range(B * SEG):
            b, j = divmod(c, SEG)
            o[b, j * T:(j + 1) * T] = np.asarray(res.results[c][nm])
        outd[nm] = o
    return outd


def kernel(**inputs):
    return run(inputs)["out"]
```

```python
import os
import numpy as np
import concourse.bass as bass
import concourse.mybir as mybir
from concourse.bass_utils import run_bass_kernel_spmd

F32 = mybir.dt.float32
BF16 = mybir.dt.bfloat16
U32 = mybir.dt.uint32
AF = mybir.ActivationFunctionType
ALU = mybir.AluOpType
AX = mybir.AxisListType

ENGS = ["pe", "act", "dve", "pool", "sp"]
N_DMA_SEMS = 32
D = 2048
NCH = 16
EPS = 1e-6
HALO = 512


class Buf:
    __slots__ = ("name", "w", "r")

    def __init__(self, name=""):
        self.name = name
        self.w = {}
        self.r = {}


class Sched:
    def __init__(self, nc):
        self.nc = nc
        self.prog = {e: [] for e in ENGS}
        self.cnt = {e: 0 for e in ENGS}
        self.seen = {e: {} for e in ENGS}
        self.sem = {e: nc.alloc_semaphore("sem_" + e) for e in ENGS}
        self.dsem = [nc.alloc_semaphore("dsem%d" % i) for i in range(N_DMA_SEMS)]
        self.dcnt = [0] * N_DMA_SEMS
        self.dnext2 = [0, 0]
        self.mute = False
        self.touched = set()

    def _deps(self, reads, writes):
        deps = {}
        for b in reads:
            for k, n in b.w.items():
                if deps.get(k, 0) < n:
                    deps[k] = n
        for b in writes:
            for d in (b.w, b.r):
                for k, n in d.items():
                    if deps.get(k, 0) < n:
                        deps[k] = n
        return deps

    def _waits(self, eng, deps):
        waits = []
        seen = self.seen[eng]
        for k, n in deps.items():
            if k == "pe" and eng == "pe":
                continue
            if seen.get(k, 0) < n:
                seen[k] = n
                waits.append((k, n))
        return waits

    def _semof(self, k):
        if isinstance(k, tuple):
            return self.dsem[k[1]], 16
        return self.sem[k], 1

    def op(self, eng, emit, reads=(), writes=()):
        if self.mute:
            return
        deps = self._deps(reads, writes)
        waits = self._waits(eng, deps)
        self.cnt[eng] += 1
        my = self.cnt[eng]
        self.prog[eng].append((waits, emit, self.sem[eng], 1))
        self.touched.update(reads)
        self.touched.update(writes)
        for b in writes:
            b.w = {eng: my}
            b.r = {}
        for b in reads:
            if b.r.get(eng, 0) < my:
                b.r[eng] = my

    def dma(self, eng, emit, reads=(), writes=()):
        if self.mute:
            return
        half = N_DMA_SEMS // 2
        q = 1 if eng == "pool" else 0
        i = q * half + self.dnext2[q]
        self.dnext2[q] = (self.dnext2[q] + 1) % half
        key = ("dma", i)
        deps = self._deps(reads, writes)
        if self.dcnt[i] > 0:
            deps[key] = max(deps.get(key, 0), self.dcnt[i])
        waits = self._waits(eng, deps)
        self.dcnt[i] += 1
        my = self.dcnt[i]
        self.prog[eng].append((waits, emit, self.dsem[i], 16))
        self.touched.update(reads)
        self.touched.update(writes)
        for b in writes:
            b.w = {key: my}
            b.r = {}
        for b in reads:
            if b.r.get(key, 0) < my:
                b.r[key] = my

    def final_wait(self, eng, bufs):
        deps = {}
        for b in bufs:
            for k, n in b.w.items():
                deps[k] = max(deps.get(k, 0), n)
        waits = self._waits(eng, deps)
        self.prog[eng].append((waits, None, None, 0))

    def barrier(self):
        deps = {e: self.cnt[e] for e in ENGS if self.cnt[e] > 0}
        for i in range(N_DMA_SEMS):
            if self.dcnt[i] > 0:
                deps[("dma", i)] = self.dcnt[i]
        for eng in ENGS:
            waits = []
            seen = self.seen[eng]
            for k, n in deps.items():
                if k == eng:
                    continue
                if seen.get(k, 0) < n:
                    seen[k] = n
                    waits.append((k, n))
            self.prog[eng].append((waits, None, None, 0))

    def emit_all(self):
        nc = self.nc

        def run(e, name):
            for waits, emit, sem, inc in self.prog[name]:
                for k, n in waits:
                    s, mult = self._semof(k)
                    e.wait_ge(s, n * mult)
                if emit is not None:
                    emit(e).then_inc(sem, inc)

        with nc.Block() as block:
            @block.tensor
            def _(e):
                run(e, "pe")

            @block.scalar
            def _(e):
                run(e, "act")

            @block.vector
            def _(e):
                run(e, "dve")

            @block.gpsimd
            def _(e):
                run(e, "pool")

            @block.sync
            def _(e):
                run(e, "sp")
        self.prog = {e: [] for e in ENGS}
        for b in self.touched:
            b.w = {}
            b.r = {}
        self.touched = set()
        self.cnt = {e: 0 for e in ENGS}
        self.seen = {e: {} for e in ENGS}
        self.dcnt = [0] * N_DMA_SEMS


class Ring:
    def __init__(self, items):
        self.items = items
        self.i = 0

    def next(self):
        it = self.items[self.i % len(self.items)]
        self.i += 1
        return it


def build(T, W, dbg=False, limit=99):
    nc = bass.Bass("TRN2", target_bir_lowering=False)
    S = Sched(nc)
    NBM = (W + T) // 512
    HB = W // 512
    NB = T // 512
    EB = 1024 if T % 1024 == 0 else 512
    ETI = EB // 128

    uid = [0]

    def sb(name, shape, dt):
        uid[0] += 1
        return nc.alloc_sbuf_tensor("%s_%d" % (name, uid[0]), shape, dt)

    def din(name, shape, dt=F32):
        return nc.dram_tensor(name, list(shape), dt, kind="ExternalInput").ap()

    xseg = din("xseg", [W + T, D])
    mem = din("mem", [256, D])
    w_in = din("w_in", [D, 5120])
    pool_w = din("pool_w", [4, 256, 256])
    w_out = din("w_out", [D, D])
    wq = din("wq", [D, D])
    wk = din("wk", [D, D])
    wv = din("wv", [D, D])
    wo = din("wo", [D, D])
    wr = din("wr", [128, NCH * 20])
    w_gate = din("w_gate", [16, D, 512])
    w_up = din("w_up", [16, D, 512])
    w_down = din("w_down", [16, 512, D])
    gains = din("gains", [5, D])
    pscale = din("pscale", [128, 8])
    lbl = din("lbl", [128, 16])
    hnorm = din("hnorm", [128, 1])
    ident = din("ident", [128, 128])
    bdmask = din("bdmask", [128, 128], U32)
    rmask = din("rmask", [128, 512])
    invcnt = din("invcnt", [128, 4 * 512])
    out = nc.dram_tensor("out", [T, D], F32, kind="ExternalOutput").ap()
    if dbg:
        x1d = nc.dram_tensor("x1d", [T, D], F32, kind="ExternalOutput").ap()
        x2d = nc.dram_tensor("x2d", [T, D], F32, kind="ExternalOutput").ap()

    hT1 = nc.dram_tensor("hT1", [NBM, 128, NCH * 512], BF16).ap()
    x1s = nc.dram_tensor("x1s", [T, D], F32).ap()
    aTs = nc.dram_tensor("aTs", [NB, 128, NCH * 512], BF16).ap()
    x2s = nc.dram_tensor("x2s", [T, D], F32).ap()
    h3Ts = nc.dram_tensor("h3Ts", [NB, 128, NCH * 512], BF16).ap()
    combs = nc.dram_tensor("combs", [T, 16], F32).ap()
    b_hT1 = [Buf() for _ in range(NBM)]
    b_x1 = [Buf() for _ in range(T // 128)]
    b_aT = [Buf() for _ in range(NB)]
    b_x2 = [Buf() for _ in range(T // 128)]
    b_h3T = [Buf() for _ in range(NB)]
    b_comb = [Buf() for _ in range(T // 128)]
    outs = []

    PS = [nc.alloc_psum_tensor("ps%d" % i, [128, 512], F32) for i in range(8)]
    BPS = [Buf("ps%d" % i) for i in range(8)]

    def psring(idx):
        return Ring([(PS[i], BPS[i]) for i in idx])

    id_f = sb("id_f", [128, 128], F32)
    id_b = sb("id_b", [128, 128], BF16)
    ones_f = sb("ones_f", [128, 128], F32)
    eps_t = sb("eps_t", [128, 1], F32)
    bconst = Buf("const")
    S.dma("sp", lambda e: e.dma_start(out=id_f[:], in_=ident), writes=[bconst])
    S.dma("pool", lambda e: e.dma_start(out=id_b[:], in_=ident), writes=[bconst])
    S.op("dve", lambda e: e.memset(ones_f[:], 1.0), writes=[bconst])
    S.op("dve", lambda e: e.memset(eps_t[:], EPS), writes=[bconst])

    flip = [0]

    def evac_copy(dst, src, reads, writes):
        flip[0] ^= 1
        if flip[0]:
            S.op("act", lambda e: e.activation(out=dst, in_=src, func=AF.Copy), reads=reads, writes=writes)
        else:
            S.op("dve", lambda e: e.tensor_copy(out=dst, in_=src), reads=reads, writes=writes)

    def mm_group(ps_ap, bps, pairs, reads):
        n = len(pairs)
        for i, (l, r) in enumerate(pairs):
            S.op("pe", lambda e, l=l, r=r, i=i: e.matmul(ps_ap, lhsT=l, rhs=r, start=(i == 0), stop=(i == n - 1)),
                 reads=reads, writes=[bps])

    class NormT:
        def __init__(self, tag, banks):
            self.tmp = Ring([(sb(tag + "junk%d" % i, [128, D], BF16), sb(tag + "xn%d" % i, [128, D], F32), sb(tag + "st%d" % i, [128, 4], F32),
                              Buf(), Buf(), Buf()) for i in range(2)])
            self.gB = sb(tag + "gB", [128, D], F32)
            self.bg = Buf()
            self.ring = psring(banks)

        def load_gain(self, row):
            S.dma("sp", lambda e: e.dma_start(out=self.gB[:], in_=gains[row:row + 1, :].to_broadcast([128, D])), writes=[self.bg])

        def run(self, src, bsrc, dst_bf, bdst, dst_f=None, bdst_f=None):
            junk, xn, st, bj, bxn, bst = self.tmp.next()
            S.op("act", lambda e: e.activation(out=junk[:], in_=src, func=AF.Square, accum_out=st[:, 0:1]),
                 reads=[bsrc], writes=[bj, bst])
            S.op("act", lambda e: e.activation(out=st[:, 1:2], in_=st[:, 0:1], func=AF.Sqrt, scale=1.0 / D, bias=eps_t[:]),
                 reads=[bst, bconst], writes=[bst])
            S.op("dve", lambda e: e.reciprocal(out=st[:, 2:3], in_=st[:, 1:2]), reads=[bst], writes=[bst])
            S.op("dve", lambda e: e.scalar_tensor_tensor(out=xn[:], in0=src, scalar=st[:, 2:3], in1=self.gB[:],
                                                         op0=ALU.mult, op1=ALU.mult),
                 reads=[bsrc, bst, self.bg], writes=[bxn])
            for b4 in range(4):
                ps, bps = self.ring.next()
                for j in range(4):
                    c = b4 * 4 + j
                    S.op("pe", lambda e, ps=ps, j=j, c=c: e.transpose(ps[:, j * 128:(j + 1) * 128], xn[:, c * 128:(c + 1) * 128], id_f[:]),
                         reads=[bxn, bconst], writes=[bps])
                src3 = ps[:].rearrange("p (c n) -> p c n", c=4)
                if dst_f is None:
                    evac_copy(dst_bf[:, b4 * 4:(b4 + 1) * 4, :], src3, [bps], [bdst])
                else:
                    flip[0] ^= 1
                    if flip[0]:
                        S.op("act", lambda e, b4=b4, src3=src3: e.activation(out=dst_f[:, b4 * 4:(b4 + 1) * 4, :], in_=src3, func=AF.Copy), reads=[bps], writes=[bdst_f])
                        S.op("act", lambda e, b4=b4, src3=src3: e.activation(out=dst_bf[:, b4 * 4:(b4 + 1) * 4, :], in_=src3, func=AF.Copy), reads=[bps], writes=[bdst])
                    else:
                        S.op("dve", lambda e, b4=b4, src3=src3: e.tensor_copy(out=dst_f[:, b4 * 4:(b4 + 1) * 4, :], in_=src3), reads=[bps], writes=[bdst_f])
                        S.op("dve", lambda e, b4=b4, src3=src3: e.tensor_copy(out=dst_bf[:, b4 * 4:(b4 + 1) * 4, :], in_=src3), reads=[bps], writes=[bdst])

    def wslice(w, r0, r1, c0, c1):
        return w[r0:r1, c0:c1].rearrange("(c p) n -> p c n", p=128)

    S.mute = limit < 1
    with nc.reset_on_exit():
        nt = NormT("n1", [0, 1, 2, 3, 4, 5, 6, 7])
        nt.load_gain(0)
        xts = Ring([(sb("n1x%d" % i, [128, D], F32), Buf()) for i in range(3)])
        hbs = Ring([(sb("n1h%d" % i, [128, NCH, 512], BF16), Buf()) for i in range(2)])
        for b in range(NBM):
            hb, bhb = hbs.next()
            for t in range(4):
                xt, bxt = xts.next()
                r0 = b * 512 + t * 128
                S.dma("sp", lambda e, xt=xt, r0=r0: e.dma_start(out=xt[:], in_=xseg[r0:r0 + 128, :]), writes=[bxt])
                nt.run(xt[:], bxt, hb[:, :, t * 128:(t + 1) * 128], bhb)
            S.dma("sp", lambda e, hb=hb, b=b: e.dma_start(out=hT1[b], in_=hb[:].rearrange("p c n -> p (c n)")),
                  reads=[bhb], writes=[b_hT1[b]])
        S.barrier()
        S.emit_all()

    def interleave(gens):
        gens = list(gens)
        while gens:
            for g in list(gens):
                try:
                    next(g)
                except StopIteration:
                    gens.remove(g)

    for p in range(4):
        S.mute = limit < 2 + p
        with nc.reset_on_exit():
            win = sb("win", [128, NCH, 5, 256], BF16)
            wout = sb("wout", [128, 4, D], BF16)
            pw = sb("pw", [128, 2, 256], BF16)
            bwin, bwout, bpw = [Buf() for _ in range(5)], [Buf() for _ in range(4)], Buf()
            for g5 in (0, 3, 1, 2, 4):
                c0 = g5 * 1024 + 256 * p
                S.dma("pool", lambda e, g5=g5, c0=c0: e.dma_start(out=win[:, :, g5, :], in_=wslice(w_in, 0, D, c0, c0 + 256)), writes=[bwin[g5]])
            S.dma("pool", lambda e: e.dma_start(out=pw[:], in_=pool_w[p].rearrange("(c q) n -> q c n", q=128)), writes=[bpw])
            for n in range(4):
                S.dma("pool", lambda e, n=n: e.dma_start(out=wout[:, 0:2, n * 512:(n + 1) * 512], in_=wslice(w_out, 256 * p, 256 * p + 256, n * 512, (n + 1) * 512)), writes=[bwout[n]])
                S.dma("pool", lambda e, n=n: e.dma_start(out=wout[:, 2:4, n * 512:(n + 1) * 512], in_=wslice(w_out, 1024 + 256 * p, 1024 + 256 * p + 256, n * 512, (n + 1) * 512)), writes=[bwout[n]])
            small = sb("msmall", [128, 64], F32)
            bsmall = Buf()
            S.dma("sp", lambda e: e.dma_start(out=small[:, 0:8], in_=pscale), writes=[bsmall])
            S.dma("sp", lambda e: e.dma_start(out=small[:, 8:24], in_=lbl), writes=[bsmall])
            S.dma("sp", lambda e: e.dma_start(out=small[:, 24:25], in_=hnorm), writes=[bsmall])
            S.op("dve", lambda e: e.tensor_tensor(out=small[:, 32:34], in0=small[:, 8 + 2 * p:10 + 2 * p], in1=small[:, 16 + 2 * p:18 + 2 * p], op=ALU.subtract),
                 reads=[bsmall], writes=[bsmall])
            S.op("act", lambda e: e.activation(out=small[:, 25:27], in_=small[:, 32:34], func=AF.Sigmoid), reads=[bsmall], writes=[bsmall])
            S.op("dve", lambda e: e.tensor_scalar(out=small[:, 27:29], in0=small[:, 25:27], scalar1=-1.0, scalar2=1.0, op0=ALU.mult, op1=ALU.add),
                 reads=[bsmall], writes=[bsmall])
            S.op("dve", lambda e: e.tensor_scalar(out=small[:, 29:31], in0=small[:, 27:29], scalar1=-1.0, scalar2=None, op0=ALU.mult),
                 reads=[bsmall], writes=[bsmall])
            rm_sb = sb("rm_sb", [128, 512], F32)
            bd_sb = sb("bd_sb", [128, 128], U32)
            ic_sb = sb("ic_sb", [128, 512], F32)
            bcm_ = Buf()
            S.dma("sp", lambda e: e.dma_start(out=rm_sb[:], in_=rmask), writes=[bcm_])
            S.dma("sp", lambda e: e.dma_start(out=bd_sb[:], in_=bdmask), writes=[bcm_])
            S.dma("sp", lambda e: e.dma_start(out=ic_sb[:], in_=invcnt[:, p * 512:(p + 1) * 512]), writes=[bcm_])

            hTr = Ring([(sb("mh%d" % i, [128, NCH, 512], BF16), Buf()) for i in range(2)])
            uT = sb("uT", [128, 2, 528], F32)
            b_uh, b_um = Buf(), Buf()
            s_a = sb("s_a", [128, 2, 528], F32)
            s_b = sb("s_b", [128, 2, 528], F32)
            bsa, bsb = Buf(), Buf()
            pooled = sb("pooled", [128, 2, 512], BF16)
            bpooled = Buf()
            tmpH = []
            for hh in range(2):
                d_ = {}
                for nm in ["q", "sig", "lf", "kk", "a", "at"]:
                    d_[nm] = sb("h%d%s" % (hh, nm), [128, 512], F32)
                    d_["b" + nm] = Buf()
                d_["e1"], d_["be1"] = d_["lf"], d_["blf"]
                d_["e2"], d_["be2"] = d_["sig"], d_["bsig"]
                tmpH.append(d_)
            sets = []
            for si in range(2):
                st_ = {"hd": []}
                for hh in range(2):
                    d_ = {}
                    for nm in ["qt", "ktT"]:
                        d_[nm] = sb("s%dh%d%s" % (si, hh, nm), [128, 512], BF16)
                        d_["b" + nm] = Buf()
                    d_["ktok"] = sb("s%dh%dktok" % (si, hh), [128, 4, 128], BF16)
                    d_["bktok"] = Buf()
                    d_["ex"] = sb("s%dh%dex" % (si, hh), [128, 32], F32)
                    d_["bex"] = Buf()
                    st_["hd"].append(d_)
                st_["gT"] = sb("s%dgT" % si, [128, 2, 512], F32)
                st_["bgT"] = [Buf(), Buf()]
                st_["v"] = sb("s%dv" % si, [128, 4, 256], BF16)
                st_["bv"] = Buf()
                st_["mixT"] = sb("s%dmixT" % si, [128, 4, 512], BF16)
                st_["bmix"] = [Buf() for _ in range(4)]
                sets.append(st_)
            rec = []
            for hh in range(2):
                d_ = {}
                d_["S"] = sb("h%dS" % hh, [128, 128], F32)
                d_["bS"] = Buf()
                d_["sref"] = Ring([(sb("h%dsr%d" % (hh, i), [128, 128], BF16), Buf()) for i in range(3)])
                d_["tst8"] = [(sb("h%dts%d" % (hh, i), [128, 128], F32), Buf()) for i in range(8)]
                d_["ssb4"] = [(sb("h%dss%d" % (hh, i), [128, 128], BF16), Buf()) for i in range(4)]
                for (tt, bb) in d_["ssb4"]:
                    S.op("pool", lambda e, tt=tt: e.memset(tt[:], 0.0), writes=[bb])
                S.op("pool", lambda e, d_=d_: e.memset(d_["S"][:], 0.0), writes=[d_["bS"]])
                rec.append(d_)
            o_sb = sb("o_sb", [128, 512], F32)
            sq_sb = sb("sq_sb", [128, 512], F32)
            rs_sb = sb("rs_sb", [128, 512], F32)
            bo, bsq, brs = Buf(), Buf(), Buf()
            accr = Ring([(sb("acc%d" % i, [128, D], F32), Buf()) for i in range(2)])
            ringA = psring([0, 1, 2, 3])
            PS_S, PS_O, PS_ST, PS_KT, PS_SS = 7, 4, 6, 7, 7

            def front(b):
                halo = b < HB
                st_ = sets[b % 2]
                mixT, bmix = st_["mixT"], st_["bmix"]
                v_sb, bv = st_["v"], st_["bv"]
                gT, bgT = st_["gT"], st_["bgT"]
                hT, bhT = hTr.next()
                S.dma("sp", lambda e, hT=hT, b=b: e.dma_start(out=hT[:].rearrange("p c n -> p (c n)"), in_=hT1[b]),
                      reads=[b_hT1[b]], writes=[bhT])
                if b == 0:
                    S.op("dve", lambda e: e.memset(uT[:, :, 0:16], 0.0), writes=[b_uh])
                else:
                    S.op("dve", lambda e: e.tensor_copy(out=uT[:, :, 0:16], in_=uT[:, :, 512:528]), reads=[b_um], writes=[b_uh])
                for j in range(2):
                    ps, bps = ringA.next()
                    mm_group(ps[:], bps, [(win[:, c, 0, j * 128:(j + 1) * 128], hT[:, c, :]) for c in range(NCH)], [bwin[0], bhT])
                    S.op("act", lambda e, ps=ps, j=j: e.activation(out=uT[:, j, 16:528], in_=ps[:], func=AF.Copy), reads=[bps], writes=[b_um])
                    yield
                for t in range(4):
                    ps, bps = ringA.next()
                    mm_group(ps[:, 0:256], bps, [(hT[:, c, t * 128:(t + 1) * 128], win[:, c, 3, :]) for c in range(NCH)], [bwin[3], bhT])
                    S.op("dve", lambda e, ps=ps, t=t: e.tensor_copy(out=v_sb[:, t, :], in_=ps[:, 0:256]), reads=[bps], writes=[bv])
                    if t % 2 == 1:
                        yield
                if not halo:
                    k = p + 1
                    src, bsrc = uT, None
                    for step in range(1, k + 1):
                        sh = 1 << (step - 1)
                        lo = 1 << step
                        dst, bdst = (s_a, bsa) if step % 2 == 1 else (s_b, bsb)
                        if step == 1:
                            S.op("pool", lambda e, dst=dst, lo=lo, sh=sh: e.tensor_tensor(out=dst[:, :, lo:528], in0=uT[:, :, lo:528], in1=uT[:, :, lo - sh:528 - sh], op=ALU.add),
                                 reads=[b_uh, b_um], writes=[bdst])
                        else:
                            S.op("pool", lambda e, dst=dst, src=src, lo=lo, sh=sh: e.tensor_tensor(out=dst[:, :, lo:528], in0=src[:, :, lo:528], in1=src[:, :, lo - sh:528 - sh], op=ALU.add),
                                 reads=[bsrc], writes=[bdst])
                        src, bsrc = dst, bdst
                    wnd = float(1 << k)
                    if b == HB:
                        other, bother = (s_b, bsb) if src is s_a else (s_a, bsa)
                        for j in range(2):
                            S.op("dve", lambda e, j=j, src=src, other=other: e.tensor_tensor(out=other[:, j, 16:528], in0=src[:, j, 16:528], in1=ic_sb[:], op=ALU.mult),
                                 reads=[bsrc, bcm_], writes=[bother])
                        S.op("dve", lambda e, other=other: e.tensor_tensor(out=pooled[:], in0=other[:, :, 16:528], in1=uT[:, :, 16:528], op=ALU.subtract),
                             reads=[bother, b_um], writes=[bpooled])
                    else:
                        S.op("dve", lambda e, src=src, wnd=wnd: e.scalar_tensor_tensor(out=pooled[:], in0=src[:, :, 16:528], scalar=1.0 / wnd, in1=uT[:, :, 16:528],
                                                                                   op0=ALU.mult, op1=ALU.subtract),
                             reads=[bsrc, b_um], writes=[bpooled])
                    yield
                for hh in range(2):
                    h_ = tmpH[hh]
                    o_ = st_["hd"][hh]
                    lb_ap = small[:, 25 + hh:26 + hh]
                    oml_ap = small[:, 27 + hh:28 + hh]
                    noml_ap = small[:, 29 + hh:30 + hh]
                    ps, bps = ringA.next()
                    mm_group(ps[:], bps, [(win[:, c, 2, hh * 128:(hh + 1) * 128], hT[:, c, :]) for c in range(NCH)], [bwin[2], bhT])
                    S.op("act", lambda e, ps=ps, h_=h_: e.activation(out=h_["sig"][:], in_=ps[:], func=AF.Sigmoid), reads=[bps], writes=[h_["bsig"]])
                    yield
                    ps, bps = ringA.next()
                    mm_group(ps[:], bps, [(win[:, c, 1, hh * 128:(hh + 1) * 128], hT[:, c, :]) for c in range(NCH)], [bwin[1], bhT])
                    S.op("act", lambda e, ps=ps, h_=h_: e.activation(out=h_["q"][:], in_=ps[:], func=AF.Silu), reads=[bps], writes=[h_["bq"]])
                    S.op("act", lambda e, h_=h_, oml_ap=oml_ap, lb_ap=lb_ap: e.activation(out=h_["lf"][:], in_=h_["sig"][:], func=AF.Ln, scale=oml_ap, bias=lb_ap),
                         reads=[h_["bsig"], bsmall], writes=[h_["blf"]])
                    S.op("dve", lambda e, h_=h_: e.tensor_tensor_scan(out=h_["a"][:], data0=rm_sb[:], data1=h_["lf"][:], initial=0.0, op0=ALU.mult, op1=ALU.add),
                         reads=[h_["blf"], bcm_], writes=[h_["ba"]])
                    yield
                    ps, bps = ringA.next()
                    mm_group(ps[:], bps, [(win[:, c, 4, hh * 128:(hh + 1) * 128], hT[:, c, :]) for c in range(NCH)], [bwin[4], bhT])
                    S.op("act", lambda e, ps=ps, hh=hh, gT=gT: e.activation(out=gT[:, hh, :], in_=ps[:], func=AF.Silu), reads=[bps], writes=[bgT[hh]])
                    a3 = h_["a"][:].rearrange("p (c n) -> p c n", c=8)
                    S.op("dve", lambda e, h_=h_, a3=a3: e.tensor_tensor(out=h_["at"][:].rearrange("p (c n) -> p c n", c=8), in0=a3, in1=a3[:, :, 31:32].to_broadcast([128, 8, 64]), op=ALU.subtract),
                         reads=[h_["ba"]], writes=[h_["bat"]])
                    S.op("pool", lambda e, h_=h_, noml_ap=noml_ap, oml_ap=oml_ap: e.tensor_scalar(out=h_["kk"][:], in0=h_["sig"][:], scalar1=noml_ap, scalar2=oml_ap, op0=ALU.mult, op1=ALU.add),
                         reads=[h_["bsig"], bsmall], writes=[h_["bkk"]])
                    yield
                    S.op("act", lambda e, h_=h_: e.activation(out=h_["e1"][:], in_=h_["at"][:], func=AF.Exp), reads=[h_["bat"]], writes=[h_["be1"]])
                    S.op("act", lambda e, h_=h_: e.activation(out=h_["e2"][:], in_=h_["at"][:], func=AF.Exp, scale=-1.0), reads=[h_["bat"]], writes=[h_["be2"]])
                    a16 = h_["a"][:].rearrange("p (c n) -> p c n", n=32)[:, :, 31:32]
                    S.op("act", lambda e, o_=o_, a16=a16: e.activation(out=o_["ex"][:, 0:16].rearrange("p (c n) -> p c n", n=1), in_=a16, func=AF.Exp),
                         reads=[h_["ba"]], writes=[o_["bex"]])
                    S.op("dve", lambda e, o_=o_, a3=a3: e.tensor_tensor(out=o_["ex"][:, 16:24].rearrange("p (c n) -> p c n", n=1), in0=a3[:, :, 63:64], in1=a3[:, :, 31:32], op=ALU.subtract),
                         reads=[h_["ba"]], writes=[o_["bex"]])
                    S.op("act", lambda e, o_=o_: e.activation(out=o_["ex"][:, 24:32], in_=o_["ex"][:, 16:24], func=AF.Exp), reads=[o_["bex"]], writes=[o_["bex"]])
                    yield
                    S.op("dve", lambda e, h_=h_, o_=o_: e.tensor_tensor(out=o_["qt"][:], in0=h_["q"][:], in1=h_["e1"][:], op=ALU.mult),
                         reads=[h_["bq"], h_["be1"]], writes=[o_["bqt"]])
                    S.op("pool", lambda e, h_=h_, o_=o_: e.tensor_tensor(out=o_["ktT"][:], in0=h_["kk"][:], in1=h_["e2"][:], op=ALU.mult),
                         reads=[h_["bkk"], h_["be2"]], writes=[o_["bktT"]])
                    yield
                    psb = PS[PS_KT][:].bitcast(BF16)
                    for t in range(4):
                        S.op("pe", lambda e, o_=o_, t=t, psb=psb: e.transpose(psb[:, t * 128:(t + 1) * 128], o_["ktT"][:, t * 128:(t + 1) * 128], id_b[:]),
                             reads=[o_["bktT"], bconst], writes=[BPS[PS_KT]])
                    S.op("act", lambda e, o_=o_, psb=psb: e.activation(out=o_["ktok"][:].rearrange("p t n -> p (t n)"), in_=psb[:, 0:512], func=AF.Copy),
                         reads=[BPS[PS_KT]], writes=[o_["bktok"]])
                    yield
                if not halo:
                    for oc in range(2):
                        ps, bps = ringA.next()
                        mm_group(ps[:], bps, [(pw[:, ic, oc * 128:(oc + 1) * 128], pooled[:, ic, :]) for ic in range(2)], [bpw, bpooled])
                        S.op("act", lambda e, ps=ps, oc=oc, mixT=mixT: e.activation(out=mixT[:, oc, :], in_=ps[:], func=AF.Copy, scale=small[:, 2 * p + oc:2 * p + oc + 1]),
                             reads=[bps, bsmall], writes=[bmix[oc]])
                    yield

            def back(b):
                halo = b < HB
                st_ = sets[b % 2]
                mixT, bmix = st_["mixT"], st_["bmix"]
                v_sb, bv = st_["v"], st_["bv"]
                gT, bgT = st_["gT"], st_["bgT"]
                for t in range(4):
                    for hh in range(2):
                        h_ = st_["hd"][hh]
                        r_ = rec[hh]
                        S.op("pe", lambda e, h_=h_, t=t: e.matmul(PS[PS_S][:, 0:128], lhsT=h_["ktT"][:, t * 128:(t + 1) * 128], rhs=h_["qt"][:, t * 128:(t + 1) * 128], start=True, stop=True),
                             reads=[h_["bktT"], h_["bqt"]], writes=[BPS[PS_S]])
                        ssb, bssb = r_["ssb4"][t]
                        S.op("dve", lambda e, ssb=ssb: e.copy_predicated(out=ssb[:], mask=bd_sb[:], data=PS[PS_S][:, 0:128]),
                             reads=[BPS[PS_S], bcm_], writes=[bssb])
                        for cc in range(2):
                            c = 2 * t + cc
                            S.op("pe", lambda e, h_=h_, t=t, cc=cc, hh=hh, v_sb=v_sb: e.matmul(PS[PS_ST][:, 0:128], lhsT=h_["ktok"][cc * 64:(cc + 1) * 64, t, :], rhs=v_sb[cc * 64:(cc + 1) * 64, t, hh * 128:(hh + 1) * 128], start=True, stop=True),
                                 reads=[h_["bktok"], bv], writes=[BPS[PS_ST]])
                            tst, btst = r_["tst8"][c]
                            S.op("act", lambda e, h_=h_, tst=tst, c=c: e.activation(out=tst[:], in_=PS[PS_ST][:, 0:128], func=AF.Copy, scale=h_["ex"][:, 24 + c:25 + c]),
                                 reads=[BPS[PS_ST], h_["bex"]], writes=[btst])
                        yield
                for t in range(4):
                    for hh in range(2):
                        h_ = st_["hd"][hh]
                        r_ = rec[hh]
                        pso, bpso = PS[PS_O + hh], BPS[PS_O + hh]
                        ssb, bssb = r_["ssb4"][t]
                        S.op("pe", lambda e, ssb=ssb, t=t, hh=hh, pso=pso, v_sb=v_sb: e.matmul(pso[:, t * 128:(t + 1) * 128], lhsT=v_sb[:, t, hh * 128:(hh + 1) * 128], rhs=ssb[:], start=True, stop=False),
                             reads=[bssb, bv], writes=[bpso])
                        for cc in range(2):
                            c = 2 * t + cc
                            sref, bsref = r_["sref"].next()
                            S.op("pool", lambda e, h_=h_, r_=r_, sref=sref, c=c: e.tensor_scalar(out=sref[:], in0=r_["S"][:], scalar1=h_["ex"][:, 2 * c:2 * c + 1], scalar2=0.0, op0=ALU.mult, op1=ALU.add),
                                 reads=[r_["bS"], h_["bex"]], writes=[bsref])
                            tst, btst = r_["tst8"][c]
                            S.op("pool", lambda e, h_=h_, r_=r_, c=c: e.tensor_scalar(out=r_["S"][:], in0=r_["S"][:], scalar1=h_["ex"][:, 2 * c + 1:2 * c + 2], scalar2=0.0, op0=ALU.mult, op1=ALU.add),
                                 reads=[r_["bS"], h_["bex"]], writes=[r_["bS"]])
                            S.op("pool", lambda e, r_=r_, tst=tst: e.tensor_tensor(out=r_["S"][:], in0=r_["S"][:], in1=tst[:], op=ALU.add),
                                 reads=[r_["bS"], btst], writes=[r_["bS"]])
                            c0 = t * 128 + cc * 64
                            S.op("pe", lambda e, h_=h_, sref=sref, c0=c0, pso=pso, cc=cc: e.matmul(pso[:, c0:c0 + 64], lhsT=sref[:], rhs=h_["qt"][:, c0:c0 + 64], start=False, stop=(cc == 1)),
                                 reads=[bsref, h_["bqt"]], writes=[bpso])
                        yield
                if halo:
                    return
                for hh in range(2):
                    pso, bpso = PS[PS_O + hh], BPS[PS_O + hh]
                    S.op("act", lambda e, pso=pso: e.activation(out=o_sb[:], in_=pso[:], func=AF.Copy), reads=[bpso], writes=[bo])
                    S.op("act", lambda e, pso=pso: e.activation(out=sq_sb[:], in_=pso[:], func=AF.Square), reads=[bpso], writes=[bsq])
                    yield
                    S.op("pe", lambda e: e.matmul(PS[PS_SS][:], lhsT=ones_f[:], rhs=sq_sb[:], start=True, stop=True), reads=[bsq, bconst], writes=[BPS[PS_SS]])
                    S.op("act", lambda e: e.activation(out=rs_sb[:], in_=PS[PS_SS][:], func=AF.Ln, scale=1.0 / 128, bias=eps_t[:]), reads=[BPS[PS_SS], bconst], writes=[brs])
                    S.op("act", lambda e: e.activation(out=rs_sb[:], in_=rs_sb[:], func=AF.Exp, scale=-0.5), reads=[brs], writes=[brs])
                    S.op("dve", lambda e: e.scalar_tensor_tensor(out=o_sb[:], in0=o_sb[:], scalar=small[:, 24:25], in1=rs_sb[:], op0=ALU.mult, op1=ALU.mult),
                         reads=[bo, brs, bsmall], writes=[bo])
                    S.op("dve", lambda e, hh=hh, mixT=mixT, gT=gT: e.tensor_tensor(out=mixT[:, 2 + hh, :], in0=o_sb[:], in1=gT[:, hh, :], op=ALU.mult),
                         reads=[bo, bgT[hh]], writes=[bmix[2 + hh]])
                    yield
                for t in range(4):
                    acc, bacc = accr.next()
                    ti = (b - HB) * 4 + t
                    if p == 0:
                        r0 = b * 512 + t * 128
                        S.dma("sp", lambda e, acc=acc, r0=r0: e.dma_start(out=acc[:], in_=xseg[r0:r0 + 128, :]), writes=[bacc])
                    else:
                        S.dma("sp", lambda e, acc=acc, ti=ti: e.dma_start(out=acc[:], in_=x1s[ti * 128:(ti + 1) * 128, :]), reads=[b_x1[ti]], writes=[bacc])
                    for n in range(4):
                        ps, bps = ringA.next()
                        mm_group(ps[:], bps, [(mixT[:, c, t * 128:(t + 1) * 128], wout[:, c, n * 512:(n + 1) * 512]) for c in range(4)], [bwout[n]] + bmix)
                        S.op("dve", lambda e, acc=acc, ps=ps, n=n: e.tensor_tensor(out=acc[:, n * 512:(n + 1) * 512], in0=ps[:], in1=acc[:, n * 512:(n + 1) * 512], op=ALU.add),
                             reads=[bps, bacc], writes=[bacc])
                        if n % 2 == 1 and not os.environ.get("NOYIELD_OP"):
                            yield
                    S.dma("sp", lambda e, acc=acc, ti=ti: e.dma_start(out=x1s[ti * 128:(ti + 1) * 128, :], in_=acc[:]), reads=[bacc], writes=[b_x1[ti]])
                    if dbg and p == 3:
                        bo_ = Buf()
                        S.dma("sp", lambda e, acc=acc, ti=ti: e.dma_start(out=x1d[ti * 128:(ti + 1) * 128, :], in_=acc[:]), reads=[bacc], writes=[bo_])
                        outs.append(bo_)

            interleave([front(0)])
            for b in range(1, NBM):
                interleave([back(b - 1), front(b)])
            interleave([back(NBM - 1)])
            S.barrier()
            S.emit_all()

    S.mute = limit < 6
    with nc.reset_on_exit():
        KT = sb("KT", [128, NCH, 256], BF16)
        V = sb("V", [128, 2, D], BF16)
        bKT, bV = Buf(), Buf()
        nt = NormT("x1n", [0, 1])
        ringB = psring([2, 3, 4, 5, 6, 7])
        with nc.reset_on_exit():
            nt.load_gain(4)
            mT = sb("mT", [128, NCH, 256], BF16)
            bmT = Buf()
            mts = Ring([(sb("memt%d" % i, [128, D], F32), Buf()) for i in range(2)])
            for mt in range(2):
                xt, bxt = mts.next()
                S.dma("sp", lambda e, xt=xt, mt=mt: e.dma_start(out=xt[:], in_=mem[mt * 128:(mt + 1) * 128, :]), writes=[bxt])
                nt.run(xt[:], bxt, mT[:, :, mt * 128:(mt + 1) * 128], bmT)
            wkr = Ring([(sb("wkv%d" % i, [128, NCH, 512], BF16), Buf()) for i in range(2)])
            for n in range(4):
                wt, bwt = wkr.next()
                S.dma("pool", lambda e, wt=wt, n=n: e.dma_start(out=wt[:], in_=wslice(wk, 0, D, n * 512, (n + 1) * 512)), writes=[bwt])
                for jj in range(4):
                    ps, bps = ringB.next()
                    mm_group(ps[:, 0:256], bps, [(wt[:, c, jj * 128:(jj + 1) * 128], mT[:, c, :]) for c in range(NCH)], [bwt, bmT])
                    evac_copy(KT[:, n * 4 + jj, :], ps[:, 0:256], [bps], [bKT])
            for n in range(4):
                wt, bwt = wkr.next()
                S.dma("pool", lambda e, wt=wt, n=n: e.dma_start(out=wt[:], in_=wslice(wv, 0, D, n * 512, (n + 1) * 512)), writes=[bwt])
                for mt in range(2):
                    ps, bps = ringB.next()
                    mm_group(ps[:], bps, [(mT[:, c, mt * 128:(mt + 1) * 128], wt[:, c, :]) for c in range(NCH)], [bwt, bmT])
                    evac_copy(V[:, mt, n * 512:(n + 1) * 512], ps[:], [bps], [bV])
            S.barrier()
            S.emit_all()
        nt.load_gain(1)
        wq_sb = sb("wq_sb", [128, NCH, D], BF16)
        bwq4 = [Buf() for _ in range(4)]
        for n in range(4):
            S.dma("pool", lambda e, n=n: e.dma_start(out=wq_sb[:, :, n * 512:(n + 1) * 512], in_=wslice(wq, 0, D, n * 512, (n + 1) * 512)), writes=[bwq4[n]])
        xts = Ring([(sb("x1x%d" % i, [128, D], F32), Buf()) for i in range(2)])
        h2T = sb("h2T", [128, NCH, 512], BF16)
        bh2 = Buf()
        qTs = [(sb("qT%d" % i, [128, NCH, 512], BF16), Buf()) for i in range(2)]
        aT, baT = sb("aT0", [128, NCH, 512], BF16), Buf()
        attr = Ring([(sb("pex%d" % i, [128, 256], F32), sb("pn%d" % i, [128, 256], BF16), sb("PT%d" % i, [128, 2, 128], BF16), sb("sst%d" % i, [128, 8], F32),
                      Buf(), Buf(), Buf(), Buf()) for i in range(3)])
        SC = 512 ** -0.5

        def x1_norm(b):
            for t in range(4):
                xt, bxt = xts.next()
                ti = b * 4 + t
                S.dma("sp", lambda e, xt=xt, ti=ti: e.dma_start(out=xt[:], in_=x1s[ti * 128:(ti + 1) * 128, :]), reads=[b_x1[ti]], writes=[bxt])
                nt.run(xt[:], bxt, h2T[:, :, t * 128:(t + 1) * 128], bh2)

        def x1_qgroup(b, j):
            qT, bqT = qTs[b % 2]
            ps, bps = ringB.next()
            mm_group(ps[:], bps, [(wq_sb[:, c, j * 128:(j + 1) * 128], h2T[:, c, :]) for c in range(NCH)], [bwq4[j // 4], bh2])
            evac_copy(qT[:, j, :], ps[:], [bps], [bqT])

        def x1_attnA(b, t, h):
            qT, bqT = qTs[b % 2]
            pex, pn, PT, sst, bpex, bpn, bPT, bsst = attr.next()
            ps, bps = ringB.next()
            mm_group(ps[:, 0:256], bps, [(qT[:, 4 * h + c4, t * 128:(t + 1) * 128], KT[:, 4 * h + c4, :]) for c4 in range(4)], [bqT, bKT])
            S.op("dve", lambda e, ps=ps, sst=sst: e.tensor_reduce(out=sst[:, 0:1], in_=ps[:, 0:256], axis=AX.X, op=ALU.max), reads=[bps], writes=[bsst])
            S.op("dve", lambda e, sst=sst: e.tensor_scalar(out=sst[:, 1:2], in0=sst[:, 0:1], scalar1=-SC, scalar2=None, op0=ALU.mult), reads=[bsst], writes=[bsst])
            S.op("act", lambda e, ps=ps, sst=sst, pex=pex: e.activation(out=pex[:], in_=ps[:, 0:256], func=AF.Exp, scale=SC, bias=sst[:, 1:2], accum_out=sst[:, 2:3]),
                 reads=[bps, bsst], writes=[bpex, bsst])
            S.op("dve", lambda e, sst=sst: e.reciprocal(out=sst[:, 3:4], in_=sst[:, 2:3]), reads=[bsst], writes=[bsst])
            S.op("dve", lambda e, sst=sst, pex=pex, pn=pn: e.tensor_scalar(out=pn[:], in0=pex[:], scalar1=sst[:, 3:4], scalar2=None, op0=ALU.mult), reads=[bpex, bsst], writes=[bpn])
            return (t, h, pn, bpn, PT, bPT)

        def x1_attnB(st):
            t, h, pn, bpn, PT, bPT = st
            ps2, bps2 = ringB.next()
            psb = ps2[:].bitcast(BF16)
            for mc in range(2):
                S.op("pe", lambda e, psb=psb, mc=mc, pn=pn: e.transpose(psb[:, mc * 128:(mc + 1) * 128], pn[:, mc * 128:(mc + 1) * 128], id_b[:]),
                     reads=[bpn, bconst], writes=[bps2])
            S.op("act", lambda e, psb=psb, PT=PT: e.activation(out=PT[:].rearrange("p c n -> p (c n)"), in_=psb[:, 0:256], func=AF.Copy), reads=[bps2], writes=[bPT])
            ps3, bps3 = ringB.next()
            for j in range(4):
                mm_group(ps3[:, j * 128:(j + 1) * 128], bps3,
                         [(V[:, mc, h * 512 + j * 128:h * 512 + (j + 1) * 128], PT[:, mc, :]) for mc in range(2)], [bV, bPT])
            evac_copy(aT[:, 4 * h:4 * h + 4, t * 128:(t + 1) * 128], ps3[:].rearrange("p (c n) -> p c n", c=4), [bps3], [baT])

        x1_norm(0)
        for j in range(NCH):
            x1_qgroup(0, j)
        for b in range(NB):
            if b + 1 < NB:
                x1_norm(b + 1)
            stA = x1_attnA(b, 0, 0)
            for k in range(16):
                stN = x1_attnA(b, (k + 1) // 4, (k + 1) % 4) if k + 1 < 16 else None
                if b + 1 < NB:
                    x1_qgroup(b + 1, k)
                x1_attnB(stA)
                stA = stN
            S.dma("sp", lambda e, b=b: e.dma_start(out=aTs[b], in_=aT[:].rearrange("p c n -> p (c n)")), reads=[baT], writes=[b_aT[b]])
        S.barrier()
        S.emit_all()

    S.mute = limit < 6.5
    with nc.reset_on_exit():
        nt = NormT("x2n", [0, 1, 2, 3])
        nt.load_gain(2)
        ringB = psring([4, 5, 6])
        wo_sb = sb("wo_sb", [128, NCH, D], BF16)
        bwo = Buf()
        for n in range(4):
            S.dma("pool", lambda e, n=n: e.dma_start(out=wo_sb[:, :, n * 512:(n + 1) * 512], in_=wslice(wo, 0, D, n * 512, (n + 1) * 512)), writes=[bwo])
        wr_sb = sb("wr_sb", [128, NCH, 20], F32)
        bwr = Buf()
        S.dma("sp", lambda e: e.dma_start(out=wr_sb[:].rearrange("p c n -> p (c n)"), in_=wr), writes=[bwr])
        aTr = Ring([(sb("aTi%d" % i, [128, NCH, 512], BF16), Buf()) for i in range(2)])
        xts = Ring([(sb("x2x%d" % i, [128, D], F32), Buf()) for i in range(3)])
        h3r = Ring([(sb("h3b%d" % i, [128, NCH, 512], BF16), Buf()) for i in range(2)])
        h3fr = Ring([(sb("h3f%d" % i, [128, NCH, 128], F32), Buf()) for i in range(2)])
        rtr = Ring([(sb("rt%d" % i, [128, 128], F32), Buf()) for i in range(2)])
        cmr = Ring([(sb("cmb%d" % i, [128, 16], F32), Buf()) for i in range(2)])
        def x2_normrouter(b, t, ti, xt, bxt, h3b, bh3b):
            h3f, bh3f = h3fr.next()
            rt, brt = rtr.next()
            nt.run(xt[:], bxt, h3b[:, :, t * 128:(t + 1) * 128], bh3b, h3f, bh3f)
            if limit == 6.5:
                S.mute = True
            psr, bpsr = PS[7], BPS[7]
            mm_group(psr[:, 0:20], bpsr, [(h3f[:, c, :], wr_sb[:, c, :]) for c in range(NCH)], [bh3f, bwr])
            cm, bcm = cmr.next()
            R_ = rt
            S.op("act", lambda e, R_=R_: e.activation(out=R_[:, 0:20], in_=psr[:, 0:20], func=AF.Copy), reads=[bpsr], writes=[brt])
            S.op("dve", lambda e, R_=R_: e.tensor_reduce(out=R_[:, 20:21], in_=R_[:, 0:4], axis=AX.X, op=ALU.max), reads=[brt], writes=[brt])
            S.op("dve", lambda e, R_=R_: e.tensor_scalar(out=R_[:, 21:22], in0=R_[:, 20:21], scalar1=-1.0, scalar2=None, op0=ALU.mult), reads=[brt], writes=[brt])
            S.op("act", lambda e, R_=R_: e.activation(out=R_[:, 24:28], in_=R_[:, 0:4], func=AF.Exp, bias=R_[:, 21:22], accum_out=R_[:, 22:23]), reads=[brt], writes=[brt])
            S.op("dve", lambda e, R_=R_: e.reciprocal(out=R_[:, 23:24], in_=R_[:, 22:23]), reads=[brt], writes=[brt])
            S.op("dve", lambda e, R_=R_: e.tensor_scalar(out=R_[:, 24:28], in0=R_[:, 0:4], scalar1=R_[:, 20:21], scalar2=None, op0=ALU.is_equal), reads=[brt], writes=[brt])
            S.op("dve", lambda e, R_=R_: e.tensor_scalar(out=R_[:, 24:28], in0=R_[:, 24:28], scalar1=-1.0, scalar2=1e30, op0=ALU.add, op1=ALU.mult), reads=[brt], writes=[brt])
            S.op("dve", lambda e, R_=R_: e.tensor_tensor(out=R_[:, 32:48].rearrange("p (g k) -> p g k", g=4), in0=R_[:, 4:20].rearrange("p (g k) -> p g k", g=4),
                                                  in1=R_[:, 24:28].rearrange("p (g k) -> p g k", k=1).to_broadcast([128, 4, 4]), op=ALU.add), reads=[brt], writes=[brt])
            S.op("dve", lambda e, R_=R_: e.max(out=R_[:, 48:56], in_=R_[:, 32:48]), reads=[brt], writes=[brt])
            S.op("dve", lambda e, R_=R_: e.tensor_tensor(out=R_[:, 56:57], in0=R_[:, 49:50], in1=R_[:, 48:49], op=ALU.subtract), reads=[brt], writes=[brt])
            S.op("act", lambda e, R_=R_: e.activation(out=R_[:, 57:58], in_=R_[:, 56:57], func=AF.Exp), reads=[brt], writes=[brt])
            S.op("dve", lambda e, R_=R_: e.tensor_scalar(out=R_[:, 58:59], in0=R_[:, 57:58], scalar1=1.0, scalar2=None, op0=ALU.add), reads=[brt], writes=[brt])
            S.op("dve", lambda e, R_=R_: e.reciprocal(out=R_[:, 59:60], in_=R_[:, 58:59]), reads=[brt], writes=[brt])
            S.op("dve", lambda e, R_=R_: e.tensor_tensor(out=R_[:, 60:61], in0=R_[:, 57:58], in1=R_[:, 59:60], op=ALU.mult), reads=[brt], writes=[brt])
            S.op("dve", lambda e, R_=R_: e.tensor_scalar(out=R_[:, 61:63], in0=R_[:, 59:61], scalar1=R_[:, 23:24], scalar2=None, op0=ALU.mult), reads=[brt], writes=[brt])
            S.op("dve", lambda e, R_=R_: e.tensor_scalar(out=R_[:, 64:80], in0=R_[:, 32:48], scalar1=R_[:, 48:49], scalar2=R_[:, 61:62], op0=ALU.is_equal, op1=ALU.mult), reads=[brt], writes=[brt])
            S.op("dve", lambda e, R_=R_: e.tensor_scalar(out=R_[:, 80:96], in0=R_[:, 32:48], scalar1=R_[:, 49:50], scalar2=R_[:, 62:63], op0=ALU.is_equal, op1=ALU.mult), reads=[brt], writes=[brt])
            S.op("dve", lambda e, cm=cm, R_=R_: e.tensor_tensor(out=cm[:], in0=R_[:, 64:80], in1=R_[:, 80:96], op=ALU.add), reads=[brt], writes=[bcm])
            S.dma("sp", lambda e, cm=cm, ti=ti: e.dma_start(out=combs[ti * 128:(ti + 1) * 128, :], in_=cm[:]), reads=[bcm], writes=[b_comb[ti]])
            if limit == 6.5:
                S.mute = False
            if t == 3:
                S.dma("sp", lambda e, h3b=h3b, b=b: e.dma_start(out=h3Ts[b], in_=h3b[:].rearrange("p c n -> p (c n)")), reads=[bh3b], writes=[b_h3T[b]])

        pend = None
        for b in range(NB):
            aT, baT = aTr.next()
            S.dma("sp", lambda e, aT=aT, b=b: e.dma_start(out=aT[:].rearrange("p c n -> p (c n)"), in_=aTs[b]), reads=[b_aT[b]], writes=[baT])
            h3b, bh3b = h3r.next()
            for t in range(4):
                ti = b * 4 + t
                xt, bxt = xts.next()
                S.dma("sp", lambda e, xt=xt, ti=ti: e.dma_start(out=xt[:], in_=x1s[ti * 128:(ti + 1) * 128, :]), reads=[b_x1[ti]], writes=[bxt])
                for n in range(4):
                    ps, bps = ringB.next()
                    mm_group(ps[:], bps, [(aT[:, c, t * 128:(t + 1) * 128], wo_sb[:, c, n * 512:(n + 1) * 512]) for c in range(NCH)], [bwo, baT])
                    S.op("dve", lambda e, xt=xt, ps=ps, n=n: e.tensor_tensor(out=xt[:, n * 512:(n + 1) * 512], in0=ps[:], in1=xt[:, n * 512:(n + 1) * 512], op=ALU.add),
                         reads=[bps, bxt], writes=[bxt])
                S.dma("sp", lambda e, xt=xt, ti=ti: e.dma_start(out=x2s[ti * 128:(ti + 1) * 128, :], in_=xt[:]), reads=[bxt], writes=[b_x2[ti]])
                if dbg:
                    bo_ = Buf()
                    S.dma("sp", lambda e, xt=xt, ti=ti: e.dma_start(out=x2d[ti * 128:(ti + 1) * 128, :], in_=xt[:]), reads=[bxt], writes=[bo_])
                    outs.append(bo_)
                if pend is not None:
                    x2_normrouter(*pend)
                pend = (b, t, ti, xt, bxt, h3b, bh3b)
        x2_normrouter(*pend)
        S.barrier()
        S.emit_all()

    S.mute = limit < 8
    with nc.reset_on_exit():
        h3T = sb("e_h3T", [128, NCH, EB], BF16)
        bh3 = Buf()
        y = sb("e_y", [128, ETI, D], F32)
        by = [Buf() for _ in range(ETI)]
        wring = Ring([(sb("e_w%d" % i, [128, 8192], BF16), Buf()) for i in range(5)])
        cmb = sb("e_cmb", [128, ETI, 16], F32)
        bcmb = Buf()
        sgr = Ring([(sb("e_sg%d" % i, [128, 512], F32), Buf()) for i in range(2)])
        hidr = Ring([(sb("e_hid%d" % i, [128, 512], BF16), Buf()) for i in range(2)])
        hTr_ = Ring([(sb("e_hT%d" % i, [128, 4, 128], BF16), Buf()) for i in range(2)])
        xts = Ring([(sb("e_x%d" % i, [128, D], F32), Buf()) for i in range(1)])
        gB = sb("e_gB", [128, D], F32)
        fst = sb("e_fst", [128, 4], F32)
        bjunk, bgB, bfst = Buf(), Buf(), Buf()
        S.dma("sp", lambda e: e.dma_start(out=gB[:], in_=gains[3:4, :].to_broadcast([128, D])), writes=[bgB])
        gur = Ring([((PS[0], BPS[0]), (PS[1], BPS[1])), ((PS[2], BPS[2]), (PS[3], BPS[3]))])
        ringY = psring([5, 6, 7])
        PS_T = 4
        pending_fn = None
        for eb in range(T // EB):
            for half in range(EB // 512):
                bi = eb * (EB // 512) + half
                S.dma("sp", lambda e, half=half, bi=bi: e.dma_start(out=h3T[:, :, half * 512:(half + 1) * 512], in_=h3Ts[bi].rearrange("p (c n) -> p c n", c=NCH)),
                      reads=[b_h3T[bi]], writes=[bh3])
            for i in range(ETI):
                ti = eb * ETI + i
                S.dma("sp", lambda e, i=i, ti=ti: e.dma_start(out=cmb[:, i, :], in_=combs[ti * 128:(ti + 1) * 128, :]), reads=[b_comb[ti]], writes=[bcmb])
            for ex in range(16):
                wg, bwg = wring.next()
                S.dma("pool", lambda e, wg=wg, ex=ex: e.dma_start(out=wg[:].rearrange("p (c n) -> p c n", c=NCH), in_=w_gate[ex].rearrange("(c p) n -> p c n", p=128)), writes=[bwg])
                wu, bwu = wring.next()
                S.dma("pool", lambda e, wu=wu, ex=ex: e.dma_start(out=wu[:].rearrange("p (c n) -> p c n", c=NCH), in_=w_up[ex].rearrange("(c p) n -> p c n", p=128)), writes=[bwu])
                wd, bwd = wring.next()
                for n in range(4):
                    S.dma("pool", lambda e, wd=wd, ex=ex, n=n: e.dma_start(out=wd[:].rearrange("p (c n) -> p c n", c=4)[:, :, n * 512:(n + 1) * 512],
                                                                    in_=w_down[ex][:, n * 512:(n + 1) * 512].rearrange("(c p) n -> p c n", p=128)), writes=[bwd])
                wg3 = wg[:].rearrange("p (c n) -> p c n", c=NCH)
                wu3 = wu[:].rearrange("p (c n) -> p c n", c=NCH)
                wd3 = wd[:].rearrange("p (c n) -> p c n", c=4)

                def GUg(i):
                    (pg, bpg), (pu, bpu) = gur.next()
                    mm_group(pg[:], bpg, [(h3T[:, c, i * 128:(i + 1) * 128], wg3[:, c, :]) for c in range(NCH)], [bh3, bwg])
                    return (pg, bpg, pu, bpu)

                def GUu(i, gu):
                    pg, bpg, pu, bpu = gu
                    mm_group(pu[:], bpu, [(h3T[:, c, i * 128:(i + 1) * 128], wu3[:, c, :]) for c in range(NCH)], [bh3, bwu])

                def HID(i, gu, ex=ex):
                    pg, bpg, pu, bpu = gu
                    sg, bsg = sgr.next()
                    S.op("act", lambda e: e.activation(out=sg[:], in_=pg[:], func=AF.Silu), reads=[bpg], writes=[bsg])
                    hid, bhid = hidr.next()
                    S.op("dve", lambda e: e.scalar_tensor_tensor(out=hid[:], in0=sg[:], scalar=cmb[:, i, ex:ex + 1], in1=pu[:], op0=ALU.mult, op1=ALU.mult),
                         reads=[bsg, bpu, bcmb], writes=[bhid])
                    return hid, bhid

                def TR(i, hb):
                    hid, bhid = hb
                    psb = PS[PS_T][:].bitcast(BF16)
                    for c4 in range(4):
                        S.op("pe", lambda e, c4=c4: e.transpose(psb[:, c4 * 128:(c4 + 1) * 128], hid[:, c4 * 128:(c4 + 1) * 128], id_b[:]),
                             reads=[bhid, bconst], writes=[BPS[PS_T]])
                    hT_, bhT_ = hTr_.next()
                    S.op("act", lambda e: e.activation(out=hT_[:].rearrange("p c n -> p (c n)"), in_=psb[:, 0:512], func=AF.Copy), reads=[BPS[PS_T]], writes=[bhT_])
                    return hT_, bhT_

                def DOWN(i, hb, ex=ex):
                    hT_, bhT_ = hb
                    for n in range(4):
                        ps, bps = ringY.next()
                        mm_group(ps[:], bps, [(hT_[:, c4, :], wd3[:, c4, n * 512:(n + 1) * 512]) for c4 in range(4)], [bhT_, bwd])
                        if ex == 0:
                            S.op("dve", lambda e, ps=ps, n=n: e.tensor_copy(out=y[:, i, n * 512:(n + 1) * 512], in_=ps[:]), reads=[bps], writes=[by[i]])
                        else:
                            S.op("dve", lambda e, ps=ps, n=n: e.tensor_tensor(out=y[:, i, n * 512:(n + 1) * 512], in0=ps[:], in1=y[:, i, n * 512:(n + 1) * 512], op=ALU.add),
                                 reads=[bps, by[i]], writes=[by[i]])

                g_cur = GUg(0)
                GUu(0, g_cur)
                for i in range(ETI):
                    hb = HID(i, g_cur)
                    g_next = GUg(i + 1) if i + 1 < ETI else None
                    if ex == 0 and pending_fn is not None:
                        pending_fn(i)
                    tb = TR(i, hb)
                    if g_next is not None:
                        GUu(i + 1, g_next)
                    DOWN(i, tb)
                    g_cur = g_next
                if ex == 0:
                    pending_fn = None

            def final_norm(i, eb=eb):
                ti = eb * ETI + i
                xt, bxt = xts.next()
                S.dma("sp", lambda e, xt=xt, ti=ti: e.dma_start(out=xt[:], in_=x2s[ti * 128:(ti + 1) * 128, :]), reads=[b_x2[ti]], writes=[bxt])
                S.op("dve", lambda e, xt=xt, i=i: e.tensor_tensor(out=y[:, i, :], in0=y[:, i, :], in1=xt[:], op=ALU.add), reads=[bxt, by[i]], writes=[by[i]])
                S.op("act", lambda e, i=i, xt=xt: e.activation(out=xt[:], in_=y[:, i, :], func=AF.Square, accum_out=fst[:, 0:1]), reads=[by[i]], writes=[bxt, bfst])
                S.op("act", lambda e: e.activation(out=fst[:, 1:2], in_=fst[:, 0:1], func=AF.Sqrt, scale=1.0 / D, bias=eps_t[:]), reads=[bfst, bconst], writes=[bfst])
                S.op("dve", lambda e: e.reciprocal(out=fst[:, 2:3], in_=fst[:, 1:2]), reads=[bfst], writes=[bfst])
                S.op("dve", lambda e, xt=xt, i=i: e.scalar_tensor_tensor(out=xt[:], in0=y[:, i, :], scalar=fst[:, 2:3], in1=gB[:], op0=ALU.mult, op1=ALU.mult),
                     reads=[by[i], bfst, bgB], writes=[bxt])
                bo_ = Buf()
                S.dma("sp", lambda e, xt=xt, ti=ti: e.dma_start(out=out[ti * 128:(ti + 1) * 128, :], in_=xt[:]), reads=[bxt], writes=[bo_])
                outs.append(bo_)

            if eb + 1 < T // EB:
                pending_fn = final_norm
            else:
                for i in range(ETI):
                    final_norm(i)
        S.mute = False
        S.final_wait("sp", outs)
        S.barrier()
        S.emit_all()
    return nc


_CACHE = {}


def _consts():
    s = np.arange(128)[:, None]
    t = np.arange(128)[None, :]
    bd = ((s // 64 == t // 64) & (s <= t)).astype(np.uint32)
    rm = np.ones((128, 512), np.float32)
    rm[:, ::64] = 0.0
    return np.eye(128, dtype=np.float32), bd, rm


def run(inputs, dbg=False, limit=99):
    x = np.asarray(inputs["x"], np.float32)
    B, SEQ, _ = x.shape
    SEG = 4
    T = SEQ // SEG
    W = HALO
    key = (T, W, dbg, limit)
    if key not in _CACHE:
        _CACHE[key] = build(T, W, dbg, limit)
    nc = _CACHE[key]
    f = lambda k: np.ascontiguousarray(np.asarray(inputs[k], np.float32))
    ident, bd, rm = _consts()
    gains = np.stack([f("norm_mix")[0], f("norm_xattn")[0], f("norm_ffn")[0], f("norm_final"), f("norm_mem")[0]], 0)
    pscale = np.ascontiguousarray(f("pool_scale")[0].reshape(8, 128).T)
    lbl = np.ascontiguousarray(f("hgrn_lb_logits").reshape(2, 8, 128).transpose(2, 0, 1).reshape(128, 16))
    hnorm = np.ascontiguousarray(f("hgrn_norm")[0].reshape(128, 1))
    wr = np.concatenate([f("router_group")[0], f("router_expert")[0]], axis=1)
    wr = np.ascontiguousarray(wr.reshape(NCH, 128, 20).transpose(1, 0, 2).reshape(128, NCH * 20))
    shared = {
        "w_in": f("w_in")[0], "pool_w": f("pool_w")[0], "w_out": f("w_out")[0],
        "wq": f("xattn_wq")[0], "wk": f("xattn_wk")[0], "wv": f("xattn_wv")[0], "wo": f("xattn_wo")[0],
        "wr": wr, "w_gate": f("w_gate")[0], "w_up": f("w_up")[0], "w_down": f("w_down")[0],
        "gains": np.ascontiguousarray(gains), "pscale": pscale, "lbl": lbl, "hnorm": hnorm,
        "ident": ident, "bdmask": bd, "rmask": rm,
    }
    memf = f("mem")
    in_maps = []
    for c in range(B * SEG):
        b, j = divmod(c, SEG)
        seg = np.zeros((W + T, D), np.float32)
        if j == 0:
            seg[W:] = x[b, 0:T]
        else:
            seg[:] = x[b, j * T - W:(j + 1) * T]
        ic = np.empty((4, 512), np.float32)
        for g, w_ in enumerate((2, 4, 8, 16)):
            if j == 0:
                ic[g] = 1.0 / np.minimum(np.arange(512) + 1, w_)
            else:
                ic[g] = 1.0 / w_
        m = dict(shared)
        m["xseg"] = seg
        m["mem"] = np.ascontiguousarray(memf[b])
        m["invcnt"] = np.ascontiguousarray(np.broadcast_to(ic.reshape(1, 4 * 512), (128, 4 * 512)))
        in_maps.append(m)
    res = run_bass_kernel_spmd(nc, in_maps, core_ids=list(range(B * SEG)))
    names = ["out"] + (["x1d", "x2d"] if dbg else [])
    outd = {}
    for nm in names:
        o = np.empty((B, SEQ, D), np.float32)
        for c in range(B * SEG):
            b, j = divmod(c, SEG)
            o[b, j * T:(j + 1) * T] = np.asarray(res.results[c][nm])
        outd[nm] = o
    return outd


def kernel(**inputs):
    return run(inputs)["out"]
```
